# Optimizing a Trainium2 kernel written in Bass

```python
import math
import jax, jax.numpy as jnp
from jax import lax
import numpy as np

D_MODEL = 2048
BATCH = 4
SEQ = 2048
DEPTH = 2
DEC_BATCH = 128
DEC_SEQ = 8
PAST_LEN = 16384
PAGE_SIZE = 128

MIX_W = D_MODEL // 2
HG_DK = 128
HG_HEADS = MIX_W // HG_DK
HG_DV = MIX_W // HG_HEADS
HG_CHUNK = 16
S5_CH = 16
S5_GROUPS = MIX_W // S5_CH
S5_STATE = 64
S5_DT_MIN = 1e-3
S5_DT_MAX = 1e-1
ML_HEADS = 4
ML_DH = MIX_W // ML_HEADS
ML_CHUNK = 64
CONV_W = 4
N_BRANCH = 3
D_FF = 256 * ((8 * D_MODEL // 3 + 255) // 256)
IN_WIDTHS = (MIX_W, MIX_W, MIX_W, MIX_W, MIX_W, MIX_W, MIX_W, N_BRANCH * D_MODEL)
N_IN = sum(IN_WIDTHS)
EPS = 1e-6
NEG_BIG = -1e30

kernel_name = 'hybrid_hgrn2_s5_mlstm_macaron_step'


def rmsnorm(x, g):
    x32 = x.astype(jnp.float32)
    y = x32 * lax.rsqrt(jnp.mean(x32 * x32, axis=-1, keepdims=True) + EPS)
    return (y * g.astype(jnp.float32)).astype(x.dtype)


def head_rms(x):
    return x * lax.rsqrt(jnp.mean(x * x, axis=-1, keepdims=True) + EPS)


def swiglu(h, w_up, w_down):
    a, b = jnp.split(jnp.einsum('bld,df->blf', h, w_up), 2, axis=-1)
    return jnp.einsum('blf,fd->bld', jax.nn.silu(a) * b, w_down)


def gla_chunked(q, k, v, logf, s0):
    bsz, seq, nh, _ = q.shape
    dv = v.shape[-1]
    pad = (-seq) % HG_CHUNK
    widths = ((0, 0), (0, pad), (0, 0), (0, 0))
    q, k, v, logf = (jnp.pad(a, widths) for a in (q, k, v, logf))
    nc = (seq + pad) // HG_CHUNK

    def blocks(a):
        return a.reshape(bsz, nc, HG_CHUNK, nh, a.shape[-1]).transpose(1, 0, 3, 2, 4)

    causal = jnp.tril(jnp.ones((HG_CHUNK, HG_CHUNK), dtype=bool))[:, :, None]

    def step(state, blk):
        qc, kc, vc, gc = blk
        b = jnp.cumsum(gc, axis=2)
        diff = b[:, :, :, None, :] - b[:, :, None, :, :]
        decay = jnp.where(causal, jnp.exp(jnp.where(causal, diff, 0.0)), 0.0)
        scores = jnp.einsum('bhtk,bhsk,bhtsk->bhts', qc, kc, decay)
        out = (jnp.einsum('bhts,bhsv->bhtv', scores, vc)
               + jnp.einsum('bhtk,bhkv->bhtv', qc * jnp.exp(b), state))
        b_end = b[:, :, -1:, :]
        state = (state * jnp.exp(b[:, :, -1, :])[..., None]
                 + jnp.einsum('bhsk,bhsv->bhkv', kc * jnp.exp(b_end - b), vc))
        return state, out

    s_final, out = lax.scan(step, s0, tuple(blocks(a) for a in (q, k, v, logf)))
    out = out.transpose(1, 0, 3, 2, 4).reshape(bsz, nc * HG_CHUNK, nh, dv)[:, :seq]
    return out, s_final


def mlstm_chunked(q, k, v, ig, logf, c0, n0, m0):
    bsz, nh, seq, dh = q.shape
    pad = (-seq) % ML_CHUNK
    pad4 = ((0, 0), (0, 0), (0, pad), (0, 0))
    pad3 = ((0, 0), (0, 0), (0, pad))
    q, k, v = (jnp.pad(a, pad4) for a in (q, k, v))
    logf = jnp.pad(logf, pad3)
    ig = jnp.pad(ig, pad3, constant_values=NEG_BIG)
    nc = (seq + pad) // ML_CHUNK

    def blocks(a):
        return jnp.moveaxis(a.reshape(a.shape[:2] + (nc, ML_CHUNK) + a.shape[3:]), 2, 0)

    causal = jnp.tril(jnp.ones((ML_CHUNK, ML_CHUNK), dtype=bool))

    def step(carry, blk):
        c, n, m = carry
        qc, kc, vc, ic, fc = blk
        b = jnp.cumsum(fc, axis=-1)
        logw = jnp.where(causal, b[..., :, None] - b[..., None, :] + ic[..., None, :], NEG_BIG)
        m_t = jnp.maximum(b + m[..., None], jnp.max(logw, axis=-1))
        w_prev = jnp.exp(b + m[..., None] - m_t)
        w = jnp.exp(logw - m_t[..., None])
        s = jnp.einsum('bhtd,bhsd->bhts', qc, kc) * w
        num = (w_prev[..., None] * jnp.einsum('bhtd,bhdv->bhtv', qc, c)
               + jnp.einsum('bhts,bhsv->bhtv', s, vc))
        den = w_prev * jnp.einsum('bhtd,bhd->bht', qc, n) + jnp.sum(s, axis=-1)
        h = num / jnp.maximum(jnp.abs(den), jnp.exp(-m_t))[..., None]
        m_end = m_t[..., -1]
        g_prev = jnp.exp(b[..., -1] + m - m_end)
        w_in = jnp.exp(b[..., -1:] - b + ic - m_end[..., None])
        c = g_prev[..., None, None] * c + jnp.einsum('bhs,bhsd,bhsv->bhdv', w_in, kc, vc)
        n = g_prev[..., None] * n + jnp.einsum('bhs,bhsd->bhd', w_in, kc)
        return (c, n, m_end), h

    (c, n, m), h = lax.scan(step, (c0, n0, m0), tuple(blocks(a) for a in (q, k, v, ig, logf)))
    h = jnp.moveaxis(h, 0, 2).reshape(bsz, nh, nc * ML_CHUNK, dh)[:, :, :seq]
    return h, c, n, m


def s5_scan(u, x0_re, x0_im, a_re, a_im, log_dt, b_re, b_im, c_re, c_im, d):
    f32 = jnp.float32
    a_re, a_im, b_re, b_im, c_re, c_im, d = (t.astype(f32) for t in (a_re, a_im, b_re, b_im, c_re, c_im, d))
    dt = jnp.exp(log_dt.astype(f32))[:, None]
    lam_re = jnp.minimum(a_re, -1e-4)
    lam_im = a_im
    mag = jnp.exp(lam_re * dt)
    ab_re = mag * jnp.cos(lam_im * dt)
    ab_im = mag * jnp.sin(lam_im * dt)
    inv = 1.0 / (lam_re * lam_re + lam_im * lam_im)
    f_re = ((ab_re - 1.0) * lam_re + ab_im * lam_im) * inv
    f_im = (ab_im * lam_re - (ab_re - 1.0) * lam_im) * inv
    bb_re = f_re[..., None] * b_re - f_im[..., None] * b_im
    bb_im = f_re[..., None] * b_im + f_im[..., None] * b_re
    bu_re = jnp.einsum('blgc,gpc->blgp', u, bb_re)
    bu_im = jnp.einsum('blgc,gpc->blgp', u, bb_im)
    bu_re = bu_re.at[:, 0].add(ab_re * x0_re - ab_im * x0_im)
    bu_im = bu_im.at[:, 0].add(ab_re * x0_im + ab_im * x0_re)
    a_rb = jnp.broadcast_to(ab_re, bu_re.shape)
    a_ib = jnp.broadcast_to(ab_im, bu_im.shape)

    def combine(e1, e2):
        a1r, a1i, b1r, b1i = e1
        a2r, a2i, b2r, b2i = e2
        return (a1r * a2r - a1i * a2i, a1r * a2i + a1i * a2r,
                a2r * b1r - a2i * b1i + b2r, a2r * b1i + a2i * b1r + b2i)

    _, _, xr, xi = lax.associative_scan(combine, (a_rb, a_ib, bu_re, bu_im), axis=1)
    y = (jnp.einsum('blgp,gcp->blgc', xr, c_re) - jnp.einsum('blgp,gcp->blgc', xi, c_im)
         + d * u)
    return y, xr[:, -1], xi[:, -1]


def causal_conv(x, buf, w, bias):
    seq = x.shape[1]
    xx = jnp.concatenate([buf, x], axis=1)
    y = bias
    for j in range(CONV_W):
        y = y + xx[:, j:j + seq] * w[j]
    return y, xx[:, seq:]


def mixer(h, l, st, p):
    f32 = jnp.float32
    s_hg, s_re, s_im, s_c, s_n, s_m, s_conv = (s.astype(f32) for s in st)
    bsz, seq, _ = h.shape
    cols = jnp.einsum('bld,de->ble', h, p['w_in'][l]).astype(f32)
    splits = np.cumsum(IN_WIDTHS)[:-1].tolist()
    hq, hf, hi, hg, su, mx, mo, gz = jnp.split(cols, splits, axis=-1)

    def heads(a, n):
        return a.reshape(bsz, seq, n, -1)

    lbs = jax.nn.softmax(p['hgrn_lower_bounds'].astype(f32), axis=0)
    lb = (jnp.cumsum(lbs, axis=0) - lbs[0])[l]
    forget = lb + (1.0 - lb) * jax.nn.sigmoid(hf)
    logf = jnp.log(forget)
    k_in = (1.0 - lb) * jax.nn.sigmoid(-hf)
    o, s_hg_new = gla_chunked(heads(jax.nn.silu(hq), HG_HEADS), heads(k_in, HG_HEADS),
                              heads(hi, HG_HEADS), heads(logf, HG_HEADS), s_hg)
    o = head_rms(o).reshape(bsz, seq, MIX_W) * p['hgrn_norm'][l] * jax.nn.silu(hg)
    br_a = jnp.einsum('blc,cd->bld', o, p['w_hgrn_out'][l])

    y, x_re, x_im = s5_scan(su.reshape(bsz, seq, S5_GROUPS, S5_CH), s_re, s_im,
                            p['s5_a_re'][l], p['s5_a_im'][l], p['s5_log_dt'][l],
                            p['s5_b_re'][l], p['s5_b_im'][l], p['s5_c_re'][l], p['s5_c_im'][l],
                            p['s5_d'][l])
    y = jax.nn.gelu(y.reshape(bsz, seq, MIX_W))
    br_b = (jnp.einsum('blc,cd->bld', y, p['w_s5_glu_a'][l])
            * jax.nn.sigmoid(jnp.einsum('blc,cd->bld', y, p['w_s5_glu_b'][l])))

    xc, conv_new = causal_conv(mx, s_conv, p['mlstm_conv_w'][l], p['mlstm_conv_b'][l])
    xc = jax.nn.silu(xc)
    q = jnp.einsum('blhd,hde->blhe', heads(xc, ML_HEADS), p['mlstm_wq'][l])
    k = jnp.einsum('blhd,hde->blhe', heads(xc, ML_HEADS), p['mlstm_wk'][l])
    v = jnp.einsum('blhd,hde->blhe', heads(mx, ML_HEADS), p['mlstm_wv'][l])
    gin = jnp.concatenate([q.reshape(bsz, seq, MIX_W), k.reshape(bsz, seq, MIX_W),
                           v.reshape(bsz, seq, MIX_W)], axis=-1)
    gates = (jnp.einsum('blc,cg->blg', gin, p['mlstm_w_gates'][l]) + p['mlstm_b_gates'][l]).astype(f32)
    ig = jnp.moveaxis(gates[..., :ML_HEADS], 2, 1)
    logf_m = jnp.moveaxis(jax.nn.log_sigmoid(gates[..., ML_HEADS:]), 2, 1)
    hm, c_new, n_new, m_new = mlstm_chunked(q.transpose(0, 2, 1, 3),
                                            k.transpose(0, 2, 1, 3) * ML_DH ** -0.5,
                                            v.transpose(0, 2, 1, 3), ig, logf_m, s_c, s_n, s_m)
    hm = (head_rms(hm).transpose(0, 2, 1, 3).reshape(bsz, seq, MIX_W)
          * p['mlstm_norm'][l] * jax.nn.sigmoid(mo))
    br_c = jnp.einsum('blc,cd->bld', hm, p['w_mlstm_out'][l])

    g = jax.nn.sigmoid(gz.reshape(bsz, seq, N_BRANCH, D_MODEL))
    merged = g[:, :, 0] * br_a + g[:, :, 1] * br_b + g[:, :, 2] * br_c
    out = jnp.einsum('bld,de->ble', merged, p['w_out'][l])
    return out, (s_hg_new, x_re, x_im, c_new, n_new, m_new, conv_new)


def layer(x, l, st, p):
    g = p['norm_gains'][l]
    ff1 = swiglu(rmsnorm(x, g[0]), p['w_ffn1_up'][l], p['w_ffn1_down'][l])
    x = x + (0.5 * rmsnorm(ff1, g[1])).astype(x.dtype)
    mix, new_st = mixer(rmsnorm(x, g[2]), l, st, p)
    x = x + rmsnorm(mix, g[3]).astype(x.dtype)
    ff2 = swiglu(rmsnorm(x, g[4]), p['w_ffn2_up'][l], p['w_ffn2_down'][l])
    x = x + (0.5 * rmsnorm(ff2, g[5])).astype(x.dtype)
    return x, new_st


def run_trunk(x, states, p):
    new = [[] for _ in states]
    for l in range(DEPTH):
        x, st = layer(x, l, tuple(s[l] for s in states), p)
        for lst, s in zip(new, st):
            lst.append(s)
    return x, tuple(jnp.stack(lst) for lst in new)


def zero_states(n):
    z = jnp.zeros
    f32 = jnp.float32
    return (z((DEPTH, n, HG_HEADS, HG_DK, HG_DV), f32),
            z((DEPTH, n, S5_GROUPS, S5_STATE), f32),
            z((DEPTH, n, S5_GROUPS, S5_STATE), f32),
            z((DEPTH, n, ML_HEADS, ML_DH, ML_DH), f32),
            z((DEPTH, n, ML_HEADS, ML_DH), f32),
            z((DEPTH, n, ML_HEADS), f32),
            z((DEPTH, n, CONV_W - 1, MIX_W), f32))


def setup_inputs(seed: int = 0) -> dict:
    key = jax.random.key(seed)
    ks = iter(jax.random.split(key, 64))
    f32 = jnp.float32

    def nrm(shape, scale):
        return scale * jax.random.normal(next(ks), shape, f32)

    L = DEPTH
    return {
        'x_prompt': nrm((BATCH, SEQ, D_MODEL), 1.0),
        'x_sample': nrm((DEC_BATCH, DEC_SEQ, D_MODEL), 1.0),
        'state_hgrn': nrm((L, DEC_BATCH, HG_HEADS, HG_DK, HG_DV), 0.5),
        'state_s5_re': nrm((L, DEC_BATCH, S5_GROUPS, S5_STATE), 0.1),
        'state_s5_im': nrm((L, DEC_BATCH, S5_GROUPS, S5_STATE), 0.1),
        'state_mlstm_c': nrm((L, DEC_BATCH, ML_HEADS, ML_DH, ML_DH), 0.05),
        'state_mlstm_n': nrm((L, DEC_BATCH, ML_HEADS, ML_DH), 0.1),
        'state_mlstm_m': nrm((L, DEC_BATCH, ML_HEADS), 1.0),
        'state_mlstm_conv': nrm((L, DEC_BATCH, CONV_W - 1, MIX_W), 1.0),
        'norm_gains': 1.0 + nrm((L, 6, D_MODEL), 0.01),
        'w_ffn1_up': nrm((L, D_MODEL, 2 * D_FF), D_MODEL ** -0.5),
        'w_ffn1_down': nrm((L, D_FF, D_MODEL), D_FF ** -0.5),
        'w_in': nrm((L, D_MODEL, N_IN), D_MODEL ** -0.5),
        'hgrn_lower_bounds': nrm((L, MIX_W), 0.1),
        'hgrn_norm': 1.0 + nrm((L, MIX_W), 0.01),
        'w_hgrn_out': nrm((L, MIX_W, D_MODEL), MIX_W ** -0.5),
        's5_a_re': -0.5 + nrm((L, S5_GROUPS, S5_STATE), 0.01),
        's5_a_im': jnp.broadcast_to(math.pi * jnp.arange(S5_STATE, dtype=f32), (L, S5_GROUPS, S5_STATE))
                   + nrm((L, S5_GROUPS, S5_STATE), 0.01),
        's5_log_dt': jax.random.uniform(next(ks), (L, S5_GROUPS), f32,
                                        math.log(S5_DT_MIN), math.log(S5_DT_MAX)),
        's5_b_re': nrm((L, S5_GROUPS, S5_STATE, S5_CH), (2 * S5_CH) ** -0.5),
        's5_b_im': nrm((L, S5_GROUPS, S5_STATE, S5_CH), (2 * S5_CH) ** -0.5),
        's5_c_re': nrm((L, S5_GROUPS, S5_CH, S5_STATE), (2 * S5_STATE) ** -0.5),
        's5_c_im': nrm((L, S5_GROUPS, S5_CH, S5_STATE), (2 * S5_STATE) ** -0.5),
        's5_d': nrm((L, S5_GROUPS, S5_CH), 1.0),
        'w_s5_glu_a': nrm((L, MIX_W, D_MODEL), MIX_W ** -0.5),
        'w_s5_glu_b': nrm((L, MIX_W, D_MODEL), MIX_W ** -0.5),
        'mlstm_conv_w': nrm((L, CONV_W, MIX_W), CONV_W ** -0.5),
        'mlstm_conv_b': nrm((L, MIX_W), 0.01),
        'mlstm_wq': nrm((L, ML_HEADS, ML_DH, ML_DH), ML_DH ** -0.5),
        'mlstm_wk': nrm((L, ML_HEADS, ML_DH, ML_DH), ML_DH ** -0.5),
        'mlstm_wv': nrm((L, ML_HEADS, ML_DH, ML_DH), ML_DH ** -0.5),
        'mlstm_w_gates': nrm((L, 3 * MIX_W, 2 * ML_HEADS), 0.1 * (3 * MIX_W) ** -0.5),
        'mlstm_b_gates': jnp.concatenate(
            [nrm((L, ML_HEADS), 0.1),
             jnp.linspace(3.0, 6.0, ML_HEADS, dtype=f32)[None] + nrm((L, ML_HEADS), 0.01)], axis=-1),
        'mlstm_norm': 1.0 + nrm((L, MIX_W), 0.01),
        'w_mlstm_out': nrm((L, MIX_W, D_MODEL), MIX_W ** -0.5),
        'w_out': nrm((L, D_MODEL, D_MODEL), D_MODEL ** -0.5),
        'w_ffn2_up': nrm((L, D_MODEL, 2 * D_FF), D_MODEL ** -0.5),
        'w_ffn2_down': nrm((L, D_FF, D_MODEL), D_FF ** -0.5),
    }


def reference(x_prompt, x_sample, state_hgrn, state_s5_re, state_s5_im, state_mlstm_c,
              state_mlstm_n, state_mlstm_m, state_mlstm_conv, norm_gains, w_ffn1_up, w_ffn1_down,
              w_in, hgrn_lower_bounds, hgrn_norm, w_hgrn_out, s5_a_re, s5_a_im, s5_log_dt,
              s5_b_re, s5_b_im, s5_c_re, s5_c_im, s5_d, w_s5_glu_a, w_s5_glu_b, mlstm_conv_w,
              mlstm_conv_b, mlstm_wq, mlstm_wk, mlstm_wv, mlstm_w_gates, mlstm_b_gates, mlstm_norm,
              w_mlstm_out, w_out, w_ffn2_up, w_ffn2_down):
    p = dict(norm_gains=norm_gains, w_ffn1_up=w_ffn1_up, w_ffn1_down=w_ffn1_down, w_in=w_in,
             hgrn_lower_bounds=hgrn_lower_bounds, hgrn_norm=hgrn_norm, w_hgrn_out=w_hgrn_out,
             s5_a_re=s5_a_re, s5_a_im=s5_a_im, s5_log_dt=s5_log_dt, s5_b_re=s5_b_re,
             s5_b_im=s5_b_im, s5_c_re=s5_c_re, s5_c_im=s5_c_im, s5_d=s5_d,
             w_s5_glu_a=w_s5_glu_a, w_s5_glu_b=w_s5_glu_b, mlstm_conv_w=mlstm_conv_w,
             mlstm_conv_b=mlstm_conv_b, mlstm_wq=mlstm_wq, mlstm_wk=mlstm_wk, mlstm_wv=mlstm_wv,
             mlstm_w_gates=mlstm_w_gates, mlstm_b_gates=mlstm_b_gates, mlstm_norm=mlstm_norm,
             w_mlstm_out=w_mlstm_out, w_out=w_out, w_ffn2_up=w_ffn2_up, w_ffn2_down=w_ffn2_down)
    y_prompt, (p_hg, p_re, p_im, p_c, p_n, p_m, p_conv) = run_trunk(x_prompt, zero_states(BATCH), p)
    sample_states = (state_hgrn, state_s5_re, state_s5_im, state_mlstm_c, state_mlstm_n,
                     state_mlstm_m, state_mlstm_conv)
    y_sample, (s_hg, s_re, s_im, s_c, s_n, s_m, s_conv) = run_trunk(x_sample, sample_states, p)
    return (y_prompt, y_sample, p_hg, p_re, p_im, p_c, p_n, p_m, p_conv,
            s_hg, s_re, s_im, s_c, s_n, s_m, s_conv)
```

```python
import contextlib
import math
import numpy as np
import concourse.bass as bass
import concourse.mybir as mybir
from concourse.bass_utils import run_bass_kernel_spmd

F32 = mybir.dt.float32
BF16 = mybir.dt.bfloat16
U8 = mybir.dt.uint8
AF = mybir.ActivationFunctionType
ALU = mybir.AluOpType
AX = mybir.AxisListType

D = 2048
DC = 16
FFD = 5632
FC = 44
MIX = 1024
NIN = 13312
EPS = 1e-6
NEG = -1e30
EPOCH = 20000


class Buf:
    __slots__ = ("name", "w", "r")

    def __init__(self, name):
        self.name = name
        self.w = None
        self.r = []


class Op:
    __slots__ = ("eng", "fn", "deps", "idx", "dma_key", "dma_cnt", "flag", "ordinal", "exempt")

    def __init__(self, eng, fn, idx):
        self.eng = eng
        self.fn = fn
        self.deps = set()
        self.idx = idx
        self.dma_key = None
        self.dma_cnt = 0
        self.flag = False
        self.ordinal = 0
        self.exempt = False


class Prog:
    PHYS = {"pe": "tensor", "act": "scalar", "dve": "vector", "pool": "gpsimd",
            "sp": "sync", "actq": "scalar", "poolq": "gpsimd"}

    def __init__(self, nc):
        self.nc = nc
        self.ops = []
        self.dma_counts = {}
        self.bufs = {}
        self.barrier_idx = None
        self.last_by_phys = {}
        self.pending_barrier = {}
        self.dmas_since = []

    def buf(self, name):
        b = self.bufs.get(name)
        if b is None:
            b = self.bufs[name] = Buf(name)
        return b

    def _track(self, op, reads, writes):
        for r in reads:
            r = self.buf(r) if isinstance(r, str) else r
            if r.w is not None:
                op.deps.add(r.w)
        for w in writes:
            w = self.buf(w) if isinstance(w, str) else w
            if w.w is not None:
                op.deps.add(w.w)
            for rr in w.r:
                op.deps.add(rr)
        for w in writes:
            w = self.buf(w) if isinstance(w, str) else w
            w.w = op.idx
            w.r = []
        for r in reads:
            r = self.buf(r) if isinstance(r, str) else r
            if r.w != op.idx:
                r.r.append(op.idx)
        op.deps.discard(op.idx)
        p = self.PHYS[op.eng]
        if not op.exempt:
            pend = self.pending_barrier.pop(p, None)
            if pend:
                op.deps.update(pend)
            if op.dma_key is None:
                self.last_by_phys[p] = op.idx
            else:
                self.dmas_since.append(op.idx)
        if op.eng == "pe":
            op.deps = set(d for d in op.deps if self.ops[d].eng != "pe")

    def barrier(self):
        lasts = set(self.last_by_phys.values()) | set(self.dmas_since)
        self.dmas_since = []
        for p in set(self.PHYS.values()):
            cur = self.pending_barrier.get(p, set())
            self.pending_barrier[p] = cur | lasts

    def op(self, eng, fn, reads=(), writes=()):
        o = Op(eng, fn, len(self.ops))
        self.ops.append(o)
        self._track(o, reads, writes)
        return o

    def dma(self, q, fn, reads=(), writes=(), key=None, exempt=False):
        o = Op(q, fn, len(self.ops))
        o.exempt = exempt
        if key is None:
            w0 = writes[0] if writes else reads[0]
            key = w0 if isinstance(w0, str) else w0.name
        o.dma_key = key
        self.dma_counts[key] = self.dma_counts.get(key, 0) + 16
        o.dma_cnt = self.dma_counts[key]
        self.ops.append(o)
        self._track(o, reads, writes)
        return o

    def emit(self, final_eng="sp"):
        nc = self.nc
        ops = self.ops
        phys_of = lambda o: self.PHYS[o.eng]
        for o in ops:
            for d in o.deps:
                ops[d].flag = True
        counters = {}
        for o in ops:
            if o.dma_key is None and o.flag:
                p = phys_of(o)
                counters[p] = counters.get(p, 0) + 1
                o.ordinal = counters[p]
        es = contextlib.ExitStack()
        sems = {}

        def getsem(name):
            s = sems.get(name)
            if s is None:
                s = sems[name] = es.enter_context(nc.semaphore("s_%d" % len(sems)))
            return s

        streams = {}
        for o in ops:
            streams.setdefault(phys_of(o), []).append(o)
        for p, n in counters.items():
            for e in range((n // EPOCH) + 1):
                getsem((p, e))
        for k in self.dma_counts:
            getsem(("dma", k))

        def sem_target(d):
            po = ops[d]
            if po.dma_key is not None:
                return ("dma", po.dma_key), po.dma_cnt
            p = phys_of(po)
            e, c = divmod(po.ordinal, EPOCH)
            if c == 0:
                e -= 1
                c = EPOCH
            return (p, e), c

        final_waits = {}
        for o in ops:
            if o.dma_key is not None:
                k = ("dma", o.dma_key)
                final_waits[k] = max(final_waits.get(k, 0), o.dma_cnt)
        fin_phys = self.PHYS[final_eng]

        with es:
            with nc.Block() as block:
                def make_section(p, lst):
                    def section(eng):
                        waited = {}
                        for o in lst:
                            need = {}
                            for d in o.deps:
                                k, c = sem_target(d)
                                if need.get(k, 0) < c:
                                    need[k] = c
                            for k, c in need.items():
                                if waited.get(k, 0) >= c:
                                    continue
                                eng.wait_ge(sems[k], c)
                                waited[k] = c
                            ins = o.fn(eng)
                            if o.dma_key is not None:
                                ins.then_inc(sems[("dma", o.dma_key)], 16)
                            elif o.flag:
                                k, c = sem_target(o.idx)
                                ins.then_inc(sems[k], 1)
                        if p == fin_phys:
                            for k, c in final_waits.items():
                                if waited.get(k, 0) < c:
                                    eng.wait_ge(sems[k], c)
                    return section

                for p, lst in streams.items():
                    getattr(block, p)(make_section(p, lst))
                if fin_phys not in streams:
                    getattr(block, fin_phys)(make_section(fin_phys, []))
        return len(ops), len(sems)


def make_consts():
    c = {}
    c["ident"] = np.eye(128, dtype=np.float32)
    t = np.arange(128)
    for mode, clen_h, slen in (("p", 32, 128), ("s", 8, 8)):
        T = 512 if mode == "p" else 128
        same = (t[:, None] // clen_h) == (t[None, :] // clen_h)
        c["hgm_" + mode] = (same & (t[:, None] <= t[None, :])).astype(np.float32)
        tt = np.arange(T)
        c["cst_" + mode] = np.broadcast_to(((tt % clen_h) != 0).astype(np.float32), (128, T)).copy()
        ns = 128 // clen_h
        c["crow_" + mode] = ((t[:, None] // clen_h) == np.arange(ns)[None, :]).astype(np.float32)
        sames = (t[:, None] // slen) == (t[None, :] // slen)
        valid_ts = sames & (t[None, :] <= t[:, None])
        c["neg_" + mode] = np.where(valid_ts, 0.0, NEG).astype(np.float32)
        c["tri_" + mode] = valid_ts.T.astype(np.float32)
        nsl = 128 // slen
        last = ((t[:, None] % slen) == slen - 1) & ((t[:, None] // slen) == np.arange(nsl)[None, :])
        c["last_" + mode] = last.astype(np.float32)
        lastrow = (t % slen) == slen - 1
        c["G_" + mode] = (sames & lastrow[:, None]).astype(np.float32)
        c["E_" + mode] = ((np.arange(nsl)[:, None]) == (t[None, :] // slen)).astype(np.float32)
        c["smask_" + mode] = ((t[:, None] // slen) == np.arange(nsl)[None, :]).astype(np.float32)
    c["s5st_p"] = np.broadcast_to((np.arange(512) != 0).astype(np.float32), (128, 512)).copy()
    c["s5st_s"] = np.broadcast_to(((np.arange(128) % 8) != 0).astype(np.float32), (128, 128)).copy()
    g2 = (np.arange(128) // 64)[:, None, None]
    j = np.arange(4)[None, :, None]
    gl = (np.arange(128) // 16)[None, None, :]
    c["s5cm"] = (gl == 2 * j + g2).astype(np.float32)
    return c


CONST_SHAPES = None


class Region:
    def __init__(self, start, size):
        self.start = start
        self.size = size
        self.cur = start

    def take(self, nbytes):
        nbytes = (nbytes + 63) // 64 * 64
        off = self.cur
        self.cur += nbytes
        assert self.cur <= self.start + self.size, ("region overflow", self.cur - self.start, self.size)
        return off

    def mark(self):
        return self.cur

    def reset(self, m=None):
        self.cur = self.start if m is None else m


def _prod(s):
    r = 1
    for v in s:
        r *= v
    return r


def build_program(NT=4, SAMPLE=True, DBG=None, NL=2, S5_POOL="dve"):
    nc = bass.Bass("TRN2", target_bir_lowering=False)
    P = Prog(nc)
    consts = make_consts()
    dbg_outs = {}

    def din(name, shape, dt=F32):
        return nc.dram_tensor(name, list(shape), dt, kind="ExternalInput").ap()

    def dout(name, shape, dt=F32):
        return nc.dram_tensor(name, list(shape), dt, kind="ExternalOutput").ap()

    def dint(name, shape, dt=F32):
        return nc.dram_tensor(name, list(shape), dt, kind="Internal").ap()

    TP = NT * 512
    xp = din("xp", [max(TP, 1), D])
    yp = dout("yp", [max(TP, 1), D])
    p_hg = dout("p_hg", [2, 8, 128, 128])
    p_re = dout("p_re", [2, 32, 128])
    p_im = dout("p_im", [2, 32, 128])
    p_c = dout("p_c", [2, 4, 256, 256])
    p_n = dout("p_n", [2, 4, 256])
    p_m = dout("p_m", [2, 4])
    p_conv = dout("p_conv", [2, 3, 1024])
    xs = din("xs", [128, D])
    ys = dout("ys", [128, D])
    st_hg = din("st_hg", [2, 16, 8, 128, 128])
    st_re = din("st_re", [2, 16, 4096])
    st_im = din("st_im", [2, 16, 4096])
    st_c = din("st_c", [2, 16, 4, 256, 256])
    st_n = din("st_n", [2, 16, 4, 256])
    st_m = din("st_m", [2, 16, 4])
    st_conv = din("st_conv", [2, 16, 3, 1024])
    s_hg = dout("s_hg", [2, 16, 8, 128, 128])
    s_re = dout("s_re", [2, 16, 4096])
    s_im = dout("s_im", [2, 16, 4096])
    s_c = dout("s_c", [2, 16, 4, 256, 256])
    s_n = dout("s_n", [2, 16, 4, 256])
    s_m = dout("s_m", [2, 16, 4])
    s_conv = dout("s_conv", [2, 16, 3, 1024])
    W = {}
    for name, shape in (("norm_gains", [2, 6, D]), ("w_ffn1_up", [2, D, 2 * FFD]), ("w_ffn1_down", [2, FFD, D]),
                        ("w_in", [2, D, NIN]), ("hgrn_lower_bounds", [2, MIX]), ("hgrn_norm", [2, MIX]),
                        ("w_hgrn_out", [2, MIX, D]), ("s5_a_re", [2, 64, 64]), ("s5_a_im", [2, 64, 64]),
                        ("s5_log_dt", [2, 64]), ("s5_b_re", [2, 64, 64, 16]), ("s5_b_im", [2, 64, 64, 16]),
                        ("s5_c_re", [2, 64, 16, 64]), ("s5_c_im", [2, 64, 16, 64]), ("s5_d", [2, 64, 16]),
                        ("w_s5_glu_a", [2, MIX, D]), ("w_s5_glu_b", [2, MIX, D]), ("mlstm_conv_w", [2, 4, MIX]),
                        ("mlstm_conv_b", [2, MIX]), ("mlstm_wq", [2, 4, 256, 256]), ("mlstm_wk", [2, 4, 256, 256]),
                        ("mlstm_wv", [2, 4, 256, 256]), ("mlstm_w_gates", [2, 3 * MIX, 8]),
                        ("mlstm_b_gates", [2, 8]), ("mlstm_norm", [2, MIX]), ("w_mlstm_out", [2, MIX, D]),
                        ("w_out", [2, D, D]), ("w_ffn2_up", [2, D, 2 * FFD]), ("w_ffn2_down", [2, FFD, D])):
        W[name] = din(name, shape)
    CD = {k: din("c_" + k, v.shape) for k, v in consts.items()}
    s5wb = dint("s5wb", [2, 8, 128, 2, 512])
    s5wc = dint("s5wc", [2, 8, 128, 2, 512])
    s5tab = dint("s5tab", [2, 8, 128, 2, 4, 512])

    ARENA = 211968
    arena = nc.alloc_sbuf_tensor("arena", [128, ARENA], U8)
    base = nc.lookup_mloc(arena).addr
    uid = [0]

    def sbt(shape, dtype, off):
        uid[0] += 1
        return nc.alloc_sbuf_tensor_at("t%d" % uid[0], list(shape), dtype, offset=base + off)

    def nbytes(shape, dtype):
        return _prod(shape[1:]) * (2 if dtype == BF16 else 4)

    def alloc(reg, shape, dtype=F32):
        return sbt(shape, dtype, reg.take(nbytes(shape, dtype)))

    R_CONST = Region(0, 19456)
    R_RING = Region(R_CONST.start + R_CONST.size, 3 * 8192)
    R_MODE = Region(R_RING.start + R_RING.size, ARENA - (R_RING.start + R_RING.size))

    banks = [nc.alloc_psum_tensor("pb%d" % i, [128, 512], F32) for i in range(8)]
    banks_b = [b.bitcast(BF16) for b in banks]
    PB = ["pb%d" % i for i in range(8)]

    def ACT(out, in_, func, reads, writes, **kw):
        P.op("act", lambda e: e.activation(out=out, in_=in_, func=func, **kw), reads, writes)

    def TT(out, in0, in1, op, reads, writes, eng="dve"):
        P.op(eng, lambda e: e.tensor_tensor(out=out, in0=in0, in1=in1, op=op), reads, writes)

    def TS(out, in0, s1, s2, op0, op1, reads, writes, eng="dve"):
        if op1 is None:
            P.op(eng, lambda e: e.tensor_scalar(out=out, in0=in0, scalar1=s1, scalar2=None, op0=op0), reads, writes)
        else:
            P.op(eng, lambda e: e.tensor_scalar(out=out, in0=in0, scalar1=s1, scalar2=s2, op0=op0, op1=op1), reads, writes)

    def STT(out, in0, scalar, in1, op0, op1, reads, writes, eng="dve"):
        P.op(eng, lambda e: e.scalar_tensor_tensor(out=out, in0=in0, scalar=scalar, in1=in1, op0=op0, op1=op1), reads, writes)

    def COPY(out, in_, reads, writes, eng="act"):
        if eng == "act":
            P.op("act", lambda e: e.activation(out=out, in_=in_, func=AF.Copy), reads, writes)
        else:
            P.op(eng, lambda e: e.tensor_copy(out=out, in_=in_), reads, writes)

    def MM(out, pairs, reads, writes, start=True, stop=True, skip=False):
        def f(e):
            n = len(pairs)
            ins = None
            for i, (l, r) in enumerate(pairs):
                kw = {}
                if skip:
                    kw["skip_group_check"] = True
                ins = e.matmul(out, l, r, start=(start and i == 0), stop=(stop and i == n - 1), **kw)
            return ins
        P.op("pe", f, reads, writes)

    def TR(out, in_, ident_ap, reads, writes):
        P.op("pe", lambda e: e.transpose(out, in_, ident_ap), reads, writes)

    def DMA(out, in_, reads, writes, q="sp", key=None, exempt=False, slow=False):
        if slow:
            P.dma(q, lambda e: e.dma_start(out=out, in_=in_, allow_slow_non_contiguous=True), reads, writes, key=key, exempt=exempt)
        else:
            P.dma(q, lambda e: e.dma_start(out=out, in_=in_), reads, writes, key=key, exempt=exempt)

    def interleave(gens):
        gens = list(gens)
        while gens:
            for g in list(gens):
                try:
                    next(g)
                except StopIteration:
                    gens.remove(g)

    def MEMSET(ap, val, writes, eng="dve"):
        P.op(eng, lambda e: e.memset(ap, val), [], writes)

    def dbg(name, ap, shape, reads):
        if DBG is None or name not in DBG:
            return
        o = dout("dbg_" + name, shape)
        dbg_outs[name] = shape
        DMA(o, ap, reads, [], key="dbg_" + name)

    ring_i = [0]
    ring_views = {}
    extra_slots = []

    def ring(shape, dtype=BF16):
        n = 3 + len(extra_slots)
        s = ring_i[0] % n
        ring_i[0] += 1
        if s < 3:
            off, bname, perm = R_RING.start + s * 8192, "ring%d" % s, True
        else:
            off, bname = extra_slots[s - 3]
            perm = False
        key = (off, tuple(shape), dtype)
        t = ring_views.get(key)
        if t is None:
            assert nbytes(shape, dtype) <= 8192, shape
            t = ring_views[key] = sbt(shape, dtype, off)
        return t, bname, perm

    def wload(src, shape, dtype=BF16, q="poolq", reads=()):
        t, b, perm = ring(shape, dtype)
        DMA(t[:], src, list(reads), [b], q=q, exempt=perm)
        return t, b

    CS = {}
    for k, v in consts.items():
        if k.startswith("E_"):
            CS[k] = alloc(R_CONST, [v.shape[0], 128], F32)
        else:
            CS[k] = alloc(R_CONST, list(v.shape), F32)
        DMA(CS[k][:], CD[k], [], ["c_" + k])
    ident_f = CS["ident"]
    ident_b = alloc(R_CONST, [128, 128], BF16)
    ones_f = alloc(R_CONST, [128, 128], F32)
    ones_b = alloc(R_CONST, [128, 128], BF16)
    P.op("dve", lambda e: e.tensor_copy(out=ident_b[:], in_=ident_f[:]), ["c_ident"], ["ident_b"])
    nident_f = alloc(R_CONST, [128, 128], F32)
    TS(nident_f[:], ident_f[:], -1.0, None, ALU.mult, None, ["c_ident"], ["nident"])
    MEMSET(ones_f[:], 1.0, ["ones_f"])
    MEMSET(ones_b[:], 1.0, ["ones_b"])
    hgm_b = {}
    for m in "ps":
        hgm_b[m] = alloc(R_CONST, [128, 128], BF16)
        P.op("dve", lambda e, m=m: e.tensor_copy(out=hgm_b[m][:], in_=CS["hgm_" + m][:]), ["c_hgm_" + m], ["hgmb_" + m])
    gains = alloc(R_CONST, [128, 2, 6, 16])
    for l in range(2):
        for k in range(6):
            DMA(gains[:, l, k, :], W["norm_gains"][l, k].rearrange("(c p) -> p c", p=128), [], ["gains"], slow=True)
    gainsh = alloc(R_CONST, [128, 2, 6, 16])
    TS(gainsh[:], gains[:], 0.5, None, ALU.mult, None, ["gains"], ["gainsh"])
    lbraw = alloc(R_CONST, [128, 2, 8])
    DMA(lbraw[:], W["hgrn_lower_bounds"].rearrange("l (c p) -> p l c", p=128), [], ["lbraw"], slow=True)
    hnorm = alloc(R_CONST, [128, 2, 8])
    DMA(hnorm[:], W["hgrn_norm"].rearrange("l (c p) -> p l c", p=128), [], ["hnorm"], slow=True)
    mnorm = alloc(R_CONST, [128, 2, 8])
    DMA(mnorm[:], W["mlstm_norm"].rearrange("l (c p) -> p l c", p=128), [], ["mnorm"], slow=True)
    convw = alloc(R_CONST, [128, 2, 4, 8])
    DMA(convw[:], W["mlstm_conv_w"].rearrange("l j (c p) -> p l j c", p=128), [], ["convw"], slow=True)
    convb = alloc(R_CONST, [128, 2, 8])
    DMA(convb[:], W["mlstm_conv_b"].rearrange("l (c p) -> p l c", p=128), [], ["convb"], slow=True)
    s5d = alloc(R_CONST, [128, 2, 8])
    DMA(s5d[:], W["s5_d"].rearrange("l (c g) k -> (g k) l c", g=8), [], ["s5d"], slow=True)
    bgate = alloc(R_CONST, [128, 2, 8])
    for l in range(2):
        DMA(bgate[:, l, :], W["mlstm_b_gates"][l].partition_broadcast(128), [], ["bgate"], key="bgate%d" % l)
    wgate = alloc(R_CONST, [128, 2, 24, 8], BF16)
    DMA(wgate[:], W["mlstm_w_gates"].rearrange("l (c p) g -> p l c g", p=128), [], ["wgate"], q="poolq")
    lb = alloc(R_CONST, [128, 2, 8])
    oml = alloc(R_CONST, [128, 2, 8])
    lbe = alloc(R_CONST, [128, 2, 8])
    lbs = alloc(R_CONST, [128, 8])
    ACT(lbe[:], lbraw[:], AF.Exp, ["lbraw"], ["lbe"])
    TT(lbs[:], lbe[:, 0, :], lbe[:, 1, :], ALU.add, ["lbe"], ["lbs"])
    P.op("dve", lambda e: e.reciprocal(out=lbs[:], in_=lbs[:]), ["lbs"], ["lbs"])
    MEMSET(lb[:, 0, :], 0.0, ["lb0"])
    TT(lb[:, 1, :], lbe[:, 1, :], lbs[:], ALU.mult, ["lbe", "lbs"], ["lb1"])
    TS(oml[:], lb[:], -1.0, 1.0, ALU.mult, ALU.add, ["lb0", "lb1"], ["oml"])
    LBR = ["lb0", "lb1", "oml"]
    s5mag = alloc(R_CONST, [128, 2, 32])

    def s5_setup(l):
        R = Region(R_MODE.start, R_MODE.size)
        are = alloc(R, [128, 32]); aim = alloc(R, [128, 32]); ldt = alloc(R, [128, 32])
        DMA(are[:], W["s5_a_re"][l].rearrange("(r g) p -> (g p) r", g=2), [], ["s5.are"], slow=True)
        DMA(aim[:], W["s5_a_im"][l].rearrange("(r g) p -> (g p) r", g=2), [], ["s5.aim"], slow=True)
        for g2 in range(2):
            DMA(ldt[g2 * 64:(g2 + 1) * 64, :], W["s5_log_dt"][l].rearrange("(r g) -> g r", g=2)[g2].partition_broadcast(64),
                [], ["s5.ldt"], slow=True, key="s5ldt%d" % g2)
        dt = alloc(R, [128, 32]); lre = alloc(R, [128, 32]); th = alloc(R, [128, 32])
        t1 = alloc(R, [128, 32]); t2 = alloc(R, [128, 32]); cs = alloc(R, [128, 32]); sn = alloc(R, [128, 32])
        abr = alloc(R, [128, 32]); abi = alloc(R, [128, 32]); inv = alloc(R, [128, 32])
        fre = alloc(R, [128, 32]); fim = alloc(R, [128, 32]); t3 = alloc(R, [128, 32]); t4 = alloc(R, [128, 32])
        mag = s5mag[:, l, :]
        ACT(dt[:], ldt[:], AF.Exp, ["s5.ldt"], ["s5.dt"])
        TS(lre[:], are[:], -1e-4, None, ALU.min, None, ["s5.are"], ["s5.lre"])
        TT(t1[:], lre[:], dt[:], ALU.mult, ["s5.lre", "s5.dt"], ["s5.t1"])
        ACT(mag, t1[:], AF.Exp, ["s5.t1"], ["s5mag%d" % l])
        TT(th[:], aim[:], dt[:], ALU.mult, ["s5.aim", "s5.dt"], ["s5.th"])
        ki = sbt([128, 32], mybir.dt.int32, R.take(128))
        TS(t1[:], th[:], 1.0 / (2 * math.pi), None, ALU.mult, None, ["s5.th"], ["s5.t1"])
        P.op("dve", lambda e: e.tensor_copy(out=ki[:], in_=t1[:]), ["s5.t1"], ["s5.ki"])
        P.op("dve", lambda e: e.tensor_copy(out=t2[:], in_=ki[:]), ["s5.ki"], ["s5.t2"])
        STT(t1[:], t2[:], -2 * math.pi, th[:], ALU.mult, ALU.add, ["s5.t2", "s5.th"], ["s5.t1"])
        ACT(sn[:], t1[:], AF.Sin, ["s5.t1"], ["s5.sn"], scale=0.25)
        TS(t2[:], t1[:], 0.25, math.pi / 2, ALU.mult, ALU.add, ["s5.t1"], ["s5.t2"])
        ACT(cs[:], t2[:], AF.Sin, ["s5.t2"], ["s5.cs"])
        for _ in range(2):
            TT(t1[:], sn[:], cs[:], ALU.mult, ["s5.sn", "s5.cs"], ["s5.t1"])
            TT(t2[:], sn[:], sn[:], ALU.mult, ["s5.sn"], ["s5.t2"])
            TS(sn[:], t1[:], 2.0, None, ALU.mult, None, ["s5.t1"], ["s5.sn"])
            TS(cs[:], t2[:], -2.0, 1.0, ALU.mult, ALU.add, ["s5.t2"], ["s5.cs"])
        TT(abr[:], mag, cs[:], ALU.mult, ["s5mag%d" % l, "s5.cs"], ["s5.abr"])
        TT(abi[:], mag, sn[:], ALU.mult, ["s5mag%d" % l, "s5.sn"], ["s5.abi"])
        TT(t1[:], lre[:], lre[:], ALU.mult, ["s5.lre"], ["s5.t1"])
        TT(t2[:], aim[:], aim[:], ALU.mult, ["s5.aim"], ["s5.t2"])
        TT(inv[:], t1[:], t2[:], ALU.add, ["s5.t1", "s5.t2"], ["s5.inv"])
        P.op("dve", lambda e: e.reciprocal(out=inv[:], in_=inv[:]), ["s5.inv"], ["s5.inv"])
        TS(t3[:], abr[:], -1.0, None, ALU.add, None, ["s5.abr"], ["s5.t3"])
        TT(t1[:], t3[:], lre[:], ALU.mult, ["s5.t3", "s5.lre"], ["s5.t1"])
        TT(t2[:], abi[:], aim[:], ALU.mult, ["s5.abi", "s5.aim"], ["s5.t2"])
        TT(t1[:], t1[:], t2[:], ALU.add, ["s5.t1", "s5.t2"], ["s5.t1"])
        TT(fre[:], t1[:], inv[:], ALU.mult, ["s5.t1", "s5.inv"], ["s5.fre"])
        TT(t4[:], abi[:], lre[:], ALU.mult, ["s5.abi", "s5.lre"], ["s5.t4"])
        TT(t2[:], t3[:], aim[:], ALU.mult, ["s5.t3", "s5.aim"], ["s5.t2"])
        TT(t4[:], t4[:], t2[:], ALU.subtract, ["s5.t4", "s5.t2"], ["s5.t4"])
        TT(fim[:], t4[:], inv[:], ALU.mult, ["s5.t4", "s5.inv"], ["s5.fim"])
        bre = alloc(R, [128, 32, 16]); bim = alloc(R, [128, 32, 16])
        DMA(bre[:], W["s5_b_re"][l].rearrange("(r g) p c -> (g p) r c", g=2), [], ["s5.bre"])
        DMA(bim[:], W["s5_b_im"][l].rearrange("(r g) p c -> (g p) r c", g=2), [], ["s5.bim"])
        bbr = alloc(R, [128, 32, 16]); bbi = alloc(R, [128, 32, 16]); tb = alloc(R, [128, 32, 16])
        fre_b = fre[:].unsqueeze(2).broadcast_to([128, 32, 16])
        fim_b = fim[:].unsqueeze(2).broadcast_to([128, 32, 16])
        TT(bbr[:], bre[:], fre_b, ALU.mult, ["s5.bre", "s5.fre"], ["s5.bbr"])
        TT(tb[:], bim[:], fim_b, ALU.mult, ["s5.bim", "s5.fim"], ["s5.tb"])
        TT(bbr[:], bbr[:], tb[:], ALU.subtract, ["s5.bbr", "s5.tb"], ["s5.bbr"])
        TT(bbi[:], bim[:], fre_b, ALU.mult, ["s5.bim", "s5.fre"], ["s5.bbi"])
        TT(tb[:], bre[:], fim_b, ALU.mult, ["s5.bre", "s5.fim"], ["s5.tb"])
        TT(bbi[:], bbi[:], tb[:], ALU.add, ["s5.bbi", "s5.tb"], ["s5.bbi"])
        cm = CS["s5cm"]
        Z = [alloc(R, [128, 8, 16]) for _ in range(2)]
        wst = [alloc(R, [128, 2, 512]) for _ in range(2)]
        cn = alloc(R, [128, 2, 64]); ct = alloc(R, [128, 128])
        zi = 0
        for kc in range(8):
            ws = wst[kc % 2]
            wsn = "s5.wst%d" % (kc % 2)
            for part, src in ((0, bbr), (1, bbi)):
                bk = (2 * kc + part) % 8
                for j in range(4):
                    r = 4 * kc + j
                    z = Z[zi % 2]; zn = "s5.Z%d" % (zi % 2); zi += 1
                    TT(z[:], cm[:, j, :].rearrange("p (g c) -> p g c", c=16),
                       src[:, r, :].unsqueeze(1).broadcast_to([128, 8, 16]), ALU.mult,
                       ["c_s5cm", "s5.bbr", "s5.bbi"], [zn])
                    TR(banks[bk][:, j * 128:(j + 1) * 128], z[:].rearrange("p g c -> p (g c)"), ident_f[:],
                       [zn, "c_ident"], [PB[bk]])
                COPY(ws[:, part, :], banks[bk][:], [PB[bk]], [wsn])
            DMA(s5wb[l, kc], ws[:], [wsn], ["d_s5wb%d" % l], key="s5wbst")
        wcs = [alloc(R, [128, 2, 512]) for _ in range(2)]
        for kc in range(8):
            ws = wcs[kc % 2]
            wsn = "s5.wcs%d" % (kc % 2)
            for part, nm in ((0, "s5_c_re"), (1, "s5_c_im")):
                bk = (2 * kc + part) % 8
                src = W[nm][l, 8 * kc:8 * kc + 8].rearrange("g c p -> (g c) p")
                for dup in range(2):
                    DMA(cn[:, dup, :], src, [], ["s5.cn"], key="s5cn%d" % dup)
                TR(banks[bk][:, 0:128], cn[:].rearrange("p a b -> p (a b)"), ident_f[:], ["s5.cn", "c_ident"], [PB[bk]])
                COPY(ct[:], banks[bk][:, 0:128], [PB[bk]], ["s5.ct"])
                for j in range(4):
                    if part == 0:
                        TT(ws[:, part, j * 128:(j + 1) * 128], ct[:], cm[:, j, :], ALU.mult, ["s5.ct", "c_s5cm"], [wsn])
                    else:
                        STT(ws[:, part, j * 128:(j + 1) * 128], ct[:], -1.0, cm[:, j, :], ALU.mult, ALU.mult,
                            ["s5.ct", "c_s5cm"], [wsn])
            DMA(s5wc[l, kc], ws[:], [wsn], ["d_s5wc%d" % l], key="s5wcst")
        tabs = [alloc(R, [128, 2, 4, 512]) for _ in range(2)]
        tq = [alloc(R, [128, 4, 256]) for _ in range(2)]
        for kc in range(8):
            tab = tabs[kc % 2]
            tn = "s5.tab%d" % (kc % 2)
            rs = slice(4 * kc, 4 * kc + 4)
            P.op("dve", lambda e, tab=tab, rs=rs: e.tensor_copy(out=tab[:, 0, :, 0:1], in_=cs[:, rs].unsqueeze(2)), ["s5.cs"], [tn])
            P.op("dve", lambda e, tab=tab, rs=rs: e.tensor_copy(out=tab[:, 1, :, 0:1], in_=sn[:, rs].unsqueeze(2)), ["s5.sn"], [tn])
            m = 1
            while m < 512:
                cmv = tab[:, 0, :, m - 1:m].broadcast_to([128, 4, m])
                smv = tab[:, 1, :, m - 1:m].broadcast_to([128, 4, m])
                c0 = tab[:, 0, :, 0:m]; s0 = tab[:, 1, :, 0:m]
                q0 = tq[0][:, :, 0:m]; q1 = tq[1][:, :, 0:m]
                TT(q0, c0, cmv, ALU.mult, [tn], ["s5.tq0"])
                TT(q1, s0, smv, ALU.mult, [tn], ["s5.tq1"])
                TT(tab[:, 0, :, m:2 * m], q0, q1, ALU.subtract, ["s5.tq0", "s5.tq1", tn], [tn])
                TT(q0, s0, cmv, ALU.mult, [tn], ["s5.tq0"])
                TT(q1, c0, smv, ALU.mult, [tn], ["s5.tq1"])
                TT(tab[:, 1, :, m:2 * m], q0, q1, ALU.add, ["s5.tq0", "s5.tq1", tn], [tn])
                m *= 2
            DMA(s5tab[l, kc], tab[:], [tn], ["d_s5tab%d" % l], key="s5tabst")

    for l in range(NL):
        s5_setup(l)
    P.barrier()

    class Mode:
        pass

    def make_mode(m):
        M = Mode()
        M.m = m
        M.T = 512 if m == "p" else 128
        M.NB = M.T // 128
        R = Region(R_MODE.start, R_MODE.size)
        M.R = R
        T = M.T
        if m == "p":
            M.HGS = alloc(R, [128, 2, 8, 128])
            M.MLC = alloc(R, [128, 2, 4, 2, 256])
            M.MLN = alloc(R, [128, 2, 4, 2])
            M.MST = alloc(R, [1, 2, 4])
            M.XSr = alloc(R, [128, 2, 32])
            M.XSi = alloc(R, [128, 2, 32])
            M.CV = alloc(R, [128, 2, 3, 8])
        M.RSTD = alloc(R, [128, T])
        M.SQ = [alloc(R, [128, T], BF16) for _ in range(2)]
        M.PNT = alloc(R, [128, T])
        M.X = alloc(R, [128, 16, T])
        M.H = alloc(R, [128, 16, T], BF16)
        M.FF = alloc(R, [128, 16, T])
        M.S = Region(R.cur, R.start + R.size - R.cur)
        return M

    def xn(c):
        return "X.%d" % c

    def hn(c):
        return "H.%d" % c

    def fn_(c):
        return "FF.%d" % c

    XALL = [xn(c) for c in range(16)]
    HALL = [hn(c) for c in range(16)]
    FALL = [fn_(c) for c in range(16)]

    def rms_stats(M, src, srcnames, bank=0):
        T = M.T
        for c in range(16):
            s = M.SQ[c % 2]; sn_ = "sq%d" % (c % 2)
            ACT(s[:], src(c), AF.Square, [srcnames[c]], [sn_])
            MM(banks[bank][:, 0:T], [(ones_b[:], s[:])], [sn_, "ones_b"], [PB[bank]], start=(c == 0), stop=(c == 15))
        ACT(M.RSTD[:], banks[bank][:, 0:T], AF.Ln, [PB[bank]], ["rstd"], scale=1.0 / D, bias=EPS)
        ACT(M.RSTD[:], M.RSTD[:], AF.Exp, ["rstd"], ["rstd"], scale=-0.5)

    def prenorm(M, l, k, barrier=False):
        rms_stats(M, lambda c: M.X[:, c, :], XALL)
        for c in range(16):
            STT(M.H[:, c, :], M.X[:, c, :], gains[:, l, k, c:c + 1], M.RSTD[:], ALU.mult, ALU.mult,
                [xn(c), "gains", "rstd"], [hn(c)])
        if barrier:
            P.barrier()

    def postnorm_residual(M, l, k, half):
        rms_stats(M, lambda c: M.FF[:, c, :], FALL)
        g = gainsh if half else gains
        gname = "gainsh" if half else "gains"
        for c in range(16):
            STT(M.PNT[:], M.FF[:, c, :], g[:, l, k, c:c + 1], M.RSTD[:], ALU.mult, ALU.mult, [fn_(c), gname, "rstd"], ["pn.t"])
            TT(M.X[:, c, :], M.X[:, c, :], M.PNT[:], ALU.add, [xn(c), "pn.t"], [xn(c)])

    def ffn(M, l, wup, wdown):
        T = M.T
        S = M.S; S.reset()
        HID = alloc(S, [128, 22, T], BF16)
        sa = [alloc(S, [128, T], BF16) for _ in range(2)]
        si = 0
        for half in range(2):
            for jj in range(11):
                j = half * 11 + jj
                ta, ba = wload(wup[l, :, 256 * j:256 * j + 256].rearrange("(kc p) n -> p kc n", p=128), [128, 16, 256])
                tb, bb = wload(wup[l, :, FFD + 256 * j:FFD + 256 * j + 256].rearrange("(kc p) n -> p kc n", p=128), [128, 16, 256])
                bs = 4 * (j % 2)
                for ch in range(2):
                    MM(banks[bs + ch][:, 0:T], [(ta[:, kc, ch * 128:(ch + 1) * 128], M.H[:, kc, :]) for kc in range(16)],
                       [ba] + HALL, [PB[bs + ch]])
                for ch in range(2):
                    MM(banks[bs + 2 + ch][:, 0:T], [(tb[:, kc, ch * 128:(ch + 1) * 128], M.H[:, kc, :]) for kc in range(16)],
                       [bb] + HALL, [PB[bs + 2 + ch]])
                for ch in range(2):
                    fl = 2 * jj + ch
                    s = sa[si % 2]; sn_ = "ffn.sa%d" % (si % 2); si += 1
                    ACT(s[:], banks[bs + ch][:, 0:T], AF.Silu, [PB[bs + ch]], [sn_])
                    TT(HID[:, fl, :], s[:], banks[bs + 2 + ch][:, 0:T], ALU.mult, [sn_, PB[bs + 2 + ch]], ["HID.%d" % fl])
            for dg in range(4):
                bs = 4 * (dg % 2)
                first = True
                for q in range(6):
                    nf = min(4, 22 - 4 * q)
                    f0 = half * 22 + 4 * q
                    tw, bw = wload(wdown[l, f0 * 128:(f0 + nf) * 128, dg * 512:(dg + 1) * 512].rearrange("(fc p) n -> p fc n", p=128),
                                   [128, nf, 512])
                    for dch in range(4):
                        pairs = [(tw[:, fc, dch * 128:(dch + 1) * 128], HID[:, 4 * q + fc, :]) for fc in range(nf)]
                        MM(banks[bs + dch][:, 0:T], pairs, [bw] + ["HID.%d" % (4 * q + fc) for fc in range(nf)], [PB[bs + dch]],
                           start=first, stop=(q == 5))
                    first = False
                for dch in range(4):
                    c = dg * 4 + dch
                    if half == 0:
                        COPY(M.FF[:, c, :], banks[bs + dch][:, 0:T], [PB[bs + dch]], [fn_(c)])
                    else:
                        TT(M.FF[:, c, :], M.FF[:, c, :], banks[bs + dch][:, 0:T], ALU.add, [fn_(c), PB[bs + dch]], [fn_(c)])
        P.barrier()

    def proj_fm(M, l, col0, ntile, consume, tok=None, bank0=0, nbank=8):
        t0, t1 = (0, M.T) if tok is None else tok
        n = t1 - t0
        bi = 0
        for tl in range(ntile):
            tw, bw = wload(W["w_in"][l, :, col0 + 256 * tl:col0 + 256 * tl + 256].rearrange("(kc p) n -> p kc n", p=128),
                           [128, 16, 256])
            for ch in range(2):
                bk = bank0 + (bi % nbank); bi += 1
                MM(banks[bk][:, 0:n], [(tw[:, kc, ch * 128:(ch + 1) * 128], M.H[:, kc, t0:t1]) for kc in range(16)],
                   [bw] + HALL, [PB[bk]])
                consume(2 * tl + ch, banks[bk][:, 0:n], PB[bk])

    def proj_tm(M, l, col0, ntile, blks, consume, bank0=0, nbank=8):
        bi = 0
        for tl in range(ntile):
            tw, bw = wload(W["w_in"][l, :, col0 + 256 * tl:col0 + 256 * tl + 256].rearrange("(kc p) n -> p kc n", p=128),
                           [128, 16, 256])
            for blk in blks:
                bk = bank0 + (bi % nbank); bi += 1
                MM(banks[bk][:, 0:256], [(M.H[:, kc, blk * 128:(blk + 1) * 128], tw[:, kc, :]) for kc in range(16)],
                   [bw] + HALL, [PB[bk]])
                consume(tl, blk, banks[bk][:, 0:256], PB[bk])

    def hgrn(M, l, OG, st):
        T = M.T; NB = M.NB; m = M.m
        clen = 32 if m == "p" else 8
        nslot_blk = 128 // clen
        nch = T // clen
        S = M.S
        S.reset()
        mk = S.mark()
        cst = CS["cst_" + m]; crow = CS["crow_" + m]
        for hgp in range(2):
            S.reset(mk)
            QT = [alloc(S, [128, T]) for _ in range(4)]
            KT = [alloc(S, [128, T]) for _ in range(4)]
            KH = [alloc(S, [128, NB, 128], BF16) for _ in range(4)]
            SHG = [alloc(S, [128, T], BF16) for _ in range(4)]
            EBE = [alloc(S, [128, nch]) for _ in range(4)]
            VT = alloc(S, [128, NB, 512], BF16)
            VM = [alloc(S, [128, 512], BF16) for _ in range(2)]
            TMP = [[alloc(S, [128, T]) for _ in range(4)] for _ in range(2)]
            AT = [[alloc(S, [128, 128], BF16) for _ in range(2)] for _ in range(2)]
            BB = [[alloc(S, [128, T], BF16) for _ in range(2)] for _ in range(2)]

            def cons_v(tl, blk, bank, bname):
                COPY(VT[:, blk, tl * 256:(tl + 1) * 256], bank, [bname], ["hg.VT%d.%d" % (blk, tl)])
            proj_tm(M, l, 2048 + hgp * 512, 2, range(NB), cons_v, bank0=0, nbank=4)
            VTN = lambda blk: ["hg.VT%d.0" % blk, "hg.VT%d.1" % blk]

            def fchain(hh, hd, bank, bname, ci):
                TA, TB, TC, TD = TMP[ci]
                nA, nB, nC, nD = ["hg.T%s%d" % (x, ci) for x in "ABCD"]
                ACT(TA[:], bank, AF.Sigmoid, [bname], [nA]); yield
                TS(TA[:], TA[:], oml[:, l, hd:hd + 1], lb[:, l, hd:hd + 1], ALU.mult, ALU.add, [nA] + LBR, [nA]); yield
                TS(TB[:], TA[:], -1.0, 1.0, ALU.mult, ALU.add, [nA], [nB]); yield
                ACT(TA[:], TA[:], AF.Ln, [nA], [nA]); yield
                P.op("dve", lambda e: e.tensor_tensor_scan(out=TC[:], data0=cst[:, 0:T], data1=TA[:], initial=0.0,
                                                            op0=ALU.mult, op1=ALU.add), [nA, "c_cst_" + m], [nC]); yield
                ACT(TD[:], TC[:], AF.Exp, [nC], [nD]); yield
                ACT(TA[:], TC[:], AF.Exp, [nC], [nA], scale=-1.0); yield
                TT(KT[hh][:], TB[:], TA[:], ALU.mult, [nB, nA], ["hg.KT%d" % hh]); yield
                ebv = TD[:].rearrange("p (c j) -> p c j", j=clen)[:, :, clen - 1:clen]
                P.op("dve", lambda e: e.tensor_copy(out=EBE[hh][:].unsqueeze(2), in_=ebv), [nD], ["hg.EBE%d" % hh]); yield
                TT(TB[:].rearrange("p (c j) -> p c j", j=clen), KT[hh][:].rearrange("p (c j) -> p c j", j=clen),
                   ebv.broadcast_to([128, nch, clen]), ALU.mult, ["hg.KT%d" % hh, nD], [nB]); yield
                bk = 4 + ci
                for blk in range(NB):
                    TR(banks[bk][:, blk * 128:(blk + 1) * 128], TB[:, blk * 128:(blk + 1) * 128], ident_f[:],
                       [nB, "c_ident"], [PB[bk]])
                yield
                COPY(KH[hh][:].rearrange("p b k -> p (b k)"), banks[bk][:, 0:T], [PB[bk]], ["hg.KH%d" % hh]); yield
                COPY(QT[hh][:], TD[:], [nD], ["hg.QT%d" % hh], eng="dve"); yield

            for pr in range(2):
                for grp, cbase in (("f", 1024), ("q", 0), ("g", 3072)):
                    pend = []

                    def cons(ci, bank, bname, grp=grp, pr=pr, pend=pend):
                        hh = 2 * pr + ci
                        hd = 4 * hgp + hh
                        if grp == "f":
                            pend.append(fchain(hh, hd, bank, bname, ci))
                        elif grp == "q":
                            TC = TMP[ci][2]; nC = "hg.TC%d" % ci
                            ACT(TC[:], bank, AF.Silu, [bname], [nC])
                            TT(QT[hh][:], QT[hh][:], TC[:], ALU.mult, ["hg.QT%d" % hh, nC], ["hg.QT%d" % hh])
                        else:
                            ACT(SHG[hh][:], bank, AF.Silu, [bname], ["hg.SHG%d" % hh])
                    proj_fm(M, l, cbase + hgp * 512 + pr * 256, 1, cons, bank0=0, nbank=4)
                    interleave(pend)
            OB = [0, 1, 2, 3]

            def intra(hh, ci):
                KTb, QTb = BB[ci]; nk = "hg.KTb%d" % ci; nq = "hg.QTb%d" % ci
                COPY(KTb[:], KT[hh][:], ["hg.KT%d" % hh], [nk], eng="dve"); yield
                COPY(QTb[:], QT[hh][:], ["hg.QT%d" % hh], [nq], eng="dve"); yield
                sb_ = 6 + ci
                for blk in range(NB):
                    sl = slice(blk * 128, (blk + 1) * 128)
                    MM(banks[sb_][:, 0:128], [(KTb[:, sl], QTb[:, sl])], [nk, nq], [PB[sb_]])
                    at = AT[ci][blk % 2]; an = "hg.AT%d%d" % (ci, blk % 2)
                    TT(at[:], banks[sb_][:, 0:128], hgm_b[m][:], ALU.mult, [PB[sb_], "hgmb_" + m], [an])
                    MM(banks[OB[hh]][:, sl], [(VT[:, blk, hh * 128:(hh + 1) * 128], at[:])], [an] + VTN(blk), [PB[OB[hh]]],
                       start=(blk == 0), stop=False, skip=True)
                    yield
            for pr in range(2):
                interleave([intra(2 * pr, 0), intra(2 * pr + 1, 1)])
            vi = 0
            for blk in range(NB):
                for ci in range(nslot_blk):
                    slot = blk * nslot_blk + ci
                    t0 = blk * 128 + ci * clen
                    vm = VM[vi % 2]; vn = "hg.VM%d" % (vi % 2); vi += 1
                    TS(vm[:], VT[:, blk, :], crow[:, ci:ci + 1], None, ALU.mult, None, VTN(blk) + ["c_crow_" + m], [vn])
                    sin, sin_names, sout, sout_names, after = st(hgp, slot)
                    ub = 4 + (slot % 2)
                    for hh in range(4):
                        MM(banks[OB[hh]][:, t0:t0 + clen], [(sin[hh], QT[hh][:, t0:t0 + clen])],
                           [sin_names[hh], "hg.QT%d" % hh], [PB[OB[hh]]], start=False, stop=False, skip=True)
                        MM(banks[ub][:, hh * 128:(hh + 1) * 128], [(KH[hh][:, blk, :], vm[:, hh * 128:(hh + 1) * 128])],
                           ["hg.KH%d" % hh, vn], [PB[ub]])
                        STT(sout[hh], sin[hh], EBE[hh][:, slot:slot + 1], banks[ub][:, hh * 128:(hh + 1) * 128], ALU.mult, ALU.add,
                            [sin_names[hh], "hg.EBE%d" % hh, PB[ub]], [sout_names[hh]])
                    if after is not None:
                        after()

            def post(hh, ci):
                hd = 4 * hgp + hh
                TA, TB, TC, TD = TMP[ci]
                nA, nB, nC, nD = ["hg.T%s%d" % (x, ci) for x in "ABCD"]
                SQ = BB[ci][0]; nsq_ = "hg.KTb%d" % ci
                bk = 6 + ci
                COPY(TA[:], banks[OB[hh]][:, 0:T], [PB[OB[hh]]], [nA]); yield
                ACT(SQ[:], TA[:], AF.Square, [nA], [nsq_]); yield
                MM(banks[bk][:, 0:T], [(ones_b[:], SQ[:])], [nsq_, "ones_b"], [PB[bk]]); yield
                ACT(TC[:], banks[bk][:, 0:T], AF.Ln, [PB[bk]], [nC], scale=1.0 / 128, bias=EPS); yield
                ACT(TC[:], TC[:], AF.Exp, [nC], [nC], scale=-0.5); yield
                STT(TA[:], TA[:], hnorm[:, l, hd:hd + 1], TC[:], ALU.mult, ALU.mult, [nA, "hnorm", nC], [nA]); yield
                TT(OG[:, hd, :], TA[:], SHG[hh][:], ALU.mult, [nA, "hg.SHG%d" % hh], ["OG.%d" % hd]); yield
            for pr in range(2):
                interleave([post(2 * pr, 0), post(2 * pr + 1, 1)])
        P.barrier()
        S.reset(mk)

    def s5(M, l, YB, XSr, XSi, XNr, XNi, xs_name, xn_name):
        T = M.T; m = M.m
        S = M.S
        S.reset()
        mk = S.mark()
        Tt = 512 if m == "p" else 8
        TAB = alloc(S, [128, 2, 4, Tt])
        U32 = [alloc(S, [128, T]) for _ in range(2)]
        BU = [[alloc(S, [128, T]) for _ in range(2)] for _ in range(2)]
        BT = [[alloc(S, [128, T]) for _ in range(2)] for _ in range(2)]
        QQ = [[alloc(S, [128, T]) for _ in range(2)] for _ in range(2)]
        D0 = [alloc(S, [128, T]) for _ in range(2)]
        YT = alloc(S, [128, T]); Y2 = alloc(S, [128, T])
        sst = CS["s5st_" + m]
        nsq = T // Tt if m == "s" else 1

        def tv(ap):
            if m == "p":
                return ap
            return ap.rearrange("p (b j) -> p b j", j=8)

        def tabv(part, j):
            if m == "p":
                return TAB[:, part, j, :]
            return TAB[:, part, j, :].unsqueeze(1).broadcast_to([128, 16, 8])

        def first(ap):
            if m == "p":
                return ap[:, 0:1]
            return ap.rearrange("p (b j) -> p b j", j=8)[:, :, 0]

        def lastc(ap):
            if m == "p":
                return ap[:, T - 1:T]
            return ap.rearrange("p (b j) -> p b j", j=8)[:, :, 7]

        for kc in range(8):
            if kc % 2 == 0:
                def cons_u(ci, bank, bname, kc=kc):
                    COPY(U32[ci][:], bank, [bname], ["s5.U%d" % ci])
                proj_fm(M, l, 4096 + 256 * (kc // 2), 1, cons_u, bank0=4, nbank=2)
            u = U32[kc % 2]; un = "s5.U%d" % (kc % 2)
            wb, wbn = wload(s5wb[l, kc], [128, 2, 512], F32, q="sp", reads=["d_s5wb%d" % l])
            wc, wcn = wload(s5wc[l, kc], [128, 2, 512], F32, q="sp", reads=["d_s5wc%d" % l])
            if m == "p":
                DMA(TAB[:], s5tab[l, kc], ["d_s5tab%d" % l], ["s5.TAB"])
            else:
                DMA(TAB[:], s5tab[l, kc][:, :, :, 0:8], ["d_s5tab%d" % l], ["s5.TAB"])
            yb = 6 + (kc % 2)
            def row(j, kc=kc, u=u, un=un, wb=wb, wbn=wbn, wc=wc, wcn=wcn, yb=yb):
                r = 4 * kc + j
                pj = j % 2
                bur, bui = BU[pj]; btr, bti = BT[pj]
                q0, q1 = QQ[pj]; nq0 = "s5.Q0%d" % pj; nq1 = "s5.Q1%d" % pj
                nbu = ["s5.BU%d%d" % (pj, i) for i in range(2)]
                nbt = ["s5.BT%d%d" % (pj, i) for i in range(2)]
                d0 = D0[pj]; nd0 = "s5.D0%d" % pj
                bkr = 2 * pj; bki = 2 * pj + 1
                pr_ = banks[bkr][:, 0:T]; pi_ = banks[bki][:, 0:T]
                for part in range(2):
                    bk = 2 * pj + part
                    MM(banks[bk][:, 0:T], [(wb[:, part, j * 128:(j + 1) * 128], u[:])], [wbn, un], [PB[bk]])
                    COPY(BU[pj][part][:], banks[bk][:, 0:T], [PB[bk]], [nbu[part]])
                    yield
                ct = tabv(0, j); sn_ = tabv(1, j)
                TT(tv(q0[:]), tv(bur[:]), ct, ALU.mult, [nbu[0], "s5.TAB"], [nq0]); yield
                TT(tv(q1[:]), tv(bui[:]), sn_, ALU.mult, [nbu[1], "s5.TAB"], [nq1]); yield
                TT(btr[:], q0[:], q1[:], ALU.add, [nq0, nq1], [nbt[0]]); yield
                TT(tv(q0[:]), tv(bui[:]), ct, ALU.mult, [nbu[1], "s5.TAB"], [nq0]); yield
                TT(tv(q1[:]), tv(bur[:]), sn_, ALU.mult, [nbu[0], "s5.TAB"], [nq1]); yield
                TT(bti[:], q0[:], q1[:], ALU.subtract, [nq0, nq1], [nbt[1]]); yield
                magc = s5mag[:, l, r:r + 1]
                STT(first(btr[:]), XSr(r), magc, first(btr[:]), ALU.mult, ALU.add, [xs_name, "s5mag%d" % l, nbt[0]], [nbt[0]]); yield
                STT(first(bti[:]), XSi(r), magc, first(bti[:]), ALU.mult, ALU.add, [xs_name, "s5mag%d" % l, nbt[1]], [nbt[1]]); yield
                ACT(d0[:], sst[:, 0:T], AF.Copy, ["c_s5st_" + m, "s5mag%d" % l], [nd0], scale=magc); yield
                for part in range(2):
                    P.op("dve", lambda e, o=BU[pj][part], d1=BT[pj][part], d0=d0: e.tensor_tensor_scan(
                        out=o[:], data0=d0[:], data1=d1[:], initial=0.0, op0=ALU.mult, op1=ALU.add),
                        [nd0, nbt[part]], [nbu[part]])
                    yield
                TT(tv(q0[:]), tv(bur[:]), ct, ALU.mult, [nbu[0], "s5.TAB"], [nq0]); yield
                TT(tv(q1[:]), tv(bui[:]), sn_, ALU.mult, [nbu[1], "s5.TAB"], [nq1]); yield
                TT(btr[:], q0[:], q1[:], ALU.subtract, [nq0, nq1], [nbt[0]]); yield
                TT(tv(q0[:]), tv(bui[:]), ct, ALU.mult, [nbu[1], "s5.TAB"], [nq0]); yield
                TT(tv(q1[:]), tv(bur[:]), sn_, ALU.mult, [nbu[0], "s5.TAB"], [nq1]); yield
                TT(bti[:], q0[:], q1[:], ALU.add, [nq0, nq1], [nbt[1]]); yield
                COPY(XNr(r), lastc(btr[:]), [nbt[0]], [xn_name], eng="dve"); yield
                COPY(XNi(r), lastc(bti[:]), [nbt[1]], [xn_name], eng="dve"); yield
                MM(banks[yb][:, 0:T], [(wc[:, 0, j * 128:(j + 1) * 128], btr[:]), (wc[:, 1, j * 128:(j + 1) * 128], bti[:])],
                   [wcn, nbt[0], nbt[1]], [PB[yb]], start=(j == 0), stop=(j == 3))
                yield
            for jp in (0, 2):
                interleave([row(jp), row(jp + 1)])
            STT(YT[:], u[:], s5d[:, l, kc:kc + 1], banks[yb][:, 0:T], ALU.mult, ALU.add, [un, "s5d", PB[yb]], ["s5.YT"])
            ACT(Y2[:], YT[:], AF.Square, ["s5.YT"], ["s5.Y2"])
            TS(Y2[:], Y2[:], 0.044715, 1.0, ALU.mult, ALU.add, ["s5.Y2"], ["s5.Y2"])
            TT(Y2[:], Y2[:], YT[:], ALU.mult, ["s5.Y2", "s5.YT"], ["s5.Y2"])
            ACT(Y2[:], Y2[:], AF.Sigmoid, ["s5.Y2"], ["s5.Y2"], scale=1.5957691216057308)
            TT(YB[:, kc, :], YT[:], Y2[:], ALU.mult, ["s5.YT", "s5.Y2"], ["YB.%d" % kc])
        P.barrier()
        S.reset(mk)

    def mlstm(M, l, HM, SS):
        T = M.T; m = M.m
        TT_ = 256 if m == "p" else 128
        NBK = TT_ // 128
        nseq = 1 if m == "p" else 16
        Ls = TT_ if m == "p" else 8
        nslot = 1 if m == "p" else 16
        S = M.S
        S.reset()
        mk0 = S.mark()
        neg = CS["neg_" + m]; tri = CS["tri_" + m]; G = CS["G_" + m]; E = CS["E_" + m]
        last = CS["last_" + m]; smask = CS["smask_" + m]
        CN = ["c_neg_" + m, "c_tri_" + m, "c_G_" + m, "c_E_" + m, "c_last_" + m, "c_smask_" + m]
        for th in range(T // TT_):
            S.reset(mk0)
            tk0 = th * TT_
            QT = alloc(S, [128, 8, TT_], BF16); KT = alloc(S, [128, 8, TT_], BF16)
            KTM = alloc(S, [128, NBK, 1024], BF16); VTM = alloc(S, [128, NBK, 1024], BF16)
            SMO = alloc(S, [128, NBK, 1024], BF16)
            GT = alloc(S, [128, NBK, 8]); LF = alloc(S, [128, NBK, 4]); LS_ = alloc(S, [128, NBK, 4])
            mk1 = S.mark()
            XC = alloc(S, [128, 8, TT_], BF16); MXB = alloc(S, [128, 8, TT_], BF16); VTt = alloc(S, [128, 8, TT_], BF16)
            MXE = [alloc(S, [128, nseq, 3 + Ls]) for _ in range(2)]
            XCt = alloc(S, [128, nseq, Ls])
            def cons_mx(ci_, bank, bname):
                fc = cons_mx.fc0 + ci_
                e = MXE[fc % 2]; en = "ml.MXE%d" % (fc % 2)
                COPY(e[:, :, 3:3 + Ls], bank.rearrange("p (b j) -> p b j", j=Ls), [bname], [en])
                COPY(e[:, :, 0:3], SS["cv_in"](fc), [SS["cv_in_name"]], [en], eng="dve")
                COPY(MXB[:, fc, :], bank, [bname], ["ml.MXB%d" % fc])
                TS(XCt[:], e[:, :, 3:3 + Ls], convw[:, l, 3, fc:fc + 1], convb[:, l, fc:fc + 1], ALU.mult, ALU.add,
                   [en, "convw", "convb"], ["ml.XCt"])
                for jj in (2, 1, 0):
                    STT(XCt[:], e[:, :, jj:jj + Ls], convw[:, l, jj, fc:fc + 1], XCt[:], ALU.mult, ALU.add,
                        [en, "convw", "ml.XCt"], ["ml.XCt"])
                ACT(XC[:, fc, :], XCt[:].rearrange("p b j -> p (b j)"), AF.Silu, ["ml.XCt"], ["ml.XC%d" % fc])
                COPY(SS["cv_out"](fc), e[:, :, Ls:Ls + 3], [en], [SS["cv_out_name"]], eng="dve")
            for tl in range(4):
                cons_mx.fc0 = 2 * tl
                proj_fm(M, l, 5120 + 256 * tl, 1, cons_mx, tok=(tk0, tk0 + TT_), bank0=0, nbank=4)
            XCN = ["ml.XC%d" % i for i in range(8)]; MXN = ["ml.MXB%d" % i for i in range(8)]
            wq, wqn = wload(W["mlstm_wq"][l].rearrange("h (dc p) e -> p h dc e", p=128), [128, 4, 2, 256])
            wk, wkn = wload(W["mlstm_wk"][l].rearrange("h (dc p) e -> p h dc e", p=128), [128, 4, 2, 256])
            wv, wvn = wload(W["mlstm_wv"][l].rearrange("h (dc p) e -> p h dc e", p=128), [128, 4, 2, 256])
            bi = 0
            for (wt, wn, src, srcn, dst, dn) in ((wq, wqn, XC, XCN, QT, "ml.QT"), (wk, wkn, XC, XCN, KT, "ml.KT"),
                                                 (wv, wvn, MXB, MXN, VTt, "ml.VTt")):
                for h in range(4):
                    for ec in range(2):
                        bk = bi % 4; bi += 1
                        MM(banks[bk][:, 0:TT_], [(wt[:, h, dc, ec * 128:(ec + 1) * 128], src[:, 2 * h + dc, :]) for dc in range(2)],
                           [wn, srcn[2 * h], srcn[2 * h + 1]], [PB[bk]])
                        COPY(dst[:, 2 * h + ec, :], banks[bk][:, 0:TT_], [PB[bk]], ["%s%d" % (dn, 2 * h + ec)])
            QTN = ["ml.QT%d" % i for i in range(8)]; KTN = ["ml.KT%d" % i for i in range(8)]; VTN_ = ["ml.VTt%d" % i for i in range(8)]
            for (wt, wn, src, srcn, dst, dn) in ((wk, wkn, XC, XCN, KTM, "ml.KTM"), (wv, wvn, MXB, MXN, VTM, "ml.VTM")):
                for blk in range(NBK):
                    for h in range(4):
                        bk = 4 + (bi % 4); bi += 1
                        MM(banks[bk][:, 0:256],
                           [(src[:, 2 * h + dc, blk * 128:(blk + 1) * 128], wt[:, h, dc, :]) for dc in range(2)],
                           [wn, srcn[2 * h], srcn[2 * h + 1]], [PB[bk]])
                        COPY(dst[:, blk, h * 256:(h + 1) * 256], banks[bk][:, 0:256], [PB[bk]], ["%s%d.%d" % (dn, blk, h)])
            for blk in range(NBK):
                srcs = [(QT, QTN), (KT, KTN), (VTt, VTN_)]
                pairs = []; rd = ["wgate"]
                for gi, (src, srcn) in enumerate(srcs):
                    for c in range(8):
                        pairs.append((src[:, c, blk * 128:(blk + 1) * 128], wgate[:, l, gi * 8 + c, :]))
                        rd.append(srcn[c])
                MM(banks[blk % 2][:, 0:8], pairs, rd, [PB[blk % 2]])
                TT(GT[:, blk, :], banks[blk % 2][:, 0:8], bgate[:, l, :], ALU.add, [PB[blk % 2], "bgate"], ["ml.GT"])
            ACT(LS_[:], GT[:, :, 4:8], AF.Sigmoid, ["ml.GT"], ["ml.LS"])
            ACT(LF[:], LS_[:], AF.Ln, ["ml.LS"], ["ml.LF"])
            def cons_mo(tl, blk, bank, bname):
                b_ = blk - (tk0 // 128)
                ACT(SMO[:, b_, tl * 256:(tl + 1) * 256], bank, AF.Sigmoid, [bname], ["ml.SMO%d.%d" % (b_, tl)])
            proj_tm(M, l, 6144, 4, range(tk0 // 128, tk0 // 128 + NBK), cons_mo, bank0=2, nbank=4)
            P.barrier()
            S.reset(mk1)
            sm = lambda n: alloc(S, [128, n])
            BMT = sm(8); MP = sm(4); BM = sm(4); VEC = sm(4); RMAX = sm(4); NMT = sm(4); WPV = sm(4)
            ENM = sm(4); DSUM = sm(4); DEN = sm(4); RDEN = sm(4); SSQ = sm(4); RSTD = sm(4); SCL = sm(4)
            BL = sm(8); D2 = sm(4); D3 = sm(4); WIN = sm(4); GP = sm(4); QN = sm(4)
            RR = alloc(S, [128, nslot, 4]); GPB = alloc(S, [128, nslot, 4]); WINM = alloc(S, [128, nslot, 4])
            D1 = [alloc(S, [128, 128]) for _ in range(2)]
            L_ = alloc(S, [128, 4, 128]); Wt = alloc(S, [128, 4, 128])
            Sb = alloc(S, [128, 4, 128], BF16); ST_ = alloc(S, [128, 4, 128], BF16)
            IA = alloc(S, [128, 4, 256]); IT = alloc(S, [128, 2, 4, 128]); ITn = alloc(S, [1, 4, 128])
            NUM = alloc(S, [128, 4, 256]); HG = alloc(S, [128, 4, 256], BF16); JK = alloc(S, [128, 256])
            CB = [alloc(S, [128, 4, 2, 256], BF16) for _ in range(1 if m == "p" else 2)]
            NBb = [alloc(S, [128, 4, 2], BF16) for _ in range(2)]
            KW = [alloc(S, [128, 256], BF16) for _ in range(2)]
            MT = BMT[:, 4:8]
            Bc = BMT[:, 0:4]
            for blk in range(NBK):
                gb = tk0 // 128 + blk
                tsl = slice(blk * 128, (blk + 1) * 128)
                IG = GT[:, blk, 0:4]
                MM(banks[0][:, 0:4], [(tri[:], LF[:, blk, :])], ["ml.LF"] + CN, [PB[0]])
                COPY(Bc, banks[0][:, 0:4], [PB[0]], ["ml.Bc"], eng="dve")
                mst, mstn = SS["m_in"]()
                MM(banks[0][:, 8:12], [(E[:], mst)], [mstn] + CN, [PB[0]])
                COPY(MP[:], banks[0][:, 8:12], [PB[0]], ["ml.MP"], eng="dve")
                TT(BM[:], Bc, MP[:], ALU.add, ["ml.Bc", "ml.MP"], ["ml.BM"])
                TT(VEC[:], IG, Bc, ALU.subtract, ["ml.GT", "ml.Bc"], ["ml.VEC"])
                for h in range(4):
                    d1 = D1[h % 2]; dn_ = "ml.D1%d" % (h % 2)
                    TS(d1[:], ident_f[:], VEC[:, h:h + 1], None, ALU.mult, None, ["c_ident", "ml.VEC"], [dn_])
                    MM(banks[1][:, h * 128:(h + 1) * 128], [(ones_f[:], d1[:])], ["ones_f", dn_], [PB[1]])
                for h in range(4):
                    STT(L_[:, h, :], banks[1][:, h * 128:(h + 1) * 128], BMT[:, h:h + 1], neg[:], ALU.add, ALU.add,
                        [PB[1], "ml.Bc"] + CN, ["ml.L"])
                P.op("dve", lambda e: e.tensor_reduce(out=RMAX[:], in_=L_[:], axis=AX.X, op=ALU.max), ["ml.L"], ["ml.RMAX"])
                TT(MT, RMAX[:], BM[:], ALU.max, ["ml.RMAX", "ml.BM"], ["ml.MT"])
                TS(NMT[:], MT, -1.0, None, ALU.mult, None, ["ml.MT"], ["ml.NMT"])
                for h in range(4):
                    ACT(Wt[:, h, :], L_[:, h, :], AF.Exp, ["ml.L", "ml.NMT"], ["ml.W"], bias=NMT[:, h:h + 1])
                TT(D2[:], BM[:], MT, ALU.subtract, ["ml.BM", "ml.MT"], ["ml.D2"])
                ACT(WPV[:], D2[:], AF.Exp, ["ml.D2"], ["ml.WPV"])
                ACT(ENM[:], NMT[:], AF.Exp, ["ml.NMT"], ["ml.ENM"])
                for h in range(4):
                    MM(banks[2][:, h * 128:(h + 1) * 128],
                       [(QT[:, 2 * h + dc, tsl], KT[:, 2 * h + dc, tsl]) for dc in range(2)],
                       [QTN[2 * h], QTN[2 * h + 1], KTN[2 * h], KTN[2 * h + 1]], [PB[2]])
                STT(Sb[:].rearrange("p h s -> p (h s)"), banks[2][:, 0:512], 1.0 / 16, Wt[:].rearrange("p h s -> p (h s)"),
                    ALU.mult, ALU.mult, [PB[2], "ml.W"], ["ml.S"])
                P.op("dve", lambda e: e.tensor_reduce(out=DSUM[:], in_=Sb[:], axis=AX.X, op=ALU.add), ["ml.S"], ["ml.DSUM"])
                for h in range(4):
                    TR(banks_b[3][:, h * 128:(h + 1) * 128], Sb[:, h, :], ident_b[:], ["ml.S", "ident_b"], [PB[3]])
                COPY(ST_[:].rearrange("p h s -> p (h s)"), banks_b[3][:, 0:512], [PB[3]], ["ml.ST"])
                VN = lambda h: "ml.VTM%d.%d" % (blk, h)
                KN = lambda h: "ml.KTM%d.%d" % (blk, h)
                for h in range(4):
                    bk = 4 + h // 2
                    MM(banks[bk][:, (h % 2) * 256:(h % 2) * 256 + 256], [(ST_[:, h, :], VTM[:, blk, h * 256:(h + 1) * 256])],
                       ["ml.ST", VN(h)], [PB[bk]])
                COPY(IA[:, 0:2, :].rearrange("p h v -> p (h v)"), banks[4][:, 0:512], [PB[4]], ["ml.IA"])
                COPY(IA[:, 2:4, :].rearrange("p h v -> p (h v)"), banks[5][:, 0:512], [PB[5]], ["ml.IA"])
                MM(banks[3][:, 0:8], [(G[:], BMT[:])], ["ml.Bc", "ml.MT"] + CN, [PB[3]])
                COPY(BL[:], banks[3][:, 0:8], [PB[3]], ["ml.BL"], eng="dve")
                TT(D2[:], BL[:, 0:4], Bc, ALU.subtract, ["ml.BL", "ml.Bc"], ["ml.D2"])
                TT(D3[:], IG, BL[:, 4:8], ALU.subtract, ["ml.GT", "ml.BL"], ["ml.D3"])
                TT(D2[:], D2[:], D3[:], ALU.add, ["ml.D2", "ml.D3"], ["ml.D2"])
                ACT(WIN[:], D2[:], AF.Exp, ["ml.D2"], ["ml.WIN"], bias=-math.log(16.0))
                TT(D3[:], BL[:, 0:4], MP[:], ALU.add, ["ml.BL", "ml.MP"], ["ml.D3"])
                TT(D3[:], D3[:], BL[:, 4:8], ALU.subtract, ["ml.D3", "ml.BL"], ["ml.D3"])
                ACT(GP[:], D3[:], AF.Exp, ["ml.D3"], ["ml.GP"])
                TT(RR[:], GP[:].unsqueeze(1).broadcast_to([128, nslot, 4]), last[:].unsqueeze(2).broadcast_to([128, nslot, 4]),
                   ALU.mult, ["ml.GP"] + CN, ["ml.RR"])
                MM(banks[3][:, 16:16 + nslot * 4], [(ones_f[:], RR[:].rearrange("p s h -> p (s h)"))], ["ones_f", "ml.RR"], [PB[3]])
                COPY(GPB[:].rearrange("p s h -> p (s h)"), banks[3][:, 16:16 + nslot * 4], [PB[3]], ["ml.GPB"], eng="dve")
                TT(WINM[:], WIN[:].unsqueeze(1).broadcast_to([128, nslot, 4]), smask[:].unsqueeze(2).broadcast_to([128, nslot, 4]),
                   ALU.mult, ["ml.WIN"] + CN, ["ml.WINM"])
                ki = 0
                for slot in range(nslot):
                    cin, cinn, nin, ninn = SS["c_in"](slot)
                    cout, coutn, nout, noutn, after = SS["c_out"](slot)
                    cb = CB[slot % len(CB)]; cbn = "ml.CB%d" % (slot % len(CB))
                    nb = NBb[slot % 2]; nbn = "ml.NBb%d" % (slot % 2)
                    COPY(cb[:].rearrange("p a h v -> p (a h v)"), cin.rearrange("p a h v -> p (a h v)"), [cinn], [cbn])
                    COPY(nb[:].rearrange("p a h -> p (a h)"), nin.rearrange("p a h -> p (a h)"), [ninn], [nbn], eng="dve")
                    t0 = blk * 128 + slot * Ls if m == "s" else blk * 128
                    ln = Ls if m == "s" else 128
                    o0 = slot * Ls if m == "s" else 0
                    for h in range(4):
                        qrd = [QTN[2 * h], QTN[2 * h + 1]]
                        for vc in range(2):
                            MM(banks[6 + vc][:, h * 128 + o0:h * 128 + o0 + ln],
                               [(cb[:, h, dc, vc * 128:(vc + 1) * 128], QT[:, 2 * h + dc, t0:t0 + ln]) for dc in range(2)],
                               [cbn] + qrd, [PB[6 + vc]])
                        MM(banks[0][0:1, h * 128 + o0:h * 128 + o0 + ln],
                           [(nb[:, h, dc:dc + 1], QT[:, 2 * h + dc, t0:t0 + ln]) for dc in range(2)], [nbn] + qrd, [PB[0]])
                    for h in range(4):
                        kw = KW[ki % 2]; kwn = "ml.KW%d" % (ki % 2)
                        bk = 4 + (ki % 2)
                        nc0 = 300 + 2 * (ki % 2)
                        ki += 1
                        TS(kw[:], KTM[:, blk, h * 256:(h + 1) * 256], WINM[:, slot, h:h + 1], None, ALU.mult, None,
                           [KN(h), "ml.WINM"], [kwn])
                        for dc in range(2):
                            MM(banks[bk][:, dc * 256:(dc + 1) * 256], [(kw[:, dc * 128:(dc + 1) * 128], VTM[:, blk, h * 256:(h + 1) * 256])],
                               [kwn, VN(h)], [PB[bk]])
                        for dc in range(2):
                            MM(banks[3][:, nc0 + dc:nc0 + dc + 1], [(kw[:, dc * 128:(dc + 1) * 128], ones_b[:, 0:1])],
                               [kwn, "ones_b"], [PB[3]])
                        STT(cout[:, h, :, :], cin[:, h, :, :], GPB[:, slot, h:h + 1],
                            banks[bk][:, 0:512].rearrange("p (a v) -> p a v", a=2), ALU.mult, ALU.add,
                            [cinn, "ml.GPB", PB[bk]], [coutn])
                        STT(nout[:, h, :], nin[:, h, :], GPB[:, slot, h:h + 1], banks[3][:, nc0:nc0 + 2], ALU.mult, ALU.add,
                            [ninn, "ml.GPB", PB[3]], [noutn])
                    if after is not None:
                        after()
                for vc in range(2):
                    COPY(IT[:, vc].rearrange("p h t -> p (h t)"), banks[6 + vc][:, 0:512], [PB[6 + vc]], ["ml.IT%d" % vc])
                COPY(ITn[:].rearrange("p h t -> p (h t)"), banks[0][0:1, 0:512], [PB[0]], ["ml.ITn"])
                for h in range(4):
                    bk = 6 + h // 2
                    for vc in range(2):
                        TR(banks[bk][:, (h % 2) * 256 + vc * 128:(h % 2) * 256 + vc * 128 + 128], IT[:, vc, h, :], ident_f[:],
                           ["ml.IT%d" % vc, "c_ident"], [PB[bk]])
                    TR(banks[1][:, h:h + 1], ITn[0:1, h, :], ident_f[0:1, 0:1], ["ml.ITn", "c_ident"], [PB[1]])
                COPY(QN[:], banks[1][:, 0:4], [PB[1]], ["ml.QN"], eng="dve")
                for h in range(4):
                    bk = 6 + h // 2
                    STT(NUM[:, h, :], banks[bk][:, (h % 2) * 256:(h % 2) * 256 + 256], WPV[:, h:h + 1], IA[:, h, :], ALU.mult, ALU.add,
                        [PB[bk], "ml.WPV", "ml.IA"], ["ml.NUM"])
                TT(DEN[:], WPV[:], QN[:], ALU.mult, ["ml.WPV", "ml.QN"], ["ml.DEN"])
                TT(DEN[:], DEN[:], DSUM[:], ALU.add, ["ml.DEN", "ml.DSUM"], ["ml.DEN"])
                ACT(DEN[:], DEN[:], AF.Abs, ["ml.DEN"], ["ml.DEN"])
                TT(DEN[:], DEN[:], ENM[:], ALU.max, ["ml.DEN", "ml.ENM"], ["ml.DEN"])
                P.op("dve", lambda e: e.reciprocal(out=RDEN[:], in_=DEN[:]), ["ml.DEN"], ["ml.RDEN"])
                for h in range(4):
                    P.op("act", lambda e, h=h: e.activation(out=JK[:], in_=NUM[:, h, :], func=AF.Square, scale=RDEN[:, h:h + 1],
                                                             accum_out=SSQ[:, h:h + 1]),
                         ["ml.NUM", "ml.RDEN"], ["ml.SSQ", "ml.JK"])
                ACT(RSTD[:], SSQ[:], AF.Ln, ["ml.SSQ"], ["ml.RSTD"], scale=1.0 / 256, bias=EPS)
                ACT(RSTD[:], RSTD[:], AF.Exp, ["ml.RSTD"], ["ml.RSTD"], scale=-0.5)
                TT(SCL[:], RDEN[:], RSTD[:], ALU.mult, ["ml.RDEN", "ml.RSTD"], ["ml.SCL"])
                for h in range(4):
                    STT(HG[:, h, :], NUM[:, h, :], SCL[:, h:h + 1], SMO[:, blk, h * 256:(h + 1) * 256], ALU.mult, ALU.mult,
                        ["ml.NUM", "ml.SCL", "ml.SMO%d.%d" % (blk, h)], ["ml.HG"])
                for h in range(4):
                    for vc in range(2):
                        TR(banks_b[2][:, (2 * h + vc) * 128:(2 * h + vc + 1) * 128], HG[:, h, vc * 128:(vc + 1) * 128], ident_b[:],
                           ["ml.HG", "ident_b"], [PB[2]])
                for c in range(8):
                    TS(HM[:, c, gb * 128:(gb + 1) * 128], banks_b[2][:, c * 128:(c + 1) * 128], mnorm[:, l, c:c + 1], None,
                       ALU.mult, None, [PB[2], "mnorm"], ["HM.%d" % c])
                mo, mon = SS["m_out"]()
                MM(banks[3][0:nseq, 100:104], [(last[:], MT)], ["ml.MT"] + CN, [PB[3]])
                COPY(mo, banks[3][0:nseq, 100:104], [PB[3]], [mon], eng="dve")
            P.barrier()
        S.reset(mk0)
    def merge(M, l, OG, YB, HM):
        T = M.T
        S = M.S; S.reset(); mk = S.mark()
        MB = alloc(S, [128, 16, T], BF16)
        SG = [alloc(S, [128, T]) for _ in range(4)]
        ACC = alloc(S, [128, T]); TM_ = alloc(S, [128, T])
        if M.m == "p":
            for i in range(2):
                extra_slots.append((S.take(8192), "ringx%d" % i))
        OGN = ["OG.%d" % i for i in range(8)]; YBN = ["YB.%d" % i for i in range(8)]; HMN = ["HM.%d" % i for i in range(8)]
        sgi = [0]

        def gate_tile(b, tp):
            return wload(W["w_in"][l, :, 7168 + b * D + 256 * tp:7168 + b * D + 256 * tp + 256].rearrange("(kc p) n -> p kc n", p=128),
                         [128, 16, 256])

        def br_tile(name, tp):
            return wload(W[name][l, :, 256 * tp:256 * tp + 256].rearrange("(kc p) n -> p kc n", p=128), [128, 8, 256])

        def gate(tw, bw, ch, bk):
            MM(banks[bk][:, 0:T], [(tw[:, kc, ch * 128:(ch + 1) * 128], M.H[:, kc, :]) for kc in range(16)], [bw] + HALL, [PB[bk]])
            sg = SG[sgi[0] % 4]; sgn = "mg.SG%d" % (sgi[0] % 4); sgi[0] += 1
            ACT(sg[:], banks[bk][:, 0:T], AF.Sigmoid, [PB[bk]], [sgn])
            return sg, sgn

        def branch(tw, bw, ch, bk, src, srcn):
            MM(banks[bk][:, 0:T], [(tw[:, kc, ch * 128:(ch + 1) * 128], src[:, kc, :]) for kc in range(8)], [bw] + srcn, [PB[bk]])

        GBK = [0, 1, 4, 5]; BBK = [2, 3, 6, 7]
        gi = [0]; bi_ = [0]

        def gbank():
            k = GBK[gi[0] % 4]; gi[0] += 1
            return k

        def bbank():
            k = BBK[bi_[0] % 4]; bi_[0] += 1
            return k

        for tp in range(8):
            tg, bg = gate_tile(0, tp)
            ta, ba = br_tile("w_hgrn_out", tp)
            for ch in range(2):
                sg, sgn = gate(tg, bg, ch, gbank())
                bk = bbank()
                branch(ta, ba, ch, bk, OG, OGN)
                j = 2 * tp + ch
                TT(M.FFA(j), sg[:], banks[bk][:, 0:T], ALU.mult, [sgn, PB[bk]], ["mg.ACC%d" % (j % 4)])
            tg, bg = gate_tile(1, tp)
            ta, ba = br_tile("w_s5_glu_a", tp)
            tb, bb = br_tile("w_s5_glu_b", tp)
            for ch in range(2):
                j = 2 * tp + ch
                sg, sgn = gate(tg, bg, ch, gbank())
                ka = bbank(); kb = bbank()
                branch(ta, ba, ch, ka, YB, YBN)
                branch(tb, bb, ch, kb, YB, YBN)
                ACT(TM_[:], banks[kb][:, 0:T], AF.Sigmoid, [PB[kb]], ["mg.TM"])
                TT(TM_[:], TM_[:], banks[ka][:, 0:T], ALU.mult, ["mg.TM", PB[ka]], ["mg.TM"])
                TT(TM_[:], TM_[:], sg[:], ALU.mult, ["mg.TM", sgn], ["mg.TM"])
                TT(M.FFA(j), M.FFA(j), TM_[:], ALU.add, ["mg.ACC%d" % (j % 4), "mg.TM"], ["mg.ACC%d" % (j % 4)])
            tg, bg = gate_tile(2, tp)
            ta, ba = br_tile("w_mlstm_out", tp)
            for ch in range(2):
                j = 2 * tp + ch
                sg, sgn = gate(tg, bg, ch, gbank())
                bk = bbank()
                branch(ta, ba, ch, bk, HM, HMN)
                TT(TM_[:], sg[:], banks[bk][:, 0:T], ALU.mult, [sgn, PB[bk]], ["mg.TM"])
                TT(MB[:, j, :], M.FFA(j), TM_[:], ALU.add, ["mg.ACC%d" % (j % 4), "mg.TM"], ["mg.MB%d" % j])
        P.barrier()
        MBN = ["mg.MB%d" % j for j in range(16)]
        bi = 0
        for tp in range(8):
            tw, bw = wload(W["w_out"][l, :, 256 * tp:256 * tp + 256].rearrange("(kc p) n -> p kc n", p=128), [128, 16, 256])
            for ch in range(2):
                j = 2 * tp + ch
                bk = bi % 4; bi += 1
                MM(banks[bk][:, 0:T], [(tw[:, kc, ch * 128:(ch + 1) * 128], MB[:, kc, :]) for kc in range(16)], [bw] + MBN, [PB[bk]])
                COPY(M.FF[:, j, :], banks[bk][:, 0:T], [PB[bk]], [fn_(j)])
        del extra_slots[:]
        P.barrier()
        S.reset(mk)

    def load_x(M, src):
        S = M.S; S.reset(); mk = S.mark()
        xin = alloc(S, [128, D])
        for blk in range(M.NB):
            DMA(xin[:], src[blk * 128:(blk + 1) * 128, :], [], ["io.xin"])
            for cg in range(4):
                bk = cg
                for c4 in range(4):
                    c = cg * 4 + c4
                    TR(banks[bk][:, c4 * 128:(c4 + 1) * 128], xin[:, c * 128:(c + 1) * 128], ident_f[:], ["io.xin", "c_ident"], [PB[bk]])
                COPY(M.X[:, cg * 4:(cg + 1) * 4, blk * 128:(blk + 1) * 128], banks[bk][:, 0:512].rearrange("p (c t) -> p c t", c=4),
                     [PB[bk]], [xn(cg * 4 + i) for i in range(4)])
        P.barrier()
        S.reset(mk)

    def store_y(M, dst):
        S = M.S; S.reset(); mk = S.mark()
        yo = [alloc(S, [128, D]) for _ in range(2)]
        for blk in range(M.NB):
            y = yo[blk % 2]; yn = "io.yo%d" % (blk % 2)
            for cg in range(4):
                bk = 4 + cg
                for c4 in range(4):
                    c = cg * 4 + c4
                    TR(banks[bk][:, c4 * 128:(c4 + 1) * 128], M.X[:, c, blk * 128:(blk + 1) * 128], ident_f[:], [xn(c), "c_ident"], [PB[bk]])
                COPY(y[:, cg * 512:(cg + 1) * 512], banks[bk][:, 0:512], [PB[bk]], [yn])
            DMA(dst[blk * 128:(blk + 1) * 128, :], y[:], [yn], [], key=yn)
        P.barrier()
        S.reset(mk)

    def layer(M, l, hooks):
        prenorm(M, l, 0)
        ffn(M, l, W["w_ffn1_up"], W["w_ffn1_down"])
        postnorm_residual(M, l, 1, True)
        prenorm(M, l, 2, barrier=True)
        OG = sbt([128, 8, M.T], BF16, M.FF_off)
        YB = sbt([128, 8, M.T], BF16, M.FF_off + 8 * M.T * 2)
        HM = sbt([128, 8, M.T], BF16, M.FF_off + 16 * M.T * 2)
        ACCt = sbt([128, 4, M.T], F32, M.FF_off + 24 * M.T * 2)
        M.FFA = lambda j: ACCt[:, j % 4, :]
        hgrn(M, l, OG, hooks["hg"])
        hooks["s5"](YB)
        mlstm(M, l, HM, hooks["ml"])
        merge(M, l, OG, YB, HM)
        postnorm_residual(M, l, 3, False)
        prenorm(M, l, 4)
        ffn(M, l, W["w_ffn2_up"], W["w_ffn2_down"])
        postnorm_residual(M, l, 5, True)

    if NT > 0:
        M = make_mode("p")
        M.FF_off = None
        M.FF_off = M.S.start - 16 * M.T * 4
        MEMSET(M.HGS[:].rearrange("p l h v -> p (l h v)"), 0.0, ["HGS%d.%d" % (l, h) for l in range(2) for h in range(8)])
        MEMSET(M.MLC[:].rearrange("p l h a v -> p (l h a v)"), 0.0, ["MLC0", "MLC1"])
        MEMSET(M.MLN[:].rearrange("p l h a -> p (l h a)"), 0.0, ["MLN0", "MLN1"])
        MEMSET(M.MST[:].rearrange("p l h -> p (l h)"), 0.0, ["MST0", "MST1"])
        MEMSET(M.XSr[:].rearrange("p l r -> p (l r)"), 0.0, ["XS0", "XS1"])
        MEMSET(M.XSi[:].rearrange("p l r -> p (l r)"), 0.0, ["XS0", "XS1"])
        MEMSET(M.CV[:].rearrange("p l j c -> p (l j c)"), 0.0, ["CV0", "CV1"])
        for ti in range(NT):
            load_x(M, xp[ti * 512:(ti + 1) * 512, :])
            for l in range(NL):
                def hg_st(hgp, slot, l=l):
                    aps = [M.HGS[:, l, 4 * hgp + hh, :] for hh in range(4)]
                    names = ["HGS%d.%d" % (l, 4 * hgp + hh) for hh in range(4)]
                    return aps, names, aps, names, None

                def s5_hook(YB, l=l):
                    s5(M, l, YB, lambda r: M.XSr[:, l, r:r + 1], lambda r: M.XSi[:, l, r:r + 1],
                       lambda r: M.XSr[:, l, r:r + 1], lambda r: M.XSi[:, l, r:r + 1], "XS%d" % l, "XS%d" % l)
                ml = {
                    "cv_in": lambda fc, l=l: M.CV[:, l, :, fc].unsqueeze(1), "cv_in_name": "CV%d" % l,
                    "cv_out": lambda fc, l=l: M.CV[:, l, :, fc].unsqueeze(1), "cv_out_name": "CV%d" % l,
                    "m_in": lambda l=l: (M.MST[0:1, l, :], "MST%d" % l),
                    "m_out": lambda l=l: (M.MST[0:1, l, :], "MST%d" % l),
                    "c_in": lambda slot, l=l: (M.MLC[:, l], "MLC%d" % l, M.MLN[:, l], "MLN%d" % l),
                    "c_out": lambda slot, l=l: (M.MLC[:, l], "MLC%d" % l, M.MLN[:, l], "MLN%d" % l, None),
                }
                layer(M, l, {"hg": hg_st, "s5": s5_hook, "ml": ml})
            store_y(M, yp[ti * 512:(ti + 1) * 512, :])
        for l in range(NL):
            DMA(p_hg[l].rearrange("h k v -> k h v"), M.HGS[:, l], ["HGS%d.%d" % (l, h) for h in range(8)], [], key="o_p_hg")
            DMA(p_c[l].rearrange("h (a p) v -> p (h a) v", p=128), M.MLC[:, l].rearrange("p h a v -> p (h a) v"), ["MLC%d" % l], [], key="o_p_c")
            DMA(p_n[l].rearrange("h (a p) -> p (h a)", p=128), M.MLN[:, l].rearrange("p h a -> p (h a)"), ["MLN%d" % l], [], key="o_p_n", slow=True)
            DMA(p_m[l:l + 1, :], M.MST[0:1, l, :], ["MST%d" % l], [], key="o_p_m")
            DMA(p_conv[l].rearrange("j (c p) -> p (j c)", p=128), M.CV[:, l].rearrange("p j c -> p (j c)"), ["CV%d" % l], [], key="o_p_cv", slow=True)
            S = M.S; S.reset()
            for part, (src, dst) in enumerate(((M.XSr, p_re), (M.XSi, p_im))):
                o = alloc(S, [32, 128])
                TR(banks[part][0:32, 0:128], src[:, l, :], ident_f[:], ["XS%d" % l, "c_ident"], [PB[part]])
                COPY(o[:], banks[part][0:32, 0:128], [PB[part]], ["io.xs%d" % part])
                DMA(dst[l], o[:], ["io.xs%d" % part], [], key="o_p_xs%d" % part)
            P.barrier()

    if SAMPLE:
        P.barrier()
        M = make_mode("s")
        M.FF_off = M.S.start - 16 * M.T * 4
        S = M.S
        HGB = [alloc(S, [128, 4, 128]) for _ in range(4)]
        CIN = [alloc(S, [128, 4, 2, 256]) for _ in range(3)]
        NINs = alloc(S, [128, 16, 4, 2]); NOUTs = alloc(S, [128, 16, 4, 2])
        MSI = alloc(S, [16, 4]); MSO = alloc(S, [16, 4])
        CVI = alloc(S, [128, 16, 3, 8]); CVO = alloc(S, [128, 16, 3, 8])
        XT = alloc(S, [16, 4096])
        XSs = [alloc(S, [128, 32, 16]) for _ in range(2)]
        XNs = [alloc(S, [128, 32, 16]) for _ in range(2)]
        M.S = Region(S.cur, S.start + S.size - S.cur)
        load_x(M, xs)
        for l in range(NL):
            hg_cnt = [0]

            def hg_st(hgp, slot, l=l):
                i = hg_cnt[0] % 4; hg_cnt[0] += 1
                buf = HGB[i]; bn = "HGB%d" % i
                DMA(buf[:], st_hg[l, slot, 4 * hgp:4 * hgp + 4].rearrange("h k v -> k h v"), [], [bn], q="poolq")
                aps = [buf[:, hh, :] for hh in range(4)]
                names = [bn] * 4

                def after():
                    DMA(s_hg[l, slot, 4 * hgp:4 * hgp + 4].rearrange("h k v -> k h v"), buf[:], [bn], [], key=bn + "o")
                return aps, names, aps, names, after

            def s5_hook(YB, l=l):
                for part, src in enumerate((st_re, st_im)):
                    DMA(XT[:], src[l], [], ["XT"])
                    for r in range(32):
                        TR(banks[part][:, r * 16:(r + 1) * 16], XT[:, r * 128:(r + 1) * 128], ident_f[0:16, 0:16], ["XT", "c_ident"], [PB[part]])
                    COPY(XSs[part][:].rearrange("p r b -> p (r b)"), banks[part][:, 0:512], [PB[part]], ["XSs"])
                s5(M, l, YB, lambda r: XSs[0][:, r, :], lambda r: XSs[1][:, r, :],
                   lambda r: XNs[0][:, r, :], lambda r: XNs[1][:, r, :], "XSs", "XNs")
                for part, dst in enumerate((s_re, s_im)):
                    for r4 in range(8):
                        bk = 2 + (r4 % 2)
                        for rr in range(4):
                            r = 4 * r4 + rr
                            TR(banks[bk][0:16, rr * 128:(rr + 1) * 128], XNs[part][:, r, :], ident_f[:], ["XNs", "c_ident"], [PB[bk]])
                        COPY(XT[:, r4 * 512:(r4 + 1) * 512], banks[bk][0:16, 0:512], [PB[bk]], ["XT"])
                    DMA(dst[l], XT[:], ["XT"], [], key="o_s_xs")
                P.barrier()

            DMA(NINs[:].rearrange("p b h a -> p (b h a)"), st_n[l].rearrange("b h (a p) -> p (b h a)", p=128), [], ["NIN"], slow=True)
            DMA(MSI[:], st_m[l], [], ["MSI"])
            DMA(CVI[:].rearrange("p b j c -> p (b j c)"), st_conv[l].rearrange("b j (c p) -> p (b j c)", p=128), [], ["CVI"], slow=True)
            c_cnt = [0]
            cmap = {}

            def c_in(slot, l=l):
                if slot not in cmap:
                    i = c_cnt[0] % 3; c_cnt[0] += 1
                    cmap[slot] = i
                    DMA(CIN[i][:].rearrange("p h a v -> p (h a) v"), st_c[l, slot].rearrange("h (a p) v -> p (h a) v", p=128), [], ["CIN%d" % i], q="poolq")
                i = cmap[slot]
                return CIN[i][:], "CIN%d" % i, NINs[:, slot], "NIN"

            def c_out(slot, l=l):
                i = cmap[slot]

                def after():
                    DMA(s_c[l, slot].rearrange("h (a p) v -> p (h a) v", p=128), CIN[i][:].rearrange("p h a v -> p (h a) v"), ["CIN%d" % i], [], key="CIN%do" % i)
                return CIN[i][:], "CIN%d" % i, NOUTs[:, slot], "NOUT", after
            ml = {
                "cv_in": lambda fc: CVI[:, :, :, fc], "cv_in_name": "CVI",
                "cv_out": lambda fc: CVO[:, :, :, fc], "cv_out_name": "CVO",
                "m_in": lambda: (MSI[:], "MSI"),
                "m_out": lambda: (MSO[:], "MSO"),
                "c_in": c_in, "c_out": c_out,
            }
            layer(M, l, {"hg": hg_st, "s5": s5_hook, "ml": ml})
            DMA(s_n[l].rearrange("b h (a p) -> p (b h a)", p=128), NOUTs[:].rearrange("p b h a -> p (b h a)"), ["NOUT"], [], key="o_s_n", slow=True)
            DMA(s_m[l], MSO[:], ["MSO"], [], key="o_s_m")
            DMA(s_conv[l].rearrange("b j (c p) -> p (b j c)", p=128), CVO[:].rearrange("p b j c -> p (b j c)"), ["CVO"], [], key="o_s_cv", slow=True)
            P.barrier()
        store_y(M, ys)

    nops, nsem = P.emit()
    return nc, consts, dbg_outs, (nops, nsem)


_CACHE = {}


def _get_program(NT=4, SAMPLE=True):
    key = (NT, SAMPLE)
    if key not in _CACHE:
        _CACHE[key] = build_program(NT, SAMPLE)
    return _CACHE[key]


WNAMES = ["norm_gains", "w_ffn1_up", "w_ffn1_down", "w_in", "hgrn_lower_bounds", "hgrn_norm", "w_hgrn_out",
          "s5_a_re", "s5_a_im", "s5_log_dt", "s5_b_re", "s5_b_im", "s5_c_re", "s5_c_im", "s5_d", "w_s5_glu_a",
          "w_s5_glu_b", "mlstm_conv_w", "mlstm_conv_b", "mlstm_wq", "mlstm_wk", "mlstm_wv", "mlstm_w_gates",
          "mlstm_b_gates", "mlstm_norm", "w_mlstm_out", "w_out", "w_ffn2_up", "w_ffn2_down"]


def kernel(**inp):
    f32 = lambda a: np.ascontiguousarray(np.asarray(a, dtype=np.float32))
    nc, consts, _, _ = _get_program(4, True)
    ncore = 8
    shared = {k: f32(inp[k]) for k in WNAMES}
    for k, v in consts.items():
        shared["c_" + k] = v
    xpr = f32(inp["x_prompt"])
    xsm = f32(inp["x_sample"])
    sts = {"st_hg": f32(inp["state_hgrn"]), "st_re": f32(inp["state_s5_re"]).reshape(2, 128, 4096),
           "st_im": f32(inp["state_s5_im"]).reshape(2, 128, 4096), "st_c": f32(inp["state_mlstm_c"]),
           "st_n": f32(inp["state_mlstm_n"]), "st_m": f32(inp["state_mlstm_m"]), "st_conv": f32(inp["state_mlstm_conv"])}
    in_maps = []
    for c in range(ncore):
        m = dict(shared)
        m["xp"] = xpr[c % 4]
        m["xs"] = xsm[16 * c:16 * c + 16].reshape(128, D)
        for k, v in sts.items():
            m[k] = np.ascontiguousarray(v[:, 16 * c:16 * c + 16])
        in_maps.append(m)
    res = run_bass_kernel_spmd(nc, in_maps, core_ids=list(range(ncore)))
    R = res.results
    y_prompt = np.stack([R[j]["yp"] for j in range(4)], 0)
    y_sample = np.concatenate([R[c]["ys"].reshape(16, 8, D) for c in range(ncore)], 0)

    def pst(name, shape):
        return np.stack([R[j][name] for j in range(4)], 1).reshape(shape)

    def sst(name, shape):
        return np.concatenate([R[c][name] for c in range(ncore)], 1).reshape(shape)
    outs = (y_prompt, y_sample,
            pst("p_hg", (2, 4, 8, 128, 128)), pst("p_re", (2, 4, 64, 64)), pst("p_im", (2, 4, 64, 64)),
            pst("p_c", (2, 4, 4, 256, 256)), pst("p_n", (2, 4, 4, 256)), pst("p_m", (2, 4, 4)), pst("p_conv", (2, 4, 3, 1024)),
            sst("s_hg", (2, 128, 8, 128, 128)), sst("s_re", (2, 128, 64, 64)), sst("s_im", (2, 128, 64, 64)),
            sst("s_c", (2, 128, 4, 256, 256)), sst("s_n", (2, 128, 4, 256)), sst("s_m", (2, 128, 4)),
            sst("s_conv", (2, 128, 3, 1024)))
    return tuple(np.ascontiguousarray(o.astype(np.float32)) for o in outs)
```

```python
import contextlib
import math
import numpy as np
import concourse.bass as bass
import concourse.mybir as mybir
from concourse.bass_utils import run_bass_kernel_spmd

F32 = mybir.dt.float32
BF16 = mybir.dt.bfloat16
U8 = mybir.dt.uint8
AF = mybir.ActivationFunctionType
ALU = mybir.AluOpType
AX = mybir.AxisListType

D = 2048
DC = 16
FFD = 5632
FC = 44
MIX = 1024
NIN = 13312
EPS = 1e-6
NEG = -1e30
EPOCH = 20000


class Buf:
    __slots__ = ("name", "w", "r")

    def __init__(self, name):
        self.name = name
        self.w = None
        self.r = []


class Op:
    __slots__ = ("eng", "fn", "deps", "idx", "dma_key", "dma_cnt", "flag", "ordinal", "exempt")

    def __init__(self, eng, fn, idx):
        self.eng = eng
        self.fn = fn
        self.deps = set()
        self.idx = idx
        self.dma_key = None
        self.dma_cnt = 0
        self.flag = False
        self.ordinal = 0
        self.exempt = False


class Prog:
    PHYS = {"pe": "tensor", "act": "scalar", "dve": "vector", "pool": "gpsimd",
            "sp": "sync", "actq": "scalar", "poolq": "gpsimd"}

    def __init__(self, nc):
        self.nc = nc
        self.ops = []
        self.dma_counts = {}
        self.bufs = {}
        self.barrier_idx = None
        self.last_by_phys = {}
        self.pending_barrier = {}
        self.dmas_since = []

    def buf(self, name):
        b = self.bufs.get(name)
        if b is None:
            b = self.bufs[name] = Buf(name)
        return b

    def _track(self, op, reads, writes):
        for r in reads:
            r = self.buf(r) if isinstance(r, str) else r
            if r.w is not None:
                op.deps.add(r.w)
        for w in writes:
            w = self.buf(w) if isinstance(w, str) else w
            if w.w is not None:
                op.deps.add(w.w)
            for rr in w.r:
                op.deps.add(rr)
        for w in writes:
            w = self.buf(w) if isinstance(w, str) else w
            w.w = op.idx
            w.r = []
        for r in reads:
            r = self.buf(r) if isinstance(r, str) else r
            if r.w != op.idx:
                r.r.append(op.idx)
        op.deps.discard(op.idx)
        p = self.PHYS[op.eng]
        if not op.exempt:
            pend = self.pending_barrier.pop(p, None)
            if pend:
                op.deps.update(pend)
            if op.dma_key is None:
                self.last_by_phys[p] = op.idx
            else:
                self.dmas_since.append(op.idx)
        if op.eng == "pe":
            op.deps = set(d for d in op.deps if self.ops[d].eng != "pe")

    def barrier(self):
        lasts = set(self.last_by_phys.values()) | set(self.dmas_since)
        self.dmas_since = []
        for p in set(self.PHYS.values()):
            cur = self.pending_barrier.get(p, set())
            self.pending_barrier[p] = cur | lasts

    def op(self, eng, fn, reads=(), writes=()):
        o = Op(eng, fn, len(self.ops))
        self.ops.append(o)
        self._track(o, reads, writes)
        return o

    def dma(self, q, fn, reads=(), writes=(), key=None, exempt=False):
        o = Op(q, fn, len(self.ops))
        o.exempt = exempt
        if key is None:
            w0 = writes[0] if writes else reads[0]
            key = w0 if isinstance(w0, str) else w0.name
        o.dma_key = key
        self.dma_counts[key] = self.dma_counts.get(key, 0) + 16
        o.dma_cnt = self.dma_counts[key]
        self.ops.append(o)
        self._track(o, reads, writes)
        return o

    def emit(self, final_eng="sp"):
        nc = self.nc
        ops = self.ops
        phys_of = lambda o: self.PHYS[o.eng]
        for o in ops:
            for d in o.deps:
                ops[d].flag = True
        counters = {}
        for o in ops:
            if o.dma_key is None and o.flag:
                p = phys_of(o)
                counters[p] = counters.get(p, 0) + 1
                o.ordinal = counters[p]
        es = contextlib.ExitStack()
        sems = {}

        def getsem(name):
            s = sems.get(name)
            if s is None:
                s = sems[name] = es.enter_context(nc.semaphore("s_%d" % len(sems)))
            return s

        streams = {}
        for o in ops:
            streams.setdefault(phys_of(o), []).append(o)
        for p, n in counters.items():
            for e in range((n // EPOCH) + 1):
                getsem((p, e))
        for k in self.dma_counts:
            getsem(("dma", k))

        def sem_target(d):
            po = ops[d]
            if po.dma_key is not None:
                return ("dma", po.dma_key), po.dma_cnt
            p = phys_of(po)
            e, c = divmod(po.ordinal, EPOCH)
            if c == 0:
                e -= 1
                c = EPOCH
            return (p, e), c

        final_waits = {}
        for o in ops:
            if o.dma_key is not None:
                k = ("dma", o.dma_key)
                final_waits[k] = max(final_waits.get(k, 0), o.dma_cnt)
        fin_phys = self.PHYS[final_eng]

        with es:
            with nc.Block() as block:
                def make_section(p, lst):
                    def section(eng):
                        waited = {}
                        for o in lst:
                            need = {}
                            for d in o.deps:
                                k, c = sem_target(d)
                                if need.get(k, 0) < c:
                                    need[k] = c
                            for k, c in need.items():
                                if waited.get(k, 0) >= c:
                                    continue
                                eng.wait_ge(sems[k], c)
                                waited[k] = c
                            ins = o.fn(eng)
                            if o.dma_key is not None:
                                ins.then_inc(sems[("dma", o.dma_key)], 16)
                            elif o.flag:
                                k, c = sem_target(o.idx)
                                ins.then_inc(sems[k], 1)
                        if p == fin_phys:
                            for k, c in final_waits.items():
                                if waited.get(k, 0) < c:
                                    eng.wait_ge(sems[k], c)
                    return section

                for p, lst in streams.items():
                    getattr(block, p)(make_section(p, lst))
                if fin_phys not in streams:
                    getattr(block, fin_phys)(make_section(fin_phys, []))
        return len(ops), len(sems)


def make_consts():
    c = {}
    c["ident"] = np.eye(128, dtype=np.float32)
    t = np.arange(128)
    for mode, clen_h, slen in (("p", 32, 128), ("s", 8, 8)):
        T = 512 if mode == "p" else 128
        same = (t[:, None] // clen_h) == (t[None, :] // clen_h)
        c["hgm_" + mode] = (same & (t[:, None] <= t[None, :])).astype(np.float32)
        tt = np.arange(T)
        c["cst_" + mode] = np.broadcast_to(((tt % clen_h) != 0).astype(np.float32), (128, T)).copy()
        ns = 128 // clen_h
        c["crow_" + mode] = ((t[:, None] // clen_h) == np.arange(ns)[None, :]).astype(np.float32)
        sames = (t[:, None] // slen) == (t[None, :] // slen)
        valid_ts = sames & (t[None, :] <= t[:, None])
        c["neg_" + mode] = np.where(valid_ts, 0.0, NEG).astype(np.float32)
        c["tri_" + mode] = valid_ts.T.astype(np.float32)
        nsl = 128 // slen
        last = ((t[:, None] % slen) == slen - 1) & ((t[:, None] // slen) == np.arange(nsl)[None, :])
        c["last_" + mode] = last.astype(np.float32)
        lastrow = (t % slen) == slen - 1
        c["G_" + mode] = (sames & lastrow[:, None]).astype(np.float32)
        c["E_" + mode] = ((np.arange(nsl)[:, None]) == (t[None, :] // slen)).astype(np.float32)
        c["smask_" + mode] = ((t[:, None] // slen) == np.arange(nsl)[None, :]).astype(np.float32)
    c["s5st_p"] = np.broadcast_to((np.arange(512) != 0).astype(np.float32), (128, 512)).copy()
    c["s5st_s"] = np.broadcast_to(((np.arange(128) % 8) != 0).astype(np.float32), (128, 128)).copy()
    g2 = (np.arange(128) // 64)[:, None, None]
    j = np.arange(4)[None, :, None]
    gl = (np.arange(128) // 16)[None, None, :]
    c["s5cm"] = (gl == 2 * j + g2).astype(np.float32)
    return c


CONST_SHAPES = None


class Region:
    def __init__(self, start, size):
        self.start = start
        self.size = size
        self.cur = start

    def take(self, nbytes):
        nbytes = (nbytes + 63) // 64 * 64
        off = self.cur
        self.cur += nbytes
        assert self.cur <= self.start + self.size, ("region overflow", self.cur - self.start, self.size)
        return off

    def mark(self):
        return self.cur

    def reset(self, m=None):
        self.cur = self.start if m is None else m


def _prod(s):
    r = 1
    for v in s:
        r *= v
    return r


def build_program(NT=4, SAMPLE=True, DBG=None, NL=2, S5_POOL="dve"):
    nc = bass.Bass("TRN2", target_bir_lowering=False)
    P = Prog(nc)
    consts = make_consts()
    dbg_outs = {}

    def din(name, shape, dt=F32):
        return nc.dram_tensor(name, list(shape), dt, kind="ExternalInput").ap()

    def dout(name, shape, dt=F32):
        return nc.dram_tensor(name, list(shape), dt, kind="ExternalOutput").ap()

    def dint(name, shape, dt=F32):
        return nc.dram_tensor(name, list(shape), dt, kind="Internal").ap()

    TP = NT * 512
    xp = din("xp", [max(TP, 1), D])
    yp = dout("yp", [max(TP, 1), D])
    p_hg = dout("p_hg", [2, 8, 128, 128])
    p_re = dout("p_re", [2, 32, 128])
    p_im = dout("p_im", [2, 32, 128])
    p_c = dout("p_c", [2, 4, 256, 256])
    p_n = dout("p_n", [2, 4, 256])
    p_m = dout("p_m", [2, 4])
    p_conv = dout("p_conv", [2, 3, 1024])
    xs = din("xs", [128, D])
    ys = dout("ys", [128, D])
    st_hg = din("st_hg", [2, 16, 8, 128, 128])
    st_re = din("st_re", [2, 16, 4096])
    st_im = din("st_im", [2, 16, 4096])
    st_c = din("st_c", [2, 16, 4, 256, 256])
    st_n = din("st_n", [2, 16, 4, 256])
    st_m = din("st_m", [2, 16, 4])
    st_conv = din("st_conv", [2, 16, 3, 1024])
    s_hg = dout("s_hg", [2, 16, 8, 128, 128])
    s_re = dout("s_re", [2, 16, 4096])
    s_im = dout("s_im", [2, 16, 4096])
    s_c = dout("s_c", [2, 16, 4, 256, 256])
    s_n = dout("s_n", [2, 16, 4, 256])
    s_m = dout("s_m", [2, 16, 4])
    s_conv = dout("s_conv", [2, 16, 3, 1024])
    W = {}
    for name, shape in (("norm_gains", [2, 6, D]), ("w_ffn1_up", [2, D, 2 * FFD]), ("w_ffn1_down", [2, FFD, D]),
                        ("w_in", [2, D, NIN]), ("hgrn_lower_bounds", [2, MIX]), ("hgrn_norm", [2, MIX]),
                        ("w_hgrn_out", [2, MIX, D]), ("s5_a_re", [2, 64, 64]), ("s5_a_im", [2, 64, 64]),
                        ("s5_log_dt", [2, 64]), ("s5_b_re", [2, 64, 64, 16]), ("s5_b_im", [2, 64, 64, 16]),
                        ("s5_c_re", [2, 64, 16, 64]), ("s5_c_im", [2, 64, 16, 64]), ("s5_d", [2, 64, 16]),
                        ("w_s5_glu_a", [2, MIX, D]), ("w_s5_glu_b", [2, MIX, D]), ("mlstm_conv_w", [2, 4, MIX]),
                        ("mlstm_conv_b", [2, MIX]), ("mlstm_wq", [2, 4, 256, 256]), ("mlstm_wk", [2, 4, 256, 256]),
                        ("mlstm_wv", [2, 4, 256, 256]), ("mlstm_w_gates", [2, 3 * MIX, 8]),
                        ("mlstm_b_gates", [2, 8]), ("mlstm_norm", [2, MIX]), ("w_mlstm_out", [2, MIX, D]),
                        ("w_out", [2, D, D]), ("w_ffn2_up", [2, D, 2 * FFD]), ("w_ffn2_down", [2, FFD, D])):
        W[name] = din(name, shape)
    CD = {k: din("c_" + k, v.shape) for k, v in consts.items()}
    s5wb = dint("s5wb", [2, 8, 128, 2, 512])
    s5wc = dint("s5wc", [2, 8, 128, 2, 512])
    s5tab = dint("s5tab", [2, 8, 128, 2, 4, 512])

    ARENA = 211968
    arena = nc.alloc_sbuf_tensor("arena", [128, ARENA], U8)
    base = nc.lookup_mloc(arena).addr
    uid = [0]

    def sbt(shape, dtype, off):
        uid[0] += 1
        return nc.alloc_sbuf_tensor_at("t%d" % uid[0], list(shape), dtype, offset=base + off)

    def nbytes(shape, dtype):
        return _prod(shape[1:]) * (2 if dtype == BF16 else 4)

    def alloc(reg, shape, dtype=F32):
        return sbt(shape, dtype, reg.take(nbytes(shape, dtype)))

    R_CONST = Region(0, 19456)
    R_RING = Region(R_CONST.start + R_CONST.size, 3 * 8192)
    R_MODE = Region(R_RING.start + R_RING.size, ARENA - (R_RING.start + R_RING.size))

    banks = [nc.alloc_psum_tensor("pb%d" % i, [128, 512], F32) for i in range(8)]
    banks_b = [b.bitcast(BF16) for b in banks]
    PB = ["pb%d" % i for i in range(8)]

    def ACT(out, in_, func, reads, writes, **kw):
        P.op("act", lambda e: e.activation(out=out, in_=in_, func=func, **kw), reads, writes)

    def TT(out, in0, in1, op, reads, writes, eng="dve"):
        P.op(eng, lambda e: e.tensor_tensor(out=out, in0=in0, in1=in1, op=op), reads, writes)

    def TS(out, in0, s1, s2, op0, op1, reads, writes, eng="dve"):
        if op1 is None:
            P.op(eng, lambda e: e.tensor_scalar(out=out, in0=in0, scalar1=s1, scalar2=None, op0=op0), reads, writes)
        else:
            P.op(eng, lambda e: e.tensor_scalar(out=out, in0=in0, scalar1=s1, scalar2=s2, op0=op0, op1=op1), reads, writes)

    def STT(out, in0, scalar, in1, op0, op1, reads, writes, eng="dve"):
        P.op(eng, lambda e: e.scalar_tensor_tensor(out=out, in0=in0, scalar=scalar, in1=in1, op0=op0, op1=op1), reads, writes)

    def COPY(out, in_, reads, writes, eng="act"):
        if eng == "act":
            P.op("act", lambda e: e.activation(out=out, in_=in_, func=AF.Copy), reads, writes)
        else:
            P.op(eng, lambda e: e.tensor_copy(out=out, in_=in_), reads, writes)

    def MM(out, pairs, reads, writes, start=True, stop=True, skip=False):
        def f(e):
            n = len(pairs)
            ins = None
            for i, (l, r) in enumerate(pairs):
                kw = {}
                if skip:
                    kw["skip_group_check"] = True
                ins = e.matmul(out, l, r, start=(start and i == 0), stop=(stop and i == n - 1), **kw)
            return ins
        P.op("pe", f, reads, writes)

    def TR(out, in_, ident_ap, reads, writes):
        P.op("pe", lambda e: e.transpose(out, in_, ident_ap), reads, writes)

    def DMA(out, in_, reads, writes, q="sp", key=None, exempt=False, slow=False):
        if slow:
            P.dma(q, lambda e: e.dma_start(out=out, in_=in_, allow_slow_non_contiguous=True), reads, writes, key=key, exempt=exempt)
        else:
            P.dma(q, lambda e: e.dma_start(out=out, in_=in_), reads, writes, key=key, exempt=exempt)

    def interleave(gens):
        gens = list(gens)
        while gens:
            for g in list(gens):
                try:
                    next(g)
                except StopIteration:
                    gens.remove(g)

    def MEMSET(ap, val, writes, eng="dve"):
        P.op(eng, lambda e: e.memset(ap, val), [], writes)

    def dbg(name, ap, shape, reads):
        if DBG is None or name not in DBG:
            return
        o = dout("dbg_" + name, shape)
        dbg_outs[name] = shape
        DMA(o, ap, reads, [], key="dbg_" + name)

    ring_i = [0]
    ring_views = {}
    extra_slots = []

    def ring(shape, dtype=BF16):
        n = 3 + len(extra_slots)
        s = ring_i[0] % n
        ring_i[0] += 1
        if s < 3:
            off, bname, perm = R_RING.start + s * 8192, "ring%d" % s, True
        else:
            off, bname = extra_slots[s - 3]
            perm = False
        key = (off, tuple(shape), dtype)
        t = ring_views.get(key)
        if t is None:
            assert nbytes(shape, dtype) <= 8192, shape
            t = ring_views[key] = sbt(shape, dtype, off)
        return t, bname, perm

    def wload(src, shape, dtype=BF16, q="poolq", reads=()):
        t, b, perm = ring(shape, dtype)
        DMA(t[:], src, list(reads), [b], q=q, exempt=perm)
        return t, b

    CS = {}
    for k, v in consts.items():
        if k.startswith("E_"):
            CS[k] = alloc(R_CONST, [v.shape[0], 128], F32)
        else:
            CS[k] = alloc(R_CONST, list(v.shape), F32)
        DMA(CS[k][:], CD[k], [], ["c_" + k])
    ident_f = CS["ident"]
    ident_b = alloc(R_CONST, [128, 128], BF16)
    ones_f = alloc(R_CONST, [128, 128], F32)
    ones_b = alloc(R_CONST, [128, 128], BF16)
    P.op("dve", lambda e: e.tensor_copy(out=ident_b[:], in_=ident_f[:]), ["c_ident"], ["ident_b"])
    nident_f = alloc(R_CONST, [128, 128], F32)
    TS(nident_f[:], ident_f[:], -1.0, None, ALU.mult, None, ["c_ident"], ["nident"])
    MEMSET(ones_f[:], 1.0, ["ones_f"])
    MEMSET(ones_b[:], 1.0, ["ones_b"])
    hgm_b = {}
    for m in "ps":
        hgm_b[m] = alloc(R_CONST, [128, 128], BF16)
        P.op("dve", lambda e, m=m: e.tensor_copy(out=hgm_b[m][:], in_=CS["hgm_" + m][:]), ["c_hgm_" + m], ["hgmb_" + m])
    gains = alloc(R_CONST, [128, 2, 6, 16])
    for l in range(2):
        for k in range(6):
            DMA(gains[:, l, k, :], W["norm_gains"][l, k].rearrange("(c p) -> p c", p=128), [], ["gains"], slow=True)
    gainsh = alloc(R_CONST, [128, 2, 6, 16])
    TS(gainsh[:], gains[:], 0.5, None, ALU.mult, None, ["gains"], ["gainsh"])
    lbraw = alloc(R_CONST, [128, 2, 8])
    DMA(lbraw[:], W["hgrn_lower_bounds"].rearrange("l (c p) -> p l c", p=128), [], ["lbraw"], slow=True)
    hnorm = alloc(R_CONST, [128, 2, 8])
    DMA(hnorm[:], W["hgrn_norm"].rearrange("l (c p) -> p l c", p=128), [], ["hnorm"], slow=True)
    mnorm = alloc(R_CONST, [128, 2, 8])
    DMA(mnorm[:], W["mlstm_norm"].rearrange("l (c p) -> p l c", p=128), [], ["mnorm"], slow=True)
    convw = alloc(R_CONST, [128, 2, 4, 8])
    DMA(convw[:], W["mlstm_conv_w"].rearrange("l j (c p) -> p l j c", p=128), [], ["convw"], slow=True)
    convb = alloc(R_CONST, [128, 2, 8])
    DMA(convb[:], W["mlstm_conv_b"].rearrange("l (c p) -> p l c", p=128), [], ["convb"], slow=True)
    s5d = alloc(R_CONST, [128, 2, 8])
    DMA(s5d[:], W["s5_d"].rearrange("l (c g) k -> (g k) l c", g=8), [], ["s5d"], slow=True)
    bgate = alloc(R_CONST, [128, 2, 8])
    for l in range(2):
        DMA(bgate[:, l, :], W["mlstm_b_gates"][l].partition_broadcast(128), [], ["bgate"], key="bgate%d" % l)
    wgate = alloc(R_CONST, [128, 2, 24, 8], BF16)
    DMA(wgate[:], W["mlstm_w_gates"].rearrange("l (c p) g -> p l c g", p=128), [], ["wgate"], q="poolq")
    lb = alloc(R_CONST, [128, 2, 8])
    oml = alloc(R_CONST, [128, 2, 8])
    lbe = alloc(R_CONST, [128, 2, 8])
    lbs = alloc(R_CONST, [128, 8])
    ACT(lbe[:], lbraw[:], AF.Exp, ["lbraw"], ["lbe"])
    TT(lbs[:], lbe[:, 0, :], lbe[:, 1, :], ALU.add, ["lbe"], ["lbs"])
    P.op("dve", lambda e: e.reciprocal(out=lbs[:], in_=lbs[:]), ["lbs"], ["lbs"])
    MEMSET(lb[:, 0, :], 0.0, ["lb0"])
    TT(lb[:, 1, :], lbe[:, 1, :], lbs[:], ALU.mult, ["lbe", "lbs"], ["lb1"])
    TS(oml[:], lb[:], -1.0, 1.0, ALU.mult, ALU.add, ["lb0", "lb1"], ["oml"])
    LBR = ["lb0", "lb1", "oml"]
    s5mag = alloc(R_CONST, [128, 2, 32])

    def s5_setup(l):
        R = Region(R_MODE.start, R_MODE.size)
        are = alloc(R, [128, 32]); aim = alloc(R, [128, 32]); ldt = alloc(R, [128, 32])
        DMA(are[:], W["s5_a_re"][l].rearrange("(r g) p -> (g p) r", g=2), [], ["s5.are"], slow=True)
        DMA(aim[:], W["s5_a_im"][l].rearrange("(r g) p -> (g p) r", g=2), [], ["s5.aim"], slow=True)
        for g2 in range(2):
            DMA(ldt[g2 * 64:(g2 + 1) * 64, :], W["s5_log_dt"][l].rearrange("(r g) -> g r", g=2)[g2].partition_broadcast(64),
                [], ["s5.ldt"], slow=True, key="s5ldt%d" % g2)
        dt = alloc(R, [128, 32]); lre = alloc(R, [128, 32]); th = alloc(R, [128, 32])
        t1 = alloc(R, [128, 32]); t2 = alloc(R, [128, 32]); cs = alloc(R, [128, 32]); sn = alloc(R, [128, 32])
        abr = alloc(R, [128, 32]); abi = alloc(R, [128, 32]); inv = alloc(R, [128, 32])
        fre = alloc(R, [128, 32]); fim = alloc(R, [128, 32]); t3 = alloc(R, [128, 32]); t4 = alloc(R, [128, 32])
        mag = s5mag[:, l, :]
        ACT(dt[:], ldt[:], AF.Exp, ["s5.ldt"], ["s5.dt"])
        TS(lre[:], are[:], -1e-4, None, ALU.min, None, ["s5.are"], ["s5.lre"])
        TT(t1[:], lre[:], dt[:], ALU.mult, ["s5.lre", "s5.dt"], ["s5.t1"])
        ACT(mag, t1[:], AF.Exp, ["s5.t1"], ["s5mag%d" % l])
        TT(th[:], aim[:], dt[:], ALU.mult, ["s5.aim", "s5.dt"], ["s5.th"])
        ki = sbt([128, 32], mybir.dt.int32, R.take(128))
        TS(t1[:], th[:], 1.0 / (2 * math.pi), None, ALU.mult, None, ["s5.th"], ["s5.t1"])
        P.op("dve", lambda e: e.tensor_copy(out=ki[:], in_=t1[:]), ["s5.t1"], ["s5.ki"])
        P.op("dve", lambda e: e.tensor_copy(out=t2[:], in_=ki[:]), ["s5.ki"], ["s5.t2"])
        STT(t1[:], t2[:], -2 * math.pi, th[:], ALU.mult, ALU.add, ["s5.t2", "s5.th"], ["s5.t1"])
        ACT(sn[:], t1[:], AF.Sin, ["s5.t1"], ["s5.sn"], scale=0.25)
        TS(t2[:], t1[:], 0.25, math.pi / 2, ALU.mult, ALU.add, ["s5.t1"], ["s5.t2"])
        ACT(cs[:], t2[:], AF.Sin, ["s5.t2"], ["s5.cs"])
        for _ in range(2):
            TT(t1[:], sn[:], cs[:], ALU.mult, ["s5.sn", "s5.cs"], ["s5.t1"])
            TT(t2[:], sn[:], sn[:], ALU.mult, ["s5.sn"], ["s5.t2"])
            TS(sn[:], t1[:], 2.0, None, ALU.mult, None, ["s5.t1"], ["s5.sn"])
            TS(cs[:], t2[:], -2.0, 1.0, ALU.mult, ALU.add, ["s5.t2"], ["s5.cs"])
        TT(abr[:], mag, cs[:], ALU.mult, ["s5mag%d" % l, "s5.cs"], ["s5.abr"])
        TT(abi[:], mag, sn[:], ALU.mult, ["s5mag%d" % l, "s5.sn"], ["s5.abi"])
        TT(t1[:], lre[:], lre[:], ALU.mult, ["s5.lre"], ["s5.t1"])
        TT(t2[:], aim[:], aim[:], ALU.mult, ["s5.aim"], ["s5.t2"])
        TT(inv[:], t1[:], t2[:], ALU.add, ["s5.t1", "s5.t2"], ["s5.inv"])
        P.op("dve", lambda e: e.reciprocal(out=inv[:], in_=inv[:]), ["s5.inv"], ["s5.inv"])
        TS(t3[:], abr[:], -1.0, None, ALU.add, None, ["s5.abr"], ["s5.t3"])
        TT(t1[:], t3[:], lre[:], ALU.mult, ["s5.t3", "s5.lre"], ["s5.t1"])
        TT(t2[:], abi[:], aim[:], ALU.mult, ["s5.abi", "s5.aim"], ["s5.t2"])
        TT(t1[:], t1[:], t2[:], ALU.add, ["s5.t1", "s5.t2"], ["s5.t1"])
        TT(fre[:], t1[:], inv[:], ALU.mult, ["s5.t1", "s5.inv"], ["s5.fre"])
        TT(t4[:], abi[:], lre[:], ALU.mult, ["s5.abi", "s5.lre"], ["s5.t4"])
        TT(t2[:], t3[:], aim[:], ALU.mult, ["s5.t3", "s5.aim"], ["s5.t2"])
        TT(t4[:], t4[:], t2[:], ALU.subtract, ["s5.t4", "s5.t2"], ["s5.t4"])
        TT(fim[:], t4[:], inv[:], ALU.mult, ["s5.t4", "s5.inv"], ["s5.fim"])
        bre = alloc(R, [128, 32, 16]); bim = alloc(R, [128, 32, 16])
        DMA(bre[:], W["s5_b_re"][l].rearrange("(r g) p c -> (g p) r c", g=2), [], ["s5.bre"])
        DMA(bim[:], W["s5_b_im"][l].rearrange("(r g) p c -> (g p) r c", g=2), [], ["s5.bim"])
        bbr = alloc(R, [128, 32, 16]); bbi = alloc(R, [128, 32, 16]); tb = alloc(R, [128, 32, 16])
        fre_b = fre[:].unsqueeze(2).broadcast_to([128, 32, 16])
        fim_b = fim[:].unsqueeze(2).broadcast_to([128, 32, 16])
        TT(bbr[:], bre[:], fre_b, ALU.mult, ["s5.bre", "s5.fre"], ["s5.bbr"])
        TT(tb[:], bim[:], fim_b, ALU.mult, ["s5.bim", "s5.fim"], ["s5.tb"])
        TT(bbr[:], bbr[:], tb[:], ALU.subtract, ["s5.bbr", "s5.tb"], ["s5.bbr"])
        TT(bbi[:], bim[:], fre_b, ALU.mult, ["s5.bim", "s5.fre"], ["s5.bbi"])
        TT(tb[:], bre[:], fim_b, ALU.mult, ["s5.bre", "s5.fim"], ["s5.tb"])
        TT(bbi[:], bbi[:], tb[:], ALU.add, ["s5.bbi", "s5.tb"], ["s5.bbi"])
        cm = CS["s5cm"]
        Z = [alloc(R, [128, 8, 16]) for _ in range(2)]
        wst = [alloc(R, [128, 2, 512]) for _ in range(2)]
        cn = alloc(R, [128, 2, 64]); ct = alloc(R, [128, 128])
        zi = 0
        for kc in range(8):
            ws = wst[kc % 2]
            wsn = "s5.wst%d" % (kc % 2)
            for part, src in ((0, bbr), (1, bbi)):
                bk = (2 * kc + part) % 8
                for j in range(4):
                    r = 4 * kc + j
                    z = Z[zi % 2]; zn = "s5.Z%d" % (zi % 2); zi += 1
                    TT(z[:], cm[:, j, :].rearrange("p (g c) -> p g c", c=16),
                       src[:, r, :].unsqueeze(1).broadcast_to([128, 8, 16]), ALU.mult,
                       ["c_s5cm", "s5.bbr", "s5.bbi"], [zn])
                    TR(banks[bk][:, j * 128:(j + 1) * 128], z[:].rearrange("p g c -> p (g c)"), ident_f[:],
                       [zn, "c_ident"], [PB[bk]])
                COPY(ws[:, part, :], banks[bk][:], [PB[bk]], [wsn])
            DMA(s5wb[l, kc], ws[:], [wsn], ["d_s5wb%d" % l], key="s5wbst")
        wcs = [alloc(R, [128, 2, 512]) for _ in range(2)]
        for kc in range(8):
            ws = wcs[kc % 2]
            wsn = "s5.wcs%d" % (kc % 2)
            for part, nm in ((0, "s5_c_re"), (1, "s5_c_im")):
                bk = (2 * kc + part) % 8
                src = W[nm][l, 8 * kc:8 * kc + 8].rearrange("g c p -> (g c) p")
                for dup in range(2):
                    DMA(cn[:, dup, :], src, [], ["s5.cn"], key="s5cn%d" % dup)
                TR(banks[bk][:, 0:128], cn[:].rearrange("p a b -> p (a b)"), ident_f[:], ["s5.cn", "c_ident"], [PB[bk]])
                COPY(ct[:], banks[bk][:, 0:128], [PB[bk]], ["s5.ct"])
                for j in range(4):
                    if part == 0:
                        TT(ws[:, part, j * 128:(j + 1) * 128], ct[:], cm[:, j, :], ALU.mult, ["s5.ct", "c_s5cm"], [wsn])
                    else:
                        STT(ws[:, part, j * 128:(j + 1) * 128], ct[:], -1.0, cm[:, j, :], ALU.mult, ALU.mult,
                            ["s5.ct", "c_s5cm"], [wsn])
            DMA(s5wc[l, kc], ws[:], [wsn], ["d_s5wc%d" % l], key="s5wcst")
        tabs = [alloc(R, [128, 2, 4, 512]) for _ in range(2)]
        tq = [alloc(R, [128, 4, 256]) for _ in range(2)]
        for kc in range(8):
            tab = tabs[kc % 2]
            tn = "s5.tab%d" % (kc % 2)
            rs = slice(4 * kc, 4 * kc + 4)
            P.op("dve", lambda e, tab=tab, rs=rs: e.tensor_copy(out=tab[:, 0, :, 0:1], in_=cs[:, rs].unsqueeze(2)), ["s5.cs"], [tn])
            P.op("dve", lambda e, tab=tab, rs=rs: e.tensor_copy(out=tab[:, 1, :, 0:1], in_=sn[:, rs].unsqueeze(2)), ["s5.sn"], [tn])
            m = 1
            while m < 512:
                cmv = tab[:, 0, :, m - 1:m].broadcast_to([128, 4, m])
                smv = tab[:, 1, :, m - 1:m].broadcast_to([128, 4, m])
                c0 = tab[:, 0, :, 0:m]; s0 = tab[:, 1, :, 0:m]
                q0 = tq[0][:, :, 0:m]; q1 = tq[1][:, :, 0:m]
                TT(q0, c0, cmv, ALU.mult, [tn], ["s5.tq0"])
                TT(q1, s0, smv, ALU.mult, [tn], ["s5.tq1"])
                TT(tab[:, 0, :, m:2 * m], q0, q1, ALU.subtract, ["s5.tq0", "s5.tq1", tn], [tn])
                TT(q0, s0, cmv, ALU.mult, [tn], ["s5.tq0"])
                TT(q1, c0, smv, ALU.mult, [tn], ["s5.tq1"])
                TT(tab[:, 1, :, m:2 * m], q0, q1, ALU.add, ["s5.tq0", "s5.tq1", tn], [tn])
                m *= 2
            DMA(s5tab[l, kc], tab[:], [tn], ["d_s5tab%d" % l], key="s5tabst")

    for l in range(NL):
        s5_setup(l)
    P.barrier()

    class Mode:
        pass

    def make_mode(m):
        M = Mode()
        M.m = m
        M.T = 512 if m == "p" else 128
        M.NB = M.T // 128
        R = Region(R_MODE.start, R_MODE.size)
        M.R = R
        T = M.T
        if m == "p":
            M.HGS = alloc(R, [128, 2, 8, 128])
            M.MLC = alloc(R, [128, 2, 4, 2, 256])
            M.MLN = alloc(R, [128, 2, 4, 2])
            M.MST = alloc(R, [1, 2, 4])
            M.XSr = alloc(R, [128, 2, 32])
            M.XSi = alloc(R, [128, 2, 32])
            M.CV = alloc(R, [128, 2, 3, 8])
        M.RSTD = alloc(R, [128, T])
        M.SQ = [alloc(R, [128, T], BF16) for _ in range(2)]
        M.PNT = alloc(R, [128, T])
        M.X = alloc(R, [128, 16, T])
        M.H = alloc(R, [128, 16, T], BF16)
        M.FF = alloc(R, [128, 16, T])
        M.S = Region(R.cur, R.start + R.size - R.cur)
        return M

    def xn(c):
        return "X.%d" % c

    def hn(c):
        return "H.%d" % c

    def fn_(c):
        return "FF.%d" % c

    XALL = [xn(c) for c in range(16)]
    HALL = [hn(c) for c in range(16)]
    FALL = [fn_(c) for c in range(16)]

    def rms_stats(M, src, srcnames, bank=0):
        T = M.T
        for c in range(16):
            s = M.SQ[c % 2]; sn_ = "sq%d" % (c % 2)
            ACT(s[:], src(c), AF.Square, [srcnames[c]], [sn_])
            MM(banks[bank][:, 0:T], [(ones_b[:], s[:])], [sn_, "ones_b"], [PB[bank]], start=(c == 0), stop=(c == 15))
        ACT(M.RSTD[:], banks[bank][:, 0:T], AF.Ln, [PB[bank]], ["rstd"], scale=1.0 / D, bias=EPS)
        ACT(M.RSTD[:], M.RSTD[:], AF.Exp, ["rstd"], ["rstd"], scale=-0.5)

    def prenorm(M, l, k, barrier=False):
        rms_stats(M, lambda c: M.X[:, c, :], XALL)
        for c in range(16):
            STT(M.H[:, c, :], M.X[:, c, :], gains[:, l, k, c:c + 1], M.RSTD[:], ALU.mult, ALU.mult,
                [xn(c), "gains", "rstd"], [hn(c)])
        if barrier:
            P.barrier()

    def postnorm_residual(M, l, k, half):
        rms_stats(M, lambda c: M.FF[:, c, :], FALL)
        g = gainsh if half else gains
        gname = "gainsh" if half else "gains"
        for c in range(16):
            STT(M.PNT[:], M.FF[:, c, :], g[:, l, k, c:c + 1], M.RSTD[:], ALU.mult, ALU.mult, [fn_(c), gname, "rstd"], ["pn.t"])
            TT(M.X[:, c, :], M.X[:, c, :], M.PNT[:], ALU.add, [xn(c), "pn.t"], [xn(c)])

    def ffn(M, l, wup, wdown):
        T = M.T
        S = M.S; S.reset()
        HID = alloc(S, [128, 22, T], BF16)
        sa = [alloc(S, [128, T], BF16) for _ in range(2)]
        si = 0
        if M.m == "p":
            for i in range(3):
                extra_slots.append((S.take(8192), "ringx%d" % i))
        for half in range(2):
            for jj in range(11):
                j = half * 11 + jj
                ta, ba = wload(wup[l, :, 256 * j:256 * j + 256].rearrange("(kc p) n -> p kc n", p=128), [128, 16, 256])
                tb, bb = wload(wup[l, :, FFD + 256 * j:FFD + 256 * j + 256].rearrange("(kc p) n -> p kc n", p=128), [128, 16, 256])
                bs = 4 * (j % 2)
                for ch in range(2):
                    MM(banks[bs + ch][:, 0:T], [(ta[:, kc, ch * 128:(ch + 1) * 128], M.H[:, kc, :]) for kc in range(16)],
                       [ba] + HALL, [PB[bs + ch]])
                for ch in range(2):
                    MM(banks[bs + 2 + ch][:, 0:T], [(tb[:, kc, ch * 128:(ch + 1) * 128], M.H[:, kc, :]) for kc in range(16)],
                       [bb] + HALL, [PB[bs + 2 + ch]])
                for ch in range(2):
                    fl = 2 * jj + ch
                    s = sa[si % 2]; sn_ = "ffn.sa%d" % (si % 2); si += 1
                    ACT(s[:], banks[bs + ch][:, 0:T], AF.Silu, [PB[bs + ch]], [sn_])
                    TT(HID[:, fl, :], s[:], banks[bs + 2 + ch][:, 0:T], ALU.mult, [sn_, PB[bs + 2 + ch]], ["HID.%d" % fl])
            for dg in range(4):
                bs = 4 * (dg % 2)
                first = True
                for q in range(6):
                    nf = min(4, 22 - 4 * q)
                    f0 = half * 22 + 4 * q
                    tw, bw = wload(wdown[l, f0 * 128:(f0 + nf) * 128, dg * 512:(dg + 1) * 512].rearrange("(fc p) n -> p fc n", p=128),
                                   [128, nf, 512])
                    for dch in range(4):
                        pairs = [(tw[:, fc, dch * 128:(dch + 1) * 128], HID[:, 4 * q + fc, :]) for fc in range(nf)]
                        MM(banks[bs + dch][:, 0:T], pairs, [bw] + ["HID.%d" % (4 * q + fc) for fc in range(nf)], [PB[bs + dch]],
                           start=first, stop=(q == 5))
                    first = False
                for dch in range(4):
                    c = dg * 4 + dch
                    if half == 0:
                        COPY(M.FF[:, c, :], banks[bs + dch][:, 0:T], [PB[bs + dch]], [fn_(c)])
                    else:
                        TT(M.FF[:, c, :], M.FF[:, c, :], banks[bs + dch][:, 0:T], ALU.add, [fn_(c), PB[bs + dch]], [fn_(c)])
        del extra_slots[:]
        P.barrier()

    def proj_fm(M, l, col0, ntile, consume, tok=None, bank0=0, nbank=8):
        t0, t1 = (0, M.T) if tok is None else tok
        n = t1 - t0
        bi = 0
        for tl in range(ntile):
            tw, bw = wload(W["w_in"][l, :, col0 + 256 * tl:col0 + 256 * tl + 256].rearrange("(kc p) n -> p kc n", p=128),
                           [128, 16, 256])
            for ch in range(2):
                bk = bank0 + (bi % nbank); bi += 1
                MM(banks[bk][:, 0:n], [(tw[:, kc, ch * 128:(ch + 1) * 128], M.H[:, kc, t0:t1]) for kc in range(16)],
                   [bw] + HALL, [PB[bk]])
                consume(2 * tl + ch, banks[bk][:, 0:n], PB[bk])

    def proj_tm(M, l, col0, ntile, blks, consume, bank0=0, nbank=8):
        bi = 0
        for tl in range(ntile):
            tw, bw = wload(W["w_in"][l, :, col0 + 256 * tl:col0 + 256 * tl + 256].rearrange("(kc p) n -> p kc n", p=128),
                           [128, 16, 256])
            for blk in blks:
                bk = bank0 + (bi % nbank); bi += 1
                MM(banks[bk][:, 0:256], [(M.H[:, kc, blk * 128:(blk + 1) * 128], tw[:, kc, :]) for kc in range(16)],
                   [bw] + HALL, [PB[bk]])
                consume(tl, blk, banks[bk][:, 0:256], PB[bk])

    def hgrn(M, l, OG, st):
        T = M.T; NB = M.NB; m = M.m
        clen = 32 if m == "p" else 8
        nslot_blk = 128 // clen
        nch = T // clen
        S = M.S
        S.reset()
        mk = S.mark()
        cst = CS["cst_" + m]; crow = CS["crow_" + m]
        for hgp in range(2):
            S.reset(mk)
            QT = [alloc(S, [128, T]) for _ in range(4)]
            KT = [alloc(S, [128, T]) for _ in range(4)]
            KH = [alloc(S, [128, NB, 128], BF16) for _ in range(4)]
            SHG = [alloc(S, [128, T], BF16) for _ in range(4)]
            EBE = [alloc(S, [128, nch]) for _ in range(4)]
            VT = alloc(S, [128, NB, 512], BF16)
            VM = [alloc(S, [128, 512], BF16) for _ in range(2)]
            XR = Region(M.FF_off + 8 * T * 2, 24 * T * 2) if m == "p" else S
            TMP = [[alloc(S, [128, T]) for _ in range(4)] for _ in range(2)] + [[alloc(XR, [128, T]) for _ in range(4)] for _ in range(2)]
            AT = [[alloc(S, [128, 128], BF16) for _ in range(2)] for _ in range(2)] + [[alloc(XR, [128, 128], BF16) for _ in range(2)] for _ in range(2)]
            BB = [[alloc(S, [128, T], BF16) for _ in range(2)] for _ in range(2)] + [[alloc(XR, [128, T], BF16) for _ in range(2)] for _ in range(2)]

            def cons_v(tl, blk, bank, bname):
                COPY(VT[:, blk, tl * 256:(tl + 1) * 256], bank, [bname], ["hg.VT%d.%d" % (blk, tl)])
            proj_tm(M, l, 2048 + hgp * 512, 2, range(NB), cons_v, bank0=0, nbank=4)
            VTN = lambda blk: ["hg.VT%d.0" % blk, "hg.VT%d.1" % blk]

            def fchain(hh, hd, bank, bname, ci):
                TA, TB, TC, TD = TMP[ci]
                nA, nB, nC, nD = ["hg.T%s%d" % (x, ci) for x in "ABCD"]
                ACT(TA[:], bank, AF.Sigmoid, [bname], [nA]); yield
                TS(TA[:], TA[:], oml[:, l, hd:hd + 1], lb[:, l, hd:hd + 1], ALU.mult, ALU.add, [nA] + LBR, [nA]); yield
                TS(TB[:], TA[:], -1.0, 1.0, ALU.mult, ALU.add, [nA], [nB]); yield
                ACT(TA[:], TA[:], AF.Ln, [nA], [nA]); yield
                P.op("dve", lambda e: e.tensor_tensor_scan(out=TC[:], data0=cst[:, 0:T], data1=TA[:], initial=0.0,
                                                            op0=ALU.mult, op1=ALU.add), [nA, "c_cst_" + m], [nC]); yield
                ACT(TD[:], TC[:], AF.Exp, [nC], [nD]); yield
                ACT(TA[:], TC[:], AF.Exp, [nC], [nA], scale=-1.0); yield
                TT(KT[hh][:], TB[:], TA[:], ALU.mult, [nB, nA], ["hg.KT%d" % hh]); yield
                ebv = TD[:].rearrange("p (c j) -> p c j", j=clen)[:, :, clen - 1:clen]
                P.op("dve", lambda e: e.tensor_copy(out=EBE[hh][:].unsqueeze(2), in_=ebv), [nD], ["hg.EBE%d" % hh]); yield
                TT(TB[:].rearrange("p (c j) -> p c j", j=clen), KT[hh][:].rearrange("p (c j) -> p c j", j=clen),
                   ebv.broadcast_to([128, nch, clen]), ALU.mult, ["hg.KT%d" % hh, nD], [nB]); yield
                bk = 4 + ci
                for blk in range(NB):
                    TR(banks[bk][:, blk * 128:(blk + 1) * 128], TB[:, blk * 128:(blk + 1) * 128], ident_f[:],
                       [nB, "c_ident"], [PB[bk]])
                yield
                COPY(KH[hh][:].rearrange("p b k -> p (b k)"), banks[bk][:, 0:T], [PB[bk]], ["hg.KH%d" % hh]); yield
                COPY(QT[hh][:], TD[:], [nD], ["hg.QT%d" % hh], eng="dve"); yield

            for grp, cbase in (("f", 1024), ("q", 0), ("g", 3072)):
                pend = []
                for pr in range(2):
                    def cons(ci, bank, bname, grp=grp, pr=pr, pend=pend):
                        hh = 2 * pr + ci
                        hd = 4 * hgp + hh
                        if grp == "f":
                            pend.append(fchain(hh, hd, bank, bname, hh))
                        elif grp == "q":
                            TC = TMP[hh][2]; nC = "hg.TC%d" % hh
                            ACT(TC[:], bank, AF.Silu, [bname], [nC])
                            TT(QT[hh][:], QT[hh][:], TC[:], ALU.mult, ["hg.QT%d" % hh, nC], ["hg.QT%d" % hh])
                        else:
                            ACT(SHG[hh][:], bank, AF.Silu, [bname], ["hg.SHG%d" % hh])
                    proj_fm(M, l, cbase + hgp * 512 + pr * 256, 1, cons, bank0=2 * pr, nbank=2)
                interleave(pend)
            OB = [0, 1, 2, 3]

            def intra(hh, ci):
                KTb, QTb = BB[ci]; nk = "hg.KTb%d" % ci; nq = "hg.QTb%d" % ci
                COPY(KTb[:], KT[hh][:], ["hg.KT%d" % hh], [nk], eng="dve"); yield
                COPY(QTb[:], QT[hh][:], ["hg.QT%d" % hh], [nq], eng="dve"); yield
                sb_ = 4 + ci
                for blk in range(NB):
                    sl = slice(blk * 128, (blk + 1) * 128)
                    MM(banks[sb_][:, 0:128], [(KTb[:, sl], QTb[:, sl])], [nk, nq], [PB[sb_]])
                    at = AT[ci][blk % 2]; an = "hg.AT%d%d" % (ci, blk % 2)
                    TT(at[:], banks[sb_][:, 0:128], hgm_b[m][:], ALU.mult, [PB[sb_], "hgmb_" + m], [an])
                    MM(banks[OB[hh]][:, sl], [(VT[:, blk, hh * 128:(hh + 1) * 128], at[:])], [an] + VTN(blk), [PB[OB[hh]]],
                       start=(blk == 0), stop=False, skip=True)
                    yield
            interleave([intra(hh, hh) for hh in range(4)])
            vi = 0
            for blk in range(NB):
                for ci in range(nslot_blk):
                    slot = blk * nslot_blk + ci
                    t0 = blk * 128 + ci * clen
                    vm = VM[vi % 2]; vn = "hg.VM%d" % (vi % 2); vi += 1
                    TS(vm[:], VT[:, blk, :], crow[:, ci:ci + 1], None, ALU.mult, None, VTN(blk) + ["c_crow_" + m], [vn])
                    sin, sin_names, sout, sout_names, after = st(hgp, slot)
                    ub = 4 + (slot % 2)
                    for hh in range(4):
                        MM(banks[OB[hh]][:, t0:t0 + clen], [(sin[hh], QT[hh][:, t0:t0 + clen])],
                           [sin_names[hh], "hg.QT%d" % hh], [PB[OB[hh]]], start=False, stop=False, skip=True)
                        MM(banks[ub][:, hh * 128:(hh + 1) * 128], [(KH[hh][:, blk, :], vm[:, hh * 128:(hh + 1) * 128])],
                           ["hg.KH%d" % hh, vn], [PB[ub]])
                        STT(sout[hh], sin[hh], EBE[hh][:, slot:slot + 1], banks[ub][:, hh * 128:(hh + 1) * 128], ALU.mult, ALU.add,
                            [sin_names[hh], "hg.EBE%d" % hh, PB[ub]], [sout_names[hh]])
                    if after is not None:
                        after()

            def post(hh, ci):
                hd = 4 * hgp + hh
                TA, TB, TC, TD = TMP[ci]
                nA, nB, nC, nD = ["hg.T%s%d" % (x, ci) for x in "ABCD"]
                SQ = BB[ci][0]; nsq_ = "hg.KTb%d" % ci
                bk = 4 + ci
                COPY(TA[:], banks[OB[hh]][:, 0:T], [PB[OB[hh]]], [nA]); yield
                ACT(SQ[:], TA[:], AF.Square, [nA], [nsq_]); yield
                MM(banks[bk][:, 0:T], [(ones_b[:], SQ[:])], [nsq_, "ones_b"], [PB[bk]]); yield
                ACT(TC[:], banks[bk][:, 0:T], AF.Ln, [PB[bk]], [nC], scale=1.0 / 128, bias=EPS); yield
                ACT(TC[:], TC[:], AF.Exp, [nC], [nC], scale=-0.5); yield
                STT(TA[:], TA[:], hnorm[:, l, hd:hd + 1], TC[:], ALU.mult, ALU.mult, [nA, "hnorm", nC], [nA]); yield
                TT(OG[:, hd, :], TA[:], SHG[hh][:], ALU.mult, [nA, "hg.SHG%d" % hh], ["OG.%d" % hd]); yield
            interleave([post(hh, hh) for hh in range(4)])
        P.barrier()
        S.reset(mk)

    def s5(M, l, YB, XSr, XSi, XNr, XNi, xs_name, xn_name):
        T = M.T; m = M.m
        S = M.S
        S.reset()
        mk = S.mark()
        Tt = 512 if m == "p" else 8
        TAB = alloc(S, [128, 2, 4, Tt])
        U32 = [alloc(S, [128, T]) for _ in range(2)]
        BU = [[alloc(S, [128, T]) for _ in range(2)] for _ in range(2)]
        BT = [[alloc(S, [128, T]) for _ in range(2)] for _ in range(2)]
        QQ = [[alloc(S, [128, T]) for _ in range(2)] for _ in range(2)]
        D0 = [alloc(S, [128, T]) for _ in range(2)]
        YT = alloc(S, [128, T]); Y2 = alloc(S, [128, T])
        sst = CS["s5st_" + m]
        nsq = T // Tt if m == "s" else 1

        def tv(ap):
            if m == "p":
                return ap
            return ap.rearrange("p (b j) -> p b j", j=8)

        def tabv(part, j):
            if m == "p":
                return TAB[:, part, j, :]
            return TAB[:, part, j, :].unsqueeze(1).broadcast_to([128, 16, 8])

        def first(ap):
            if m == "p":
                return ap[:, 0:1]
            return ap.rearrange("p (b j) -> p b j", j=8)[:, :, 0]

        def lastc(ap):
            if m == "p":
                return ap[:, T - 1:T]
            return ap.rearrange("p (b j) -> p b j", j=8)[:, :, 7]

        for kc in range(8):
            if kc % 2 == 0:
                def cons_u(ci, bank, bname, kc=kc):
                    COPY(U32[ci][:], bank, [bname], ["s5.U%d" % ci])
                proj_fm(M, l, 4096 + 256 * (kc // 2), 1, cons_u, bank0=4, nbank=2)
            u = U32[kc % 2]; un = "s5.U%d" % (kc % 2)
            wb, wbn = wload(s5wb[l, kc], [128, 2, 512], F32, q="sp", reads=["d_s5wb%d" % l])
            wc, wcn = wload(s5wc[l, kc], [128, 2, 512], F32, q="sp", reads=["d_s5wc%d" % l])
            if m == "p":
                DMA(TAB[:], s5tab[l, kc], ["d_s5tab%d" % l], ["s5.TAB"])
            else:
                DMA(TAB[:], s5tab[l, kc][:, :, :, 0:8], ["d_s5tab%d" % l], ["s5.TAB"])
            yb = 6 + (kc % 2)
            def row(j, kc=kc, u=u, un=un, wb=wb, wbn=wbn, wc=wc, wcn=wcn, yb=yb):
                r = 4 * kc + j
                pj = j % 2
                bur, bui = BU[pj]; btr, bti = BT[pj]
                q0, q1 = QQ[pj]; nq0 = "s5.Q0%d" % pj; nq1 = "s5.Q1%d" % pj
                nbu = ["s5.BU%d%d" % (pj, i) for i in range(2)]
                nbt = ["s5.BT%d%d" % (pj, i) for i in range(2)]
                d0 = D0[pj]; nd0 = "s5.D0%d" % pj
                bkr = 2 * pj; bki = 2 * pj + 1
                pr_ = banks[bkr][:, 0:T]; pi_ = banks[bki][:, 0:T]
                for part in range(2):
                    bk = 2 * pj + part
                    MM(banks[bk][:, 0:T], [(wb[:, part, j * 128:(j + 1) * 128], u[:])], [wbn, un], [PB[bk]])
                    COPY(BU[pj][part][:], banks[bk][:, 0:T], [PB[bk]], [nbu[part]])
                    yield
                ct = tabv(0, j); sn_ = tabv(1, j)
                TT(tv(q0[:]), tv(bur[:]), ct, ALU.mult, [nbu[0], "s5.TAB"], [nq0]); yield
                TT(tv(q1[:]), tv(bui[:]), sn_, ALU.mult, [nbu[1], "s5.TAB"], [nq1]); yield
                TT(btr[:], q0[:], q1[:], ALU.add, [nq0, nq1], [nbt[0]]); yield
                TT(tv(q0[:]), tv(bui[:]), ct, ALU.mult, [nbu[1], "s5.TAB"], [nq0]); yield
                TT(tv(q1[:]), tv(bur[:]), sn_, ALU.mult, [nbu[0], "s5.TAB"], [nq1]); yield
                TT(bti[:], q0[:], q1[:], ALU.subtract, [nq0, nq1], [nbt[1]]); yield
                magc = s5mag[:, l, r:r + 1]
                STT(first(btr[:]), XSr(r), magc, first(btr[:]), ALU.mult, ALU.add, [xs_name, "s5mag%d" % l, nbt[0]], [nbt[0]]); yield
                STT(first(bti[:]), XSi(r), magc, first(bti[:]), ALU.mult, ALU.add, [xs_name, "s5mag%d" % l, nbt[1]], [nbt[1]]); yield
                ACT(d0[:], sst[:, 0:T], AF.Copy, ["c_s5st_" + m, "s5mag%d" % l], [nd0], scale=magc); yield
                for part in range(2):
                    P.op("dve", lambda e, o=BU[pj][part], d1=BT[pj][part], d0=d0: e.tensor_tensor_scan(
                        out=o[:], data0=d0[:], data1=d1[:], initial=0.0, op0=ALU.mult, op1=ALU.add),
                        [nd0, nbt[part]], [nbu[part]])
                    yield
                TT(tv(q0[:]), tv(bur[:]), ct, ALU.mult, [nbu[0], "s5.TAB"], [nq0]); yield
                TT(tv(q1[:]), tv(bui[:]), sn_, ALU.mult, [nbu[1], "s5.TAB"], [nq1]); yield
                TT(btr[:], q0[:], q1[:], ALU.subtract, [nq0, nq1], [nbt[0]]); yield
                TT(tv(q0[:]), tv(bui[:]), ct, ALU.mult, [nbu[1], "s5.TAB"], [nq0]); yield
                TT(tv(q1[:]), tv(bur[:]), sn_, ALU.mult, [nbu[0], "s5.TAB"], [nq1]); yield
                TT(bti[:], q0[:], q1[:], ALU.add, [nq0, nq1], [nbt[1]]); yield
                COPY(XNr(r), lastc(btr[:]), [nbt[0]], [xn_name], eng="dve"); yield
                COPY(XNi(r), lastc(bti[:]), [nbt[1]], [xn_name], eng="dve"); yield
                MM(banks[yb][:, 0:T], [(wc[:, 0, j * 128:(j + 1) * 128], btr[:]), (wc[:, 1, j * 128:(j + 1) * 128], bti[:])],
                   [wcn, nbt[0], nbt[1]], [PB[yb]], start=(j == 0), stop=(j == 3))
                yield
            for jp in (0, 2):
                interleave([row(jp), row(jp + 1)])
            STT(YT[:], u[:], s5d[:, l, kc:kc + 1], banks[yb][:, 0:T], ALU.mult, ALU.add, [un, "s5d", PB[yb]], ["s5.YT"])
            ACT(Y2[:], YT[:], AF.Square, ["s5.YT"], ["s5.Y2"])
            TS(Y2[:], Y2[:], 0.044715, 1.0, ALU.mult, ALU.add, ["s5.Y2"], ["s5.Y2"])
            TT(Y2[:], Y2[:], YT[:], ALU.mult, ["s5.Y2", "s5.YT"], ["s5.Y2"])
            ACT(Y2[:], Y2[:], AF.Sigmoid, ["s5.Y2"], ["s5.Y2"], scale=1.5957691216057308)
            TT(YB[:, kc, :], YT[:], Y2[:], ALU.mult, ["s5.YT", "s5.Y2"], ["YB.%d" % kc])
        P.barrier()
        S.reset(mk)

    def mlstm(M, l, HM, SS):
        T = M.T; m = M.m
        TT_ = 256 if m == "p" else 128
        NBK = TT_ // 128
        nseq = 1 if m == "p" else 16
        Ls = TT_ if m == "p" else 8
        nslot = 1 if m == "p" else 16
        S = M.S
        S.reset()
        mk0 = S.mark()
        neg = CS["neg_" + m]; tri = CS["tri_" + m]; G = CS["G_" + m]; E = CS["E_" + m]
        last = CS["last_" + m]; smask = CS["smask_" + m]
        CN = ["c_neg_" + m, "c_tri_" + m, "c_G_" + m, "c_E_" + m, "c_last_" + m, "c_smask_" + m]
        for th in range(T // TT_):
            S.reset(mk0)
            tk0 = th * TT_
            QT = alloc(S, [128, 8, TT_], BF16); KT = alloc(S, [128, 8, TT_], BF16)
            KTM = alloc(S, [128, NBK, 1024], BF16); VTM = alloc(S, [128, NBK, 1024], BF16)
            SMO = alloc(S, [128, NBK, 1024], BF16)
            GT = alloc(S, [128, NBK, 8]); LF = alloc(S, [128, NBK, 4]); LS_ = alloc(S, [128, NBK, 4])
            mk1 = S.mark()
            XC = alloc(S, [128, 8, TT_], BF16); MXB = alloc(S, [128, 8, TT_], BF16); VTt = alloc(S, [128, 8, TT_], BF16)
            MXE = [alloc(S, [128, nseq, 3 + Ls]) for _ in range(2)]
            XCt = alloc(S, [128, nseq, Ls])
            def cons_mx(ci_, bank, bname):
                fc = cons_mx.fc0 + ci_
                e = MXE[fc % 2]; en = "ml.MXE%d" % (fc % 2)
                COPY(e[:, :, 3:3 + Ls], bank.rearrange("p (b j) -> p b j", j=Ls), [bname], [en])
                COPY(e[:, :, 0:3], SS["cv_in"](fc), [SS["cv_in_name"]], [en], eng="dve")
                COPY(MXB[:, fc, :], bank, [bname], ["ml.MXB%d" % fc])
                TS(XCt[:], e[:, :, 3:3 + Ls], convw[:, l, 3, fc:fc + 1], convb[:, l, fc:fc + 1], ALU.mult, ALU.add,
                   [en, "convw", "convb"], ["ml.XCt"])
                for jj in (2, 1, 0):
                    STT(XCt[:], e[:, :, jj:jj + Ls], convw[:, l, jj, fc:fc + 1], XCt[:], ALU.mult, ALU.add,
                        [en, "convw", "ml.XCt"], ["ml.XCt"])
                ACT(XC[:, fc, :], XCt[:].rearrange("p b j -> p (b j)"), AF.Silu, ["ml.XCt"], ["ml.XC%d" % fc])
                COPY(SS["cv_out"](fc), e[:, :, Ls:Ls + 3], [en], [SS["cv_out_name"]], eng="dve")
            for tl in range(4):
                cons_mx.fc0 = 2 * tl
                proj_fm(M, l, 5120 + 256 * tl, 1, cons_mx, tok=(tk0, tk0 + TT_), bank0=0, nbank=4)
            XCN = ["ml.XC%d" % i for i in range(8)]; MXN = ["ml.MXB%d" % i for i in range(8)]
            wq, wqn = wload(W["mlstm_wq"][l].rearrange("h (dc p) e -> p h dc e", p=128), [128, 4, 2, 256])
            wk, wkn = wload(W["mlstm_wk"][l].rearrange("h (dc p) e -> p h dc e", p=128), [128, 4, 2, 256])
            wv, wvn = wload(W["mlstm_wv"][l].rearrange("h (dc p) e -> p h dc e", p=128), [128, 4, 2, 256])
            bi = 0
            for (wt, wn, src, srcn, dst, dn) in ((wq, wqn, XC, XCN, QT, "ml.QT"), (wk, wkn, XC, XCN, KT, "ml.KT"),
                                                 (wv, wvn, MXB, MXN, VTt, "ml.VTt")):
                for h in range(4):
                    for ec in range(2):
                        bk = bi % 4; bi += 1
                        MM(banks[bk][:, 0:TT_], [(wt[:, h, dc, ec * 128:(ec + 1) * 128], src[:, 2 * h + dc, :]) for dc in range(2)],
                           [wn, srcn[2 * h], srcn[2 * h + 1]], [PB[bk]])
                        COPY(dst[:, 2 * h + ec, :], banks[bk][:, 0:TT_], [PB[bk]], ["%s%d" % (dn, 2 * h + ec)])
            QTN = ["ml.QT%d" % i for i in range(8)]; KTN = ["ml.KT%d" % i for i in range(8)]; VTN_ = ["ml.VTt%d" % i for i in range(8)]
            for (wt, wn, src, srcn, dst, dn) in ((wk, wkn, XC, XCN, KTM, "ml.KTM"), (wv, wvn, MXB, MXN, VTM, "ml.VTM")):
                for blk in range(NBK):
                    for h in range(4):
                        bk = 4 + (bi % 4); bi += 1
                        MM(banks[bk][:, 0:256],
                           [(src[:, 2 * h + dc, blk * 128:(blk + 1) * 128], wt[:, h, dc, :]) for dc in range(2)],
                           [wn, srcn[2 * h], srcn[2 * h + 1]], [PB[bk]])
                        COPY(dst[:, blk, h * 256:(h + 1) * 256], banks[bk][:, 0:256], [PB[bk]], ["%s%d.%d" % (dn, blk, h)])
            for blk in range(NBK):
                srcs = [(QT, QTN), (KT, KTN), (VTt, VTN_)]
                pairs = []; rd = ["wgate"]
                for gi, (src, srcn) in enumerate(srcs):
                    for c in range(8):
                        pairs.append((src[:, c, blk * 128:(blk + 1) * 128], wgate[:, l, gi * 8 + c, :]))
                        rd.append(srcn[c])
                MM(banks[blk % 2][:, 0:8], pairs, rd, [PB[blk % 2]])
                TT(GT[:, blk, :], banks[blk % 2][:, 0:8], bgate[:, l, :], ALU.add, [PB[blk % 2], "bgate"], ["ml.GT"])
            ACT(LS_[:], GT[:, :, 4:8], AF.Sigmoid, ["ml.GT"], ["ml.LS"])
            ACT(LF[:], LS_[:], AF.Ln, ["ml.LS"], ["ml.LF"])
            def cons_mo(tl, blk, bank, bname):
                b_ = blk - (tk0 // 128)
                ACT(SMO[:, b_, tl * 256:(tl + 1) * 256], bank, AF.Sigmoid, [bname], ["ml.SMO%d.%d" % (b_, tl)])
            proj_tm(M, l, 6144, 4, range(tk0 // 128, tk0 // 128 + NBK), cons_mo, bank0=2, nbank=4)
            P.barrier()
            S.reset(mk1)
            sm = lambda n: alloc(S, [128, n])
            BMT = sm(8); MP = sm(4); BM = sm(4); VEC = sm(4); RMAX = sm(4); NMT = sm(4); WPV = sm(4)
            ENM = sm(4); DSUM = sm(4); DEN = sm(4); RDEN = sm(4); SSQ = sm(4); RSTD = sm(4); SCL = sm(4)
            BL = sm(8); D2 = sm(4); D3 = sm(4); WIN = sm(4); GP = sm(4); QN = sm(4)
            RR = alloc(S, [128, nslot, 4]); GPB = alloc(S, [128, nslot, 4]); WINM = alloc(S, [128, nslot, 4])
            D1 = [alloc(S, [128, 128]) for _ in range(2)]
            L_ = alloc(S, [128, 4, 128]); Wt = alloc(S, [128, 4, 128])
            Sb = alloc(S, [128, 4, 128], BF16); ST_ = alloc(S, [128, 4, 128], BF16)
            IA = alloc(S, [128, 4, 256]); IT = alloc(S, [128, 2, 4, 128]); ITn = alloc(S, [1, 4, 128])
            NUM = alloc(S, [128, 4, 256]); HG = alloc(S, [128, 4, 256], BF16); JK = alloc(S, [128, 256])
            CB = [alloc(S, [128, 4, 2, 256], BF16) for _ in range(1 if m == "p" else 2)]
            NBb = [alloc(S, [128, 4, 2], BF16) for _ in range(2)]
            KW = [alloc(S, [128, 256], BF16) for _ in range(2)]
            MT = BMT[:, 4:8]
            Bc = BMT[:, 0:4]
            for blk in range(NBK):
                gb = tk0 // 128 + blk
                tsl = slice(blk * 128, (blk + 1) * 128)
                IG = GT[:, blk, 0:4]
                MM(banks[0][:, 0:4], [(tri[:], LF[:, blk, :])], ["ml.LF"] + CN, [PB[0]])
                COPY(Bc, banks[0][:, 0:4], [PB[0]], ["ml.Bc"], eng="dve")
                mst, mstn = SS["m_in"]()
                MM(banks[0][:, 8:12], [(E[:], mst)], [mstn] + CN, [PB[0]])
                COPY(MP[:], banks[0][:, 8:12], [PB[0]], ["ml.MP"], eng="dve")
                TT(BM[:], Bc, MP[:], ALU.add, ["ml.Bc", "ml.MP"], ["ml.BM"])
                TT(VEC[:], IG, Bc, ALU.subtract, ["ml.GT", "ml.Bc"], ["ml.VEC"])
                for h in range(4):
                    d1 = D1[h % 2]; dn_ = "ml.D1%d" % (h % 2)
                    TS(d1[:], ident_f[:], VEC[:, h:h + 1], None, ALU.mult, None, ["c_ident", "ml.VEC"], [dn_])
                    MM(banks[1][:, h * 128:(h + 1) * 128], [(ones_f[:], d1[:])], ["ones_f", dn_], [PB[1]])
                for h in range(4):
                    STT(L_[:, h, :], banks[1][:, h * 128:(h + 1) * 128], BMT[:, h:h + 1], neg[:], ALU.add, ALU.add,
                        [PB[1], "ml.Bc"] + CN, ["ml.L"])
                P.op("dve", lambda e: e.tensor_reduce(out=RMAX[:], in_=L_[:], axis=AX.X, op=ALU.max), ["ml.L"], ["ml.RMAX"])
                TT(MT, RMAX[:], BM[:], ALU.max, ["ml.RMAX", "ml.BM"], ["ml.MT"])
                TS(NMT[:], MT, -1.0, None, ALU.mult, None, ["ml.MT"], ["ml.NMT"])
                for h in range(4):
                    ACT(Wt[:, h, :], L_[:, h, :], AF.Exp, ["ml.L", "ml.NMT"], ["ml.W"], bias=NMT[:, h:h + 1])
                TT(D2[:], BM[:], MT, ALU.subtract, ["ml.BM", "ml.MT"], ["ml.D2"])
                ACT(WPV[:], D2[:], AF.Exp, ["ml.D2"], ["ml.WPV"])
                ACT(ENM[:], NMT[:], AF.Exp, ["ml.NMT"], ["ml.ENM"])
                for h in range(4):
                    MM(banks[2][:, h * 128:(h + 1) * 128],
                       [(QT[:, 2 * h + dc, tsl], KT[:, 2 * h + dc, tsl]) for dc in range(2)],
                       [QTN[2 * h], QTN[2 * h + 1], KTN[2 * h], KTN[2 * h + 1]], [PB[2]])
                STT(Sb[:].rearrange("p h s -> p (h s)"), banks[2][:, 0:512], 1.0 / 16, Wt[:].rearrange("p h s -> p (h s)"),
                    ALU.mult, ALU.mult, [PB[2], "ml.W"], ["ml.S"])
                P.op("dve", lambda e: e.tensor_reduce(out=DSUM[:], in_=Sb[:], axis=AX.X, op=ALU.add), ["ml.S"], ["ml.DSUM"])
                for h in range(4):
                    TR(banks_b[3][:, h * 128:(h + 1) * 128], Sb[:, h, :], ident_b[:], ["ml.S", "ident_b"], [PB[3]])
                COPY(ST_[:].rearrange("p h s -> p (h s)"), banks_b[3][:, 0:512], [PB[3]], ["ml.ST"])
                VN = lambda h: "ml.VTM%d.%d" % (blk, h)
                KN = lambda h: "ml.KTM%d.%d" % (blk, h)
                for h in range(4):
                    bk = 4 + h // 2
                    MM(banks[bk][:, (h % 2) * 256:(h % 2) * 256 + 256], [(ST_[:, h, :], VTM[:, blk, h * 256:(h + 1) * 256])],
                       ["ml.ST", VN(h)], [PB[bk]])
                COPY(IA[:, 0:2, :].rearrange("p h v -> p (h v)"), banks[4][:, 0:512], [PB[4]], ["ml.IA"])
                COPY(IA[:, 2:4, :].rearrange("p h v -> p (h v)"), banks[5][:, 0:512], [PB[5]], ["ml.IA"])
                MM(banks[3][:, 0:8], [(G[:], BMT[:])], ["ml.Bc", "ml.MT"] + CN, [PB[3]])
                COPY(BL[:], banks[3][:, 0:8], [PB[3]], ["ml.BL"], eng="dve")
                TT(D2[:], BL[:, 0:4], Bc, ALU.subtract, ["ml.BL", "ml.Bc"], ["ml.D2"])
                TT(D3[:], IG, BL[:, 4:8], ALU.subtract, ["ml.GT", "ml.BL"], ["ml.D3"])
                TT(D2[:], D2[:], D3[:], ALU.add, ["ml.D2", "ml.D3"], ["ml.D2"])
                ACT(WIN[:], D2[:], AF.Exp, ["ml.D2"], ["ml.WIN"], bias=-math.log(16.0))
                TT(D3[:], BL[:, 0:4], MP[:], ALU.add, ["ml.BL", "ml.MP"], ["ml.D3"])
                TT(D3[:], D3[:], BL[:, 4:8], ALU.subtract, ["ml.D3", "ml.BL"], ["ml.D3"])
                ACT(GP[:], D3[:], AF.Exp, ["ml.D3"], ["ml.GP"])
                TT(RR[:], GP[:].unsqueeze(1).broadcast_to([128, nslot, 4]), last[:].unsqueeze(2).broadcast_to([128, nslot, 4]),
                   ALU.mult, ["ml.GP"] + CN, ["ml.RR"])
                MM(banks[3][:, 16:16 + nslot * 4], [(ones_f[:], RR[:].rearrange("p s h -> p (s h)"))], ["ones_f", "ml.RR"], [PB[3]])
                COPY(GPB[:].rearrange("p s h -> p (s h)"), banks[3][:, 16:16 + nslot * 4], [PB[3]], ["ml.GPB"], eng="dve")
                TT(WINM[:], WIN[:].unsqueeze(1).broadcast_to([128, nslot, 4]), smask[:].unsqueeze(2).broadcast_to([128, nslot, 4]),
                   ALU.mult, ["ml.WIN"] + CN, ["ml.WINM"])
                ki = 0
                for slot in range(nslot):
                    cin, cinn, nin, ninn = SS["c_in"](slot)
                    cout, coutn, nout, noutn, after = SS["c_out"](slot)
                    cb = CB[slot % len(CB)]; cbn = "ml.CB%d" % (slot % len(CB))
                    nb = NBb[slot % 2]; nbn = "ml.NBb%d" % (slot % 2)
                    COPY(cb[:].rearrange("p a h v -> p (a h v)"), cin.rearrange("p a h v -> p (a h v)"), [cinn], [cbn])
                    COPY(nb[:].rearrange("p a h -> p (a h)"), nin.rearrange("p a h -> p (a h)"), [ninn], [nbn], eng="dve")
                    t0 = blk * 128 + slot * Ls if m == "s" else blk * 128
                    ln = Ls if m == "s" else 128
                    o0 = slot * Ls if m == "s" else 0
                    for h in range(4):
                        qrd = [QTN[2 * h], QTN[2 * h + 1]]
                        for vc in range(2):
                            MM(banks[6 + vc][:, h * 128 + o0:h * 128 + o0 + ln],
                               [(cb[:, h, dc, vc * 128:(vc + 1) * 128], QT[:, 2 * h + dc, t0:t0 + ln]) for dc in range(2)],
                               [cbn] + qrd, [PB[6 + vc]])
                        MM(banks[0][0:1, h * 128 + o0:h * 128 + o0 + ln],
                           [(nb[:, h, dc:dc + 1], QT[:, 2 * h + dc, t0:t0 + ln]) for dc in range(2)], [nbn] + qrd, [PB[0]])
                    for h in range(4):
                        kw = KW[ki % 2]; kwn = "ml.KW%d" % (ki % 2)
                        bk = 4 + (ki % 2)
                        nc0 = 300 + 2 * (ki % 2)
                        ki += 1
                        TS(kw[:], KTM[:, blk, h * 256:(h + 1) * 256], WINM[:, slot, h:h + 1], None, ALU.mult, None,
                           [KN(h), "ml.WINM"], [kwn])
                        for dc in range(2):
                            MM(banks[bk][:, dc * 256:(dc + 1) * 256], [(kw[:, dc * 128:(dc + 1) * 128], VTM[:, blk, h * 256:(h + 1) * 256])],
                               [kwn, VN(h)], [PB[bk]])
                        for dc in range(2):
                            MM(banks[3][:, nc0 + dc:nc0 + dc + 1], [(kw[:, dc * 128:(dc + 1) * 128], ones_b[:, 0:1])],
                               [kwn, "ones_b"], [PB[3]])
                        STT(cout[:, h, :, :], cin[:, h, :, :], GPB[:, slot, h:h + 1],
                            banks[bk][:, 0:512].rearrange("p (a v) -> p a v", a=2), ALU.mult, ALU.add,
                            [cinn, "ml.GPB", PB[bk]], [coutn])
                        STT(nout[:, h, :], nin[:, h, :], GPB[:, slot, h:h + 1], banks[3][:, nc0:nc0 + 2], ALU.mult, ALU.add,
                            [ninn, "ml.GPB", PB[3]], [noutn])
                    if after is not None:
                        after()
                for vc in range(2):
                    COPY(IT[:, vc].rearrange("p h t -> p (h t)"), banks[6 + vc][:, 0:512], [PB[6 + vc]], ["ml.IT%d" % vc])
                COPY(ITn[:].rearrange("p h t -> p (h t)"), banks[0][0:1, 0:512], [PB[0]], ["ml.ITn"])
                for h in range(4):
                    bk = 6 + h // 2
                    for vc in range(2):
                        TR(banks[bk][:, (h % 2) * 256 + vc * 128:(h % 2) * 256 + vc * 128 + 128], IT[:, vc, h, :], ident_f[:],
                           ["ml.IT%d" % vc, "c_ident"], [PB[bk]])
                    TR(banks[1][:, h:h + 1], ITn[0:1, h, :], ident_f[0:1, 0:1], ["ml.ITn", "c_ident"], [PB[1]])
                COPY(QN[:], banks[1][:, 0:4], [PB[1]], ["ml.QN"], eng="dve")
                for h in range(4):
                    bk = 6 + h // 2
                    STT(NUM[:, h, :], banks[bk][:, (h % 2) * 256:(h % 2) * 256 + 256], WPV[:, h:h + 1], IA[:, h, :], ALU.mult, ALU.add,
                        [PB[bk], "ml.WPV", "ml.IA"], ["ml.NUM"])
                TT(DEN[:], WPV[:], QN[:], ALU.mult, ["ml.WPV", "ml.QN"], ["ml.DEN"])
                TT(DEN[:], DEN[:], DSUM[:], ALU.add, ["ml.DEN", "ml.DSUM"], ["ml.DEN"])
                ACT(DEN[:], DEN[:], AF.Abs, ["ml.DEN"], ["ml.DEN"])
                TT(DEN[:], DEN[:], ENM[:], ALU.max, ["ml.DEN", "ml.ENM"], ["ml.DEN"])
                P.op("dve", lambda e: e.reciprocal(out=RDEN[:], in_=DEN[:]), ["ml.DEN"], ["ml.RDEN"])
                for h in range(4):
                    P.op("act", lambda e, h=h: e.activation(out=JK[:], in_=NUM[:, h, :], func=AF.Square, scale=RDEN[:, h:h + 1],
                                                             accum_out=SSQ[:, h:h + 1]),
                         ["ml.NUM", "ml.RDEN"], ["ml.SSQ", "ml.JK"])
                ACT(RSTD[:], SSQ[:], AF.Ln, ["ml.SSQ"], ["ml.RSTD"], scale=1.0 / 256, bias=EPS)
                ACT(RSTD[:], RSTD[:], AF.Exp, ["ml.RSTD"], ["ml.RSTD"], scale=-0.5)
                TT(SCL[:], RDEN[:], RSTD[:], ALU.mult, ["ml.RDEN", "ml.RSTD"], ["ml.SCL"])
                for h in range(4):
                    STT(HG[:, h, :], NUM[:, h, :], SCL[:, h:h + 1], SMO[:, blk, h * 256:(h + 1) * 256], ALU.mult, ALU.mult,
                        ["ml.NUM", "ml.SCL", "ml.SMO%d.%d" % (blk, h)], ["ml.HG"])
                for h in range(4):
                    for vc in range(2):
                        TR(banks_b[2][:, (2 * h + vc) * 128:(2 * h + vc + 1) * 128], HG[:, h, vc * 128:(vc + 1) * 128], ident_b[:],
                           ["ml.HG", "ident_b"], [PB[2]])
                for c in range(8):
                    TS(HM[:, c, gb * 128:(gb + 1) * 128], banks_b[2][:, c * 128:(c + 1) * 128], mnorm[:, l, c:c + 1], None,
                       ALU.mult, None, [PB[2], "mnorm"], ["HM.%d" % c])
                mo, mon = SS["m_out"]()
                MM(banks[3][0:nseq, 100:104], [(last[:], MT)], ["ml.MT"] + CN, [PB[3]])
                COPY(mo, banks[3][0:nseq, 100:104], [PB[3]], [mon], eng="dve")
            P.barrier()
        S.reset(mk0)
    def merge(M, l, OG, YB, HM):
        T = M.T
        S = M.S; S.reset(); mk = S.mark()
        MB = alloc(S, [128, 16, T], BF16)
        SG = [alloc(S, [128, T]) for _ in range(4)]
        ACC = alloc(S, [128, T]); TM_ = alloc(S, [128, T])
        if M.m == "p":
            for i in range(2):
                extra_slots.append((S.take(8192), "ringx%d" % i))
        OGN = ["OG.%d" % i for i in range(8)]; YBN = ["YB.%d" % i for i in range(8)]; HMN = ["HM.%d" % i for i in range(8)]
        sgi = [0]

        def gate_tile(b, tp):
            return wload(W["w_in"][l, :, 7168 + b * D + 256 * tp:7168 + b * D + 256 * tp + 256].rearrange("(kc p) n -> p kc n", p=128),
                         [128, 16, 256])

        def br_tile(name, tp):
            return wload(W[name][l, :, 256 * tp:256 * tp + 256].rearrange("(kc p) n -> p kc n", p=128), [128, 8, 256])

        def gate(tw, bw, ch, bk):
            MM(banks[bk][:, 0:T], [(tw[:, kc, ch * 128:(ch + 1) * 128], M.H[:, kc, :]) for kc in range(16)], [bw] + HALL, [PB[bk]])
            sg = SG[sgi[0] % 4]; sgn = "mg.SG%d" % (sgi[0] % 4); sgi[0] += 1
            ACT(sg[:], banks[bk][:, 0:T], AF.Sigmoid, [PB[bk]], [sgn])
            return sg, sgn

        def branch(tw, bw, ch, bk, src, srcn):
            MM(banks[bk][:, 0:T], [(tw[:, kc, ch * 128:(ch + 1) * 128], src[:, kc, :]) for kc in range(8)], [bw] + srcn, [PB[bk]])

        GBK = [0, 1, 4, 5]; BBK = [2, 3, 6, 7]
        gi = [0]; bi_ = [0]

        def gbank():
            k = GBK[gi[0] % 4]; gi[0] += 1
            return k

        def bbank():
            k = BBK[bi_[0] % 4]; bi_[0] += 1
            return k

        for tp in range(8):
            tg, bg = gate_tile(0, tp)
            ta, ba = br_tile("w_hgrn_out", tp)
            for ch in range(2):
                sg, sgn = gate(tg, bg, ch, gbank())
                bk = bbank()
                branch(ta, ba, ch, bk, OG, OGN)
                j = 2 * tp + ch
                TT(M.FFA(j), sg[:], banks[bk][:, 0:T], ALU.mult, [sgn, PB[bk]], ["mg.ACC%d" % (j % 4)])
            tg, bg = gate_tile(1, tp)
            ta, ba = br_tile("w_s5_glu_a", tp)
            tb, bb = br_tile("w_s5_glu_b", tp)
            for ch in range(2):
                j = 2 * tp + ch
                sg, sgn = gate(tg, bg, ch, gbank())
                ka = bbank(); kb = bbank()
                branch(ta, ba, ch, ka, YB, YBN)
                branch(tb, bb, ch, kb, YB, YBN)
                ACT(TM_[:], banks[kb][:, 0:T], AF.Sigmoid, [PB[kb]], ["mg.TM"])
                TT(TM_[:], TM_[:], banks[ka][:, 0:T], ALU.mult, ["mg.TM", PB[ka]], ["mg.TM"])
                TT(TM_[:], TM_[:], sg[:], ALU.mult, ["mg.TM", sgn], ["mg.TM"])
                TT(M.FFA(j), M.FFA(j), TM_[:], ALU.add, ["mg.ACC%d" % (j % 4), "mg.TM"], ["mg.ACC%d" % (j % 4)])
            tg, bg = gate_tile(2, tp)
            ta, ba = br_tile("w_mlstm_out", tp)
            for ch in range(2):
                j = 2 * tp + ch
                sg, sgn = gate(tg, bg, ch, gbank())
                bk = bbank()
                branch(ta, ba, ch, bk, HM, HMN)
                TT(TM_[:], sg[:], banks[bk][:, 0:T], ALU.mult, [sgn, PB[bk]], ["mg.TM"])
                TT(MB[:, j, :], M.FFA(j), TM_[:], ALU.add, ["mg.ACC%d" % (j % 4), "mg.TM"], ["mg.MB%d" % j])
        P.barrier()
        MBN = ["mg.MB%d" % j for j in range(16)]
        bi = 0
        for tp in range(8):
            tw, bw = wload(W["w_out"][l, :, 256 * tp:256 * tp + 256].rearrange("(kc p) n -> p kc n", p=128), [128, 16, 256])
            for ch in range(2):
                j = 2 * tp + ch
                bk = bi % 4; bi += 1
                MM(banks[bk][:, 0:T], [(tw[:, kc, ch * 128:(ch + 1) * 128], MB[:, kc, :]) for kc in range(16)], [bw] + MBN, [PB[bk]])
                COPY(M.FF[:, j, :], banks[bk][:, 0:T], [PB[bk]], [fn_(j)])
        del extra_slots[:]
        P.barrier()
        S.reset(mk)

    def load_x(M, src):
        S = M.S; S.reset(); mk = S.mark()
        xin = alloc(S, [128, D])
        for blk in range(M.NB):
            DMA(xin[:], src[blk * 128:(blk + 1) * 128, :], [], ["io.xin"])
            for cg in range(4):
                bk = cg
                for c4 in range(4):
                    c = cg * 4 + c4
                    TR(banks[bk][:, c4 * 128:(c4 + 1) * 128], xin[:, c * 128:(c + 1) * 128], ident_f[:], ["io.xin", "c_ident"], [PB[bk]])
                COPY(M.X[:, cg * 4:(cg + 1) * 4, blk * 128:(blk + 1) * 128], banks[bk][:, 0:512].rearrange("p (c t) -> p c t", c=4),
                     [PB[bk]], [xn(cg * 4 + i) for i in range(4)])
        P.barrier()
        S.reset(mk)

    def store_y(M, dst):
        S = M.S; S.reset(); mk = S.mark()
        yo = [alloc(S, [128, D]) for _ in range(2)]
        for blk in range(M.NB):
            y = yo[blk % 2]; yn = "io.yo%d" % (blk % 2)
            for cg in range(4):
                bk = 4 + cg
                for c4 in range(4):
                    c = cg * 4 + c4
                    TR(banks[bk][:, c4 * 128:(c4 + 1) * 128], M.X[:, c, blk * 128:(blk + 1) * 128], ident_f[:], [xn(c), "c_ident"], [PB[bk]])
                COPY(y[:, cg * 512:(cg + 1) * 512], banks[bk][:, 0:512], [PB[bk]], [yn])
            DMA(dst[blk * 128:(blk + 1) * 128, :], y[:], [yn], [], key=yn)
        P.barrier()
        S.reset(mk)

    def layer(M, l, hooks):
        prenorm(M, l, 0)
        ffn(M, l, W["w_ffn1_up"], W["w_ffn1_down"])
        postnorm_residual(M, l, 1, True)
        prenorm(M, l, 2, barrier=True)
        OG = sbt([128, 8, M.T], BF16, M.FF_off)
        YB = sbt([128, 8, M.T], BF16, M.FF_off + 8 * M.T * 2)
        HM = sbt([128, 8, M.T], BF16, M.FF_off + 16 * M.T * 2)
        ACCt = sbt([128, 4, M.T], F32, M.FF_off + 24 * M.T * 2)
        M.FFA = lambda j: ACCt[:, j % 4, :]
        hgrn(M, l, OG, hooks["hg"])
        hooks["s5"](YB)
        mlstm(M, l, HM, hooks["ml"])
        merge(M, l, OG, YB, HM)
        postnorm_residual(M, l, 3, False)
        prenorm(M, l, 4)
        ffn(M, l, W["w_ffn2_up"], W["w_ffn2_down"])
        postnorm_residual(M, l, 5, True)

    if NT > 0:
        M = make_mode("p")
        M.FF_off = None
        M.FF_off = M.S.start - 16 * M.T * 4
        MEMSET(M.HGS[:].rearrange("p l h v -> p (l h v)"), 0.0, ["HGS%d.%d" % (l, h) for l in range(2) for h in range(8)])
        MEMSET(M.MLC[:].rearrange("p l h a v -> p (l h a v)"), 0.0, ["MLC0", "MLC1"])
        MEMSET(M.MLN[:].rearrange("p l h a -> p (l h a)"), 0.0, ["MLN0", "MLN1"])
        MEMSET(M.MST[:].rearrange("p l h -> p (l h)"), 0.0, ["MST0", "MST1"])
        MEMSET(M.XSr[:].rearrange("p l r -> p (l r)"), 0.0, ["XS0", "XS1"])
        MEMSET(M.XSi[:].rearrange("p l r -> p (l r)"), 0.0, ["XS0", "XS1"])
        MEMSET(M.CV[:].rearrange("p l j c -> p (l j c)"), 0.0, ["CV0", "CV1"])
        for ti in range(NT):
            load_x(M, xp[ti * 512:(ti + 1) * 512, :])
            for l in range(NL):
                def hg_st(hgp, slot, l=l):
                    aps = [M.HGS[:, l, 4 * hgp + hh, :] for hh in range(4)]
                    names = ["HGS%d.%d" % (l, 4 * hgp + hh) for hh in range(4)]
                    return aps, names, aps, names, None

                def s5_hook(YB, l=l):
                    s5(M, l, YB, lambda r: M.XSr[:, l, r:r + 1], lambda r: M.XSi[:, l, r:r + 1],
                       lambda r: M.XSr[:, l, r:r + 1], lambda r: M.XSi[:, l, r:r + 1], "XS%d" % l, "XS%d" % l)
                ml = {
                    "cv_in": lambda fc, l=l: M.CV[:, l, :, fc].unsqueeze(1), "cv_in_name": "CV%d" % l,
                    "cv_out": lambda fc, l=l: M.CV[:, l, :, fc].unsqueeze(1), "cv_out_name": "CV%d" % l,
                    "m_in": lambda l=l: (M.MST[0:1, l, :], "MST%d" % l),
                    "m_out": lambda l=l: (M.MST[0:1, l, :], "MST%d" % l),
                    "c_in": lambda slot, l=l: (M.MLC[:, l], "MLC%d" % l, M.MLN[:, l], "MLN%d" % l),
                    "c_out": lambda slot, l=l: (M.MLC[:, l], "MLC%d" % l, M.MLN[:, l], "MLN%d" % l, None),
                }
                layer(M, l, {"hg": hg_st, "s5": s5_hook, "ml": ml})
            store_y(M, yp[ti * 512:(ti + 1) * 512, :])
        for l in range(NL):
            DMA(p_hg[l].rearrange("h k v -> k h v"), M.HGS[:, l], ["HGS%d.%d" % (l, h) for h in range(8)], [], key="o_p_hg")
            DMA(p_c[l].rearrange("h (a p) v -> p (h a) v", p=128), M.MLC[:, l].rearrange("p h a v -> p (h a) v"), ["MLC%d" % l], [], key="o_p_c")
            DMA(p_n[l].rearrange("h (a p) -> p (h a)", p=128), M.MLN[:, l].rearrange("p h a -> p (h a)"), ["MLN%d" % l], [], key="o_p_n", slow=True)
            DMA(p_m[l:l + 1, :], M.MST[0:1, l, :], ["MST%d" % l], [], key="o_p_m")
            DMA(p_conv[l].rearrange("j (c p) -> p (j c)", p=128), M.CV[:, l].rearrange("p j c -> p (j c)"), ["CV%d" % l], [], key="o_p_cv", slow=True)
            S = M.S; S.reset()
            for part, (src, dst) in enumerate(((M.XSr, p_re), (M.XSi, p_im))):
                o = alloc(S, [32, 128])
                TR(banks[part][0:32, 0:128], src[:, l, :], ident_f[:], ["XS%d" % l, "c_ident"], [PB[part]])
                COPY(o[:], banks[part][0:32, 0:128], [PB[part]], ["io.xs%d" % part])
                DMA(dst[l], o[:], ["io.xs%d" % part], [], key="o_p_xs%d" % part)
            P.barrier()

    if SAMPLE:
        P.barrier()
        M = make_mode("s")
        M.FF_off = M.S.start - 16 * M.T * 4
        S = M.S
        HGB = [alloc(S, [128, 4, 128]) for _ in range(4)]
        CIN = [alloc(S, [128, 4, 2, 256]) for _ in range(3)]
        NINs = alloc(S, [128, 16, 4, 2]); NOUTs = alloc(S, [128, 16, 4, 2])
        MSI = alloc(S, [16, 4]); MSO = alloc(S, [16, 4])
        CVI = alloc(S, [128, 16, 3, 8]); CVO = alloc(S, [128, 16, 3, 8])
        XT = alloc(S, [16, 4096])
        XSs = [alloc(S, [128, 32, 16]) for _ in range(2)]
        XNs = [alloc(S, [128, 32, 16]) for _ in range(2)]
        M.S = Region(S.cur, S.start + S.size - S.cur)
        load_x(M, xs)
        for l in range(NL):
            hg_cnt = [0]

            def hg_st(hgp, slot, l=l):
                i = hg_cnt[0] % 4; hg_cnt[0] += 1
                buf = HGB[i]; bn = "HGB%d" % i
                DMA(buf[:], st_hg[l, slot, 4 * hgp:4 * hgp + 4].rearrange("h k v -> k h v"), [], [bn], q="poolq")
                aps = [buf[:, hh, :] for hh in range(4)]
                names = [bn] * 4

                def after():
                    DMA(s_hg[l, slot, 4 * hgp:4 * hgp + 4].rearrange("h k v -> k h v"), buf[:], [bn], [], key=bn + "o")
                return aps, names, aps, names, after

            def s5_hook(YB, l=l):
                for part, src in enumerate((st_re, st_im)):
                    DMA(XT[:], src[l], [], ["XT"])
                    for r in range(32):
                        TR(banks[part][:, r * 16:(r + 1) * 16], XT[:, r * 128:(r + 1) * 128], ident_f[0:16, 0:16], ["XT", "c_ident"], [PB[part]])
                    COPY(XSs[part][:].rearrange("p r b -> p (r b)"), banks[part][:, 0:512], [PB[part]], ["XSs"])
                s5(M, l, YB, lambda r: XSs[0][:, r, :], lambda r: XSs[1][:, r, :],
                   lambda r: XNs[0][:, r, :], lambda r: XNs[1][:, r, :], "XSs", "XNs")
                for part, dst in enumerate((s_re, s_im)):
                    for r4 in range(8):
                        bk = 2 + (r4 % 2)
                        for rr in range(4):
                            r = 4 * r4 + rr
                            TR(banks[bk][0:16, rr * 128:(rr + 1) * 128], XNs[part][:, r, :], ident_f[:], ["XNs", "c_ident"], [PB[bk]])
                        COPY(XT[:, r4 * 512:(r4 + 1) * 512], banks[bk][0:16, 0:512], [PB[bk]], ["XT"])
                    DMA(dst[l], XT[:], ["XT"], [], key="o_s_xs")
                P.barrier()

            DMA(NINs[:].rearrange("p b h a -> p (b h a)"), st_n[l].rearrange("b h (a p) -> p (b h a)", p=128), [], ["NIN"], slow=True)
            DMA(MSI[:], st_m[l], [], ["MSI"])
            DMA(CVI[:].rearrange("p b j c -> p (b j c)"), st_conv[l].rearrange("b j (c p) -> p (b j c)", p=128), [], ["CVI"], slow=True)
            c_cnt = [0]
            cmap = {}

            def c_in(slot, l=l):
                if slot not in cmap:
                    i = c_cnt[0] % 3; c_cnt[0] += 1
                    cmap[slot] = i
                    DMA(CIN[i][:].rearrange("p h a v -> p (h a) v"), st_c[l, slot].rearrange("h (a p) v -> p (h a) v", p=128), [], ["CIN%d" % i], q="poolq")
                i = cmap[slot]
                return CIN[i][:], "CIN%d" % i, NINs[:, slot], "NIN"

            def c_out(slot, l=l):
                i = cmap[slot]

                def after():
                    DMA(s_c[l, slot].rearrange("h (a p) v -> p (h a) v", p=128), CIN[i][:].rearrange("p h a v -> p (h a) v"), ["CIN%d" % i], [], key="CIN%do" % i)
                return CIN[i][:], "CIN%d" % i, NOUTs[:, slot], "NOUT", after
            ml = {
                "cv_in": lambda fc: CVI[:, :, :, fc], "cv_in_name": "CVI",
                "cv_out": lambda fc: CVO[:, :, :, fc], "cv_out_name": "CVO",
                "m_in": lambda: (MSI[:], "MSI"),
                "m_out": lambda: (MSO[:], "MSO"),
                "c_in": c_in, "c_out": c_out,
            }
            layer(M, l, {"hg": hg_st, "s5": s5_hook, "ml": ml})
            DMA(s_n[l].rearrange("b h (a p) -> p (b h a)", p=128), NOUTs[:].rearrange("p b h a -> p (b h a)"), ["NOUT"], [], key="o_s_n", slow=True)
            DMA(s_m[l], MSO[:], ["MSO"], [], key="o_s_m")
            DMA(s_conv[l].rearrange("b j (c p) -> p (b j c)", p=128), CVO[:].rearrange("p b j c -> p (b j c)"), ["CVO"], [], key="o_s_cv", slow=True)
            P.barrier()
        store_y(M, ys)

    nops, nsem = P.emit()
    return nc, consts, dbg_outs, (nops, nsem)


_CACHE = {}


def _get_program(NT=4, SAMPLE=True):
    key = (NT, SAMPLE)
    if key not in _CACHE:
        _CACHE[key] = build_program(NT, SAMPLE)
    return _CACHE[key]


WNAMES = ["norm_gains", "w_ffn1_up", "w_ffn1_down", "w_in", "hgrn_lower_bounds", "hgrn_norm", "w_hgrn_out",
          "s5_a_re", "s5_a_im", "s5_log_dt", "s5_b_re", "s5_b_im", "s5_c_re", "s5_c_im", "s5_d", "w_s5_glu_a",
          "w_s5_glu_b", "mlstm_conv_w", "mlstm_conv_b", "mlstm_wq", "mlstm_wk", "mlstm_wv", "mlstm_w_gates",
          "mlstm_b_gates", "mlstm_norm", "w_mlstm_out", "w_out", "w_ffn2_up", "w_ffn2_down"]


def kernel(**inp):
    f32 = lambda a: np.ascontiguousarray(np.asarray(a, dtype=np.float32))
    nc, consts, _, _ = _get_program(4, True)
    ncore = 8
    shared = {k: f32(inp[k]) for k in WNAMES}
    for k, v in consts.items():
        shared["c_" + k] = v
    xpr = f32(inp["x_prompt"])
    xsm = f32(inp["x_sample"])
    sts = {"st_hg": f32(inp["state_hgrn"]), "st_re": f32(inp["state_s5_re"]).reshape(2, 128, 4096),
           "st_im": f32(inp["state_s5_im"]).reshape(2, 128, 4096), "st_c": f32(inp["state_mlstm_c"]),
           "st_n": f32(inp["state_mlstm_n"]), "st_m": f32(inp["state_mlstm_m"]), "st_conv": f32(inp["state_mlstm_conv"])}
    in_maps = []
    for c in range(ncore):
        m = dict(shared)
        m["xp"] = xpr[c % 4]
        m["xs"] = xsm[16 * c:16 * c + 16].reshape(128, D)
        for k, v in sts.items():
            m[k] = np.ascontiguousarray(v[:, 16 * c:16 * c + 16])
        in_maps.append(m)
    res = run_bass_kernel_spmd(nc, in_maps, core_ids=list(range(ncore)))
    R = res.results
    y_prompt = np.stack([R[j]["yp"] for j in range(4)], 0)
    y_sample = np.concatenate([R[c]["ys"].reshape(16, 8, D) for c in range(ncore)], 0)

    def pst(name, shape):
        return np.stack([R[j][name] for j in range(4)], 1).reshape(shape)

    def sst(name, shape):
        return np.concatenate([R[c][name] for c in range(ncore)], 1).reshape(shape)
    outs = (y_prompt, y_sample,
            pst("p_hg", (2, 4, 8, 128, 128)), pst("p_re", (2, 4, 64, 64)), pst("p_im", (2, 4, 64, 64)),
            pst("p_c", (2, 4, 4, 256, 256)), pst("p_n", (2, 4, 4, 256)), pst("p_m", (2, 4, 4)), pst("p_conv", (2, 4, 3, 1024)),
            sst("s_hg", (2, 128, 8, 128, 128)), sst("s_re", (2, 128, 64, 64)), sst("s_im", (2, 128, 64, 64)),
            sst("s_c", (2, 128, 4, 256, 256)), sst("s_n", (2, 128, 4, 256)), sst("s_m", (2, 128, 4)),
            sst("s_conv", (2, 128, 3, 1024)))
    return tuple(np.ascontiguousarray(o.astype(np.float32)) for o in outs)
```
